# Optimizing a Trainium2 kernel written in Bass

```python
import math
import jax, jax.numpy as jnp
from jax import lax
import numpy as np

D_MODEL = 1024
BATCH = 32
SEQ = 2048
DEPTH = 1

HEAD_DIM = 64
MOBA_HEADS = 8
MOBA_WIDTH = MOBA_HEADS * HEAD_DIM
MOBA_BLOCK = 256
MOBA_TOPK = 3
MOBA_Q_CHUNK = 16
DIFF_HEADS = 4
DIFF_QK_DIM = HEAD_DIM
DIFF_V_DIM = 2 * HEAD_DIM
DIFF_WIDTH = DIFF_HEADS * DIFF_V_DIM
MIX_WIDTH = MOBA_WIDTH + DIFF_WIDTH
IN_SPLITS = (MOBA_WIDTH, MOBA_WIDTH, MOBA_WIDTH,
             DIFF_HEADS * 2 * DIFF_QK_DIM, DIFF_HEADS * 2 * DIFF_QK_DIM, DIFF_WIDTH)
IN_COLS = sum(IN_SPLITS)
ATTN_Q_BLOCK = 128
ROPE_THETA = 10000.0
EPS = 1e-6
NEG_INF = -1e30
PEER_HEADS = 8
PEER_NKEYS = 128
PEER_EXPERTS = PEER_NKEYS * PEER_NKEYS
PEER_DK = 256
PEER_HALF = PEER_DK // 2
PEER_TOPK = 16
PEER_TOK_CHUNK = 128

kernel_name = "hymba_moba_diffattn_peer_layer"


def rms_norm(x, gain):
    xf = x.astype(jnp.float32)
    y = xf * lax.rsqrt(jnp.mean(xf * xf, axis=-1, keepdims=True) + EPS)
    return (y * gain.astype(jnp.float32)).astype(x.dtype)


def rope_tables(seq):
    pos = jnp.arange(seq, dtype=jnp.float32)
    inv = 1.0 / (ROPE_THETA ** (jnp.arange(0, HEAD_DIM, 2, dtype=jnp.float32) / HEAD_DIM))
    ang = pos[:, None] * inv[None, :]
    return jnp.cos(ang), jnp.sin(ang)


def apply_rope(x, cos, sin):
    x1, x2 = jnp.split(x, 2, axis=-1)
    c = cos.astype(x.dtype)
    s = sin.astype(x.dtype)
    return jnp.concatenate([x1 * c - x2 * s, x2 * c + x1 * s], axis=-1)


def moba_attention(q, k, v):
    B, H, S, Dh = q.shape
    nb = -(-S // MOBA_BLOCK)
    pad = nb * MOBA_BLOCK - S
    kp = jnp.pad(k, ((0, 0), (0, 0), (0, pad), (0, 0)))
    vp = jnp.pad(v, ((0, 0), (0, 0), (0, pad), (0, 0)))
    k_blocks = kp.reshape(B, H, nb, MOBA_BLOCK, Dh)
    v_blocks = vp.reshape(B, H, nb, MOBA_BLOCK, Dh)
    k_mean = jnp.mean(k_blocks.astype(jnp.float32), axis=3)
    gate = jnp.einsum('bhsd,bhnd->bhsn', q.astype(jnp.float32), k_mean)
    q_block = jnp.arange(S) // MOBA_BLOCK
    past = jnp.arange(nb)[None, :] < q_block[:, None]
    gate = jnp.where(past, gate, NEG_INF)
    k_eff = min(MOBA_TOPK, nb)
    _, sel = lax.top_k(gate, k_eff)
    valid = sel < q_block[:, None]

    n_chunks = S // MOBA_Q_CHUNK

    def to_chunks(t):
        t = t.reshape(B, H, n_chunks, MOBA_Q_CHUNK, *t.shape[3:])
        return jnp.moveaxis(t, 2, 0)

    b_idx = jnp.arange(B)[:, None, None, None]
    h_idx = jnp.arange(H)[None, :, None, None]
    scale = Dh ** -0.5
    key_off = jnp.arange(MOBA_BLOCK)

    def chunk_fn(args):
        c, qc, selc, validc = args
        q_pos = c * MOBA_Q_CHUNK + jnp.arange(MOBA_Q_CHUNK)
        blk = (c * MOBA_Q_CHUNK) // MOBA_BLOCK
        k_sel = k_blocks[b_idx, h_idx, selc]
        v_sel = v_blocks[b_idx, h_idx, selc]
        s_sel = jnp.einsum('bhcd,bhckld->bhckl', qc, k_sel).astype(jnp.float32) * scale
        s_sel = jnp.where(validc[..., None], s_sel, NEG_INF)
        k_own = lax.dynamic_slice_in_dim(kp, blk * MOBA_BLOCK, MOBA_BLOCK, axis=2)
        v_own = lax.dynamic_slice_in_dim(vp, blk * MOBA_BLOCK, MOBA_BLOCK, axis=2)
        s_own = jnp.einsum('bhcd,bhld->bhcl', qc, k_own).astype(jnp.float32) * scale
        own_pos = blk * MOBA_BLOCK + key_off
        s_own = jnp.where(own_pos[None, :] <= q_pos[:, None], s_own, NEG_INF)
        n_sel = k_eff * MOBA_BLOCK
        logits = jnp.concatenate([s_sel.reshape(B, H, MOBA_Q_CHUNK, n_sel), s_own], axis=-1)
        p = jax.nn.softmax(logits, axis=-1).astype(v.dtype)
        p_sel = p[..., :n_sel].reshape(B, H, MOBA_Q_CHUNK, k_eff, MOBA_BLOCK)
        p_own = p[..., n_sel:]
        return (jnp.einsum('bhckl,bhckld->bhcd', p_sel, v_sel)
                + jnp.einsum('bhcl,bhld->bhcd', p_own, v_own))

    outs = lax.map(chunk_fn, (jnp.arange(n_chunks), to_chunks(q), to_chunks(sel), to_chunks(valid)))
    return jnp.moveaxis(outs, 0, 2).reshape(B, H, S, Dh)


def diff_attention(q, k, v, lam):
    B, H, _, S, Dk = q.shape
    Dv = v.shape[-1]
    nqb = S // ATTN_Q_BLOCK
    scale = Dk ** -0.5
    key_pos = jnp.arange(S)
    qb = jnp.moveaxis(q.reshape(B, H, 2, nqb, ATTN_Q_BLOCK, Dk), 3, 0)

    def block_fn(args):
        i, qi = args
        q_pos = i * ATTN_Q_BLOCK + jnp.arange(ATTN_Q_BLOCK)
        s = jnp.einsum('bhmqd,bhmkd->bhmqk', qi, k).astype(jnp.float32) * scale
        s = jnp.where(key_pos[None, :] <= q_pos[:, None], s, NEG_INF)
        p = jax.nn.softmax(s, axis=-1)
        a = p[:, :, 0] - lam * p[:, :, 1]
        return jnp.einsum('bhqk,bhkd->bhqd', a.astype(v.dtype), v)

    outs = lax.map(block_fn, (jnp.arange(nqb), qb))
    return jnp.moveaxis(outs, 0, 2).reshape(B, H, S, Dv)


def peer_ffn(x, w_query, sub_keys, expert_down, expert_up):
    B, S, D = x.shape
    T = B * S
    n_chunks = T // PEER_TOK_CHUNK
    xt = x.reshape(n_chunks, PEER_TOK_CHUNK, D)

    def chunk_fn(xc):
        C = xc.shape[0]
        q = (xc @ w_query).reshape(C, PEER_HEADS, 2, PEER_HALF)
        sc = jnp.einsum('thpd,hpnd->thpn', q, sub_keys).astype(jnp.float32)
        top_s, top_i = lax.top_k(sc, PEER_TOPK)
        cand_s = (top_s[:, :, 0, :, None] + top_s[:, :, 1, None, :]).reshape(C, PEER_HEADS, PEER_TOPK * PEER_TOPK)
        cand_i = (top_i[:, :, 0, :, None] * PEER_NKEYS + top_i[:, :, 1, None, :]).reshape(C, PEER_HEADS, PEER_TOPK * PEER_TOPK)
        best_s, best_pos = lax.top_k(cand_s, PEER_TOPK)
        experts = jnp.take_along_axis(cand_i, best_pos, axis=-1)
        gates = jax.nn.softmax(best_s, axis=-1)
        u = expert_down[experts]
        vv = expert_up[experts]
        act = jax.nn.gelu(jnp.einsum('cd,chkd->chk', xc, u).astype(jnp.float32), approximate=False)
        w = (gates * act).astype(xc.dtype)
        return jnp.einsum('chk,chkd->cd', w, vv)

    out = lax.map(chunk_fn, xt)
    return out.reshape(B, S, D)


def setup_inputs(seed: int = 0) -> dict:
    key = jax.random.key(seed)
    ks = jax.random.split(key, 20)
    f32 = jnp.float32

    def nrm(k, shape, scale):
        return jax.random.normal(k, shape, f32) * scale

    def gain(k, shape):
        return 1.0 + 0.01 * jax.random.normal(k, shape, f32)

    L = DEPTH
    return {
        "x": jax.random.normal(ks[0], (BATCH, SEQ, D_MODEL), f32),
        "attn_norm": gain(ks[1], (L, D_MODEL)),
        "w_in": nrm(ks[2], (L, D_MODEL, IN_COLS), D_MODEL ** -0.5),
        "q_norm_moba": gain(ks[3], (L, HEAD_DIM)),
        "k_norm_moba": gain(ks[4], (L, HEAD_DIM)),
        "q_norm_diff": gain(ks[5], (L, 2, DIFF_QK_DIM)),
        "k_norm_diff": gain(ks[6], (L, 2, DIFF_QK_DIM)),
        "lambda_q1": nrm(ks[7], (L, DIFF_QK_DIM), 0.1),
        "lambda_k1": nrm(ks[8], (L, DIFF_QK_DIM), 0.1),
        "lambda_q2": nrm(ks[9], (L, DIFF_QK_DIM), 0.1),
        "lambda_k2": nrm(ks[10], (L, DIFF_QK_DIM), 0.1),
        "moba_out_gain": gain(ks[11], (L, MOBA_HEADS, HEAD_DIM)),
        "diff_out_gain": gain(ks[12], (L, DIFF_HEADS, DIFF_V_DIM)),
        "w_out": nrm(ks[13], (L, MIX_WIDTH, D_MODEL), MIX_WIDTH ** -0.5),
        "ffn_norm": gain(ks[14], (L, D_MODEL)),
        "peer_query": nrm(ks[15], (L, D_MODEL, PEER_HEADS * PEER_DK), D_MODEL ** -0.5),
        "peer_sub_keys": nrm(ks[16], (L, PEER_HEADS, 2, PEER_NKEYS, PEER_HALF), PEER_HALF ** -0.5),
        "peer_down": nrm(ks[17], (L, PEER_EXPERTS, D_MODEL), D_MODEL ** -0.5),
        "peer_up": nrm(ks[18], (L, PEER_EXPERTS, D_MODEL), (PEER_HEADS * PEER_TOPK) ** -0.5),
    }


def reference(x, attn_norm, w_in, q_norm_moba, k_norm_moba, q_norm_diff, k_norm_diff,
              lambda_q1, lambda_k1, lambda_q2, lambda_k2, moba_out_gain, diff_out_gain,
              w_out, ffn_norm, peer_query, peer_sub_keys, peer_down, peer_up):
    B, S, D = x.shape
    cos, sin = rope_tables(S)
    split_pts = [int(p) for p in np.cumsum(IN_SPLITS)[:-1]]
    for i in range(DEPTH):
        h = rms_norm(x, attn_norm[i])
        proj = h @ w_in[i]
        mq, mk, mv, dq, dk, dv = jnp.split(proj, split_pts, axis=-1)
        mq = mq.reshape(B, S, MOBA_HEADS, HEAD_DIM).transpose(0, 2, 1, 3)
        mk = mk.reshape(B, S, MOBA_HEADS, HEAD_DIM).transpose(0, 2, 1, 3)
        mv = mv.reshape(B, S, MOBA_HEADS, HEAD_DIM).transpose(0, 2, 1, 3)
        mq = apply_rope(rms_norm(mq, q_norm_moba[i]), cos, sin)
        mk = apply_rope(rms_norm(mk, k_norm_moba[i]), cos, sin)
        a_out = moba_attention(mq, mk, mv)
        a_out = rms_norm(a_out.transpose(0, 2, 1, 3), moba_out_gain[i])
        dq = dq.reshape(B, S, DIFF_HEADS, 2, DIFF_QK_DIM).transpose(0, 2, 3, 1, 4)
        dk = dk.reshape(B, S, DIFF_HEADS, 2, DIFF_QK_DIM).transpose(0, 2, 3, 1, 4)
        dv = dv.reshape(B, S, DIFF_HEADS, DIFF_V_DIM).transpose(0, 2, 1, 3)
        dq = apply_rope(rms_norm(dq, q_norm_diff[i][:, None, :]), cos, sin)
        dk = apply_rope(rms_norm(dk, k_norm_diff[i][:, None, :]), cos, sin)
        lambda_init = 0.8 - 0.6 * math.exp(-0.3 * i)
        lam = (jnp.exp(jnp.sum(lambda_q1[i].astype(jnp.float32) * lambda_k1[i].astype(jnp.float32)))
               - jnp.exp(jnp.sum(lambda_q2[i].astype(jnp.float32) * lambda_k2[i].astype(jnp.float32)))
               + lambda_init)
        b_out = diff_attention(dq, dk, dv, lam)
        b_out = rms_norm(b_out.transpose(0, 2, 1, 3), diff_out_gain[i]) * (1.0 - lambda_init)
        mixed = jnp.concatenate([a_out.reshape(B, S, MOBA_WIDTH),
                                 b_out.reshape(B, S, DIFF_WIDTH).astype(a_out.dtype)], axis=-1)
        x = x + mixed @ w_out[i]
        x = x + peer_ffn(rms_norm(x, ffn_norm[i]), peer_query[i], peer_sub_keys[i], peer_down[i], peer_up[i])
    return x
```

```python
import math
from contextlib import ExitStack

import numpy as np
import concourse.bass as bass
import concourse.mybir as mybir
from concourse.bass_utils import run_bass_kernel_spmd

F32 = mybir.dt.float32
BF16 = mybir.dt.bfloat16
U32 = mybir.dt.uint32
U8 = mybir.dt.uint8
AF = mybir.ActivationFunctionType
ALU = mybir.AluOpType
AX = mybir.AxisListType

ENGS = ["pe", "dve", "act", "pool", "sp"]
N_CORES = 8
D = 1024
S = 2048
NT = S // 128
EPS = 1e-6
NEG = -30000.0


class Op:
    __slots__ = ("eng", "fn", "idx", "deps", "is_dma", "dsem", "dval", "signal", "sval")

    def __init__(self, eng, fn, idx, is_dma):
        self.eng = eng
        self.fn = fn
        self.idx = idx
        self.deps = []
        self.is_dma = is_dma
        self.dsem = None
        self.dval = 0
        self.signal = False
        self.sval = 0


class Prog:
    def __init__(self, nc):
        self.nc = nc
        self.ops = {e: [] for e in ENGS}
        self.last_w = {}
        self.readers = {}
        self.seen = {e: {} for e in ENGS}
        self.dma_sems = {}
        self.barrier_deps = []
        self.dma_ops = []

    def _skey(self, dep):
        if dep.is_dma:
            return ("d", dep.dsem), dep.dval
        return ("e", dep.eng), dep.idx

    def op(self, eng, fn, r=(), w=(), dma=None):
        lst = self.ops[eng]
        o = Op(eng, fn, len(lst), dma is not None)
        if dma is not None:
            ent = self.dma_sems.setdefault(dma, [len(self.dma_sems), 0])
            ent[1] += 16
            o.dsem = dma
            o.dval = ent[1]
            self.dma_ops.append(o)
        cand = {}

        def add(dep):
            if dep is None:
                return
            if (not dep.is_dma) and dep.eng == eng and eng == "pe":
                return
            k, v = self._skey(dep)
            if k not in cand or cand[k][0] < v:
                cand[k] = (v, dep)

        for d in self.barrier_deps:
            add(d)
        for k in r:
            add(self.last_w.get(k))
        for k in w:
            add(self.last_w.get(k))
            for rd in self.readers.get(k, ()):
                add(rd)
        seen = self.seen[eng]
        for k, (v, dep) in cand.items():
            if seen.get(k, -1) >= v:
                continue
            seen[k] = v
            o.deps.append(dep)
            if not dep.is_dma:
                dep.signal = True
        for k in r:
            self.readers.setdefault(k, []).append(o)
        for k in w:
            self.last_w[k] = o
            self.readers[k] = []
        lst.append(o)
        return o

    def barrier(self):
        deps = [lst[-1] for lst in self.ops.values() if lst]
        deps = [d for d in deps if not d.is_dma]
        self.barrier_deps = deps + list(self.dma_ops)
        self.dma_ops = []

    def emit(self, final_ops):
        nc = self.nc
        with ExitStack() as es:
            esem = {e: es.enter_context(nc.semaphore("s_" + e)) for e in ENGS}
            dsem = {k: es.enter_context(nc.semaphore("d%d" % v[0])) for k, v in self.dma_sems.items()}
            for o in final_ops:
                if not o.is_dma:
                    o.signal = True
            for e in ENGS:
                c = 0
                for o in self.ops[e]:
                    if o.signal and not o.is_dma:
                        c += 1
                        o.sval = c
            block = es.enter_context(nc.Block())
            reg = {"pe": block.tensor, "dve": block.vector, "act": block.scalar,
                   "pool": block.gpsimd, "sp": block.sync}

            def make(e):
                ops = self.ops[e]

                def body(eng):
                    def wait(d):
                        if d.is_dma:
                            eng.wait_ge(dsem[d.dsem], d.dval)
                        else:
                            eng.wait_ge(esem[d.eng], d.sval)
                    for o in ops:
                        for d in o.deps:
                            wait(d)
                        meth, args, kw = o.fn
                        ins = getattr(eng, meth)(*args, **kw)
                        if o.is_dma:
                            ins.then_inc(dsem[o.dsem], 16)
                        elif o.signal:
                            ins.then_inc(esem[e], 1)
                    if e == "sp":
                        for d in final_ops:
                            wait(d)
                return body

            for e in ENGS:
                reg[e](make(e))


class Arena:
    def __init__(self, tile, size):
        self.t = tile
        self.size = size
        self.off = 0

    def alloc(self, shape, dt, nbytes_el):
        n = 1
        for s in shape:
            n *= s
        nb = n * nbytes_el
        nb_al = (nb + 63) // 64 * 64
        assert self.off + nb_al <= self.size, ("arena overflow", self.off, nb_al, self.size)
        ap = self.t[:, self.off:self.off + nb]
        self.off += nb_al
        if dt is not U8:
            ap = ap.bitcast(dt)
        if len(shape) == 2:
            ap = ap.rearrange("p (a b) -> p a b", b=shape[1])
        elif len(shape) == 3:
            ap = ap.rearrange("p (a b c) -> p a b c", b=shape[1], c=shape[2])
        return ap


def rope_consts():
    pos = np.arange(S, dtype=np.float32)
    inv = (1.0 / (np.float32(10000.0) ** (np.arange(0, 64, 2, dtype=np.float32) / np.float32(64)))).astype(np.float32)
    ang = (pos[:, None] * inv[None, :]).astype(np.float32)
    cos = np.cos(ang).astype(np.float32).reshape(NT, 128, 32).transpose(1, 0, 2)
    sin = np.sin(ang).astype(np.float32).reshape(NT, 128, 32).transpose(1, 0, 2)
    return np.ascontiguousarray(cos), np.ascontiguousarray(sin)


def host_consts():
    cos, sin = rope_consts()
    k = np.arange(128)[:, None]
    q = np.arange(128)[None, :]
    tri = np.where(k <= q, 0.0, NEG).astype(np.float32)
    kaug = np.zeros((64, S), np.float32)
    for n in range(8):
        kaug[n, n * 256:(n + 1) * 256] = 1.0
    past = np.zeros((NT, 8), np.float32)
    own = np.full((NT, 8), -1e30, np.float32)
    for t in range(NT):
        qb = t // 2
        for n in range(8):
            if n >= qb:
                past[t, n] = -1e30
            if n == qb or (qb <= 3 and n <= qb):
                own[t, n] = 0.0
    past = np.ascontiguousarray(np.broadcast_to(past[None], (128, NT, 8))).astype(np.float32)
    own = np.ascontiguousarray(np.broadcast_to(own[None], (128, NT, 8))).astype(np.float32)
    ident = np.eye(128, dtype=np.float32)
    iota = np.ascontiguousarray(np.broadcast_to(np.arange(128, dtype=np.float32)[None], (128, 128)))
    return {"c_iota": iota, "c_cos": cos, "c_sin": sin, "c_tri": tri, "c_kaug": kaug, "c_past": past,
            "c_own": own, "c_ident": ident}


def build(B_loc, do_peer=True, do_attn=True, n_groups=None):
    nc = bass.Bass("TRN2", target_bir_lowering=False)

    def din(name, shape, dt=F32):
        return nc.dram_tensor(name, list(shape), dt, kind="ExternalInput").ap()

    x = din("x", [B_loc, S, D])
    attn_norm = din("attn_norm", [1, D])
    w_in = din("w_in", [D, 3072])
    qnm = din("q_norm_moba", [1, 64])
    knm = din("k_norm_moba", [1, 64])
    qnd = din("q_norm_diff", [2, 64])
    knd = din("k_norm_diff", [2, 64])
    lq1 = din("lambda_q1", [1, 64])
    lk1 = din("lambda_k1", [1, 64])
    lq2 = din("lambda_q2", [1, 64])
    lk2 = din("lambda_k2", [1, 64])
    mog = din("moba_out_gain", [1, 512])
    dog = din("diff_out_gain", [1, 512])
    w_out = din("w_out", [D, D])
    c_cos = din("c_cos", [128, NT, 32])
    c_sin = din("c_sin", [128, NT, 32])
    c_tri = din("c_tri", [128, 128])
    c_kaug = din("c_kaug", [64, S])
    c_past = din("c_past", [128, NT, 8])
    c_own = din("c_own", [128, NT, 8])
    c_ident = din("c_ident", [128, 128])
    ffn_norm = din("ffn_norm", [1, D])
    peer_query = din("peer_query", [D, 2048])
    peer_sub_keys = din("peer_sub_keys", [2048, 128])
    peer_down = din("peer_down", [16384, D])
    peer_up = din("peer_up", [16384, D])
    c_iota = din("c_iota", [128, 128])
    downT_s = nc.dram_tensor("downT_s", [128, 128, 8, 128], BF16).ap()
    up_s = nc.dram_tensor("up_s", [128, 128, D], BF16).ap()
    out = nc.dram_tensor("out", [B_loc, S, D], F32, kind="ExternalOutput").ap()

    P = Prog(nc)
    with ExitStack() as es:
        ARENA_BYTES = 212000
        arena_t = es.enter_context(nc.sbuf_tensor("arena", [128, ARENA_BYTES], U8))
        A = Arena(arena_t, ARENA_BYTES)

        def sb(shape, dt=F32):
            return A.alloc(shape, dt, 2 if dt is BF16 else 4)

        def ps(name, shape, dt=F32):
            return es.enter_context(nc.psum_tensor(name, shape, dt))

        S0 = ps("S0", [128, 512])
        S1 = ps("S1", [128, 512])
        acc = [ps("acc0", [128, 4, 256]), ps("acc1", [128, 4, 256])]
        pj = ps("pj", [128, 512])
        ptr = ps("ptr", [128, 1024], BF16)
        Sb = [S0, S1]

        ident_f = sb([128]); ident_b = sb([128], BF16)
        epst = sb([1])
        mark_shared = A.off
        tri_b = sb([128], BF16)
        cos_t = sb([NT, 32]); sin_t = sb([NT, 32])
        past_t = sb([NT, 8]); own_t = sb([NT, 8])
        g1 = sb([D])
        gq_m = sb([256]); gq_d = sb([256])
        og_m = sb([512]); og_d = sb([512])
        lam4 = sb([4, 64]); lamt = sb([2, 64]); lams = sb([2]); lame = sb([2]); neglam = sb([1])
        wout = sb([8, D], BF16)

        def I(eng, meth, *args, r=(), w=(), dma=None, **kw):
            return P.op(eng, (meth, args, kw), r=r, w=w, dma=dma)

        def dma(eng, out_ap, in_ap, key, r=(), w=()):
            return I(eng, "dma_start", out=out_ap, in_=in_ap, r=r, w=w, dma=key)

        dma("sp", ident_f, c_ident, "ident_f", w=["ident_f"])
        dma("pool", ident_b, c_ident, "ident_b", w=["ident_b"])
        dma("pool", tri_b, c_tri, "tri_b", w=["tri_b"])
        dma("sp", cos_t, c_cos, "cos_t", w=["cos_t"])
        dma("sp", sin_t, c_sin, "sin_t", w=["sin_t"])
        dma("sp", past_t, c_past, "past_t", w=["past_t"])
        dma("sp", own_t, c_own, "own_t", w=["own_t"])
        dma("sp", g1, attn_norm[0:1, :].to_broadcast([128, D]), "g1", w=["g1"])
        for i in range(2):
            dma("sp", gq_m[:, i * 64:(i + 1) * 64], qnm[0:1, :].to_broadcast([128, 64]), "gq_m", w=["gq_m"])
            dma("sp", gq_m[:, 128 + i * 64:128 + (i + 1) * 64], knm[0:1, :].to_broadcast([128, 64]), "gq_m", w=["gq_m"])
            dma("sp", gq_d[:, i * 64:(i + 1) * 64], qnd[i:i + 1, :].to_broadcast([128, 64]), "gq_d", w=["gq_d"])
            dma("sp", gq_d[:, 128 + i * 64:128 + (i + 1) * 64], knd[i:i + 1, :].to_broadcast([128, 64]), "gq_d", w=["gq_d"])
        dma("sp", og_m, mog[0:1, :].to_broadcast([128, 512]), "og_m", w=["og_m"])
        dma("sp", og_d, dog[0:1, :].to_broadcast([128, 512]), "og_d", w=["og_d"])
        for i, v in enumerate((lq1, lk1, lq2, lk2)):
            dma("sp", lam4[:, i, :], v[0:1, :].to_broadcast([128, 64]), "lam4", w=["lam4"])
        dma("pool", wout, w_out.rearrange("(c p) n -> p c n", p=128), "wout", w=["wout"])
        I("pool", "memset", epst, EPS, w=["epst"])
        I("dve", "tensor_tensor", out=lamt, in0=lam4[:, 0:4:2, :], in1=lam4[:, 1:4:2, :], op=ALU.mult,
             r=["lam4"], w=["lamt"])
        I("dve", "tensor_reduce", out=lams, in_=lamt, axis=AX.X, op=ALU.add, r=["lamt"], w=["lams"])
        I("act", "activation", out=lame, in_=lams, func=AF.Exp, r=["lams"], w=["lame"])
        I("dve", "tensor_tensor", out=neglam, in0=lame[:, 1:2], in1=lame[:, 0:1], op=ALU.subtract,
             r=["lame"], w=["neglam"])
        I("dve", "tensor_scalar_add", neglam, neglam, -0.2, r=["neglam"], w=["neglam"])
        I("dve", "tensor_scalar_mul", og_d, og_d, 0.8, r=["og_d"], w=["og_d"])

        mark_A = A.off
        hT = sb([8, S], BF16)
        mixed = sb([NT, D], BF16)
        QKs = [sb([4, S], BF16), sb([4, S], BF16)]
        Vbs = [sb([NT, 130], BF16), sb([NT, 130], BF16)]
        wg = [sb([8, 384], BF16), sb([8, 384], BF16)]
        xt = sb([D]); hb = sb([D], BF16); sqj = sb([D], BF16)
        ss = sb([1]); lnv = sb([1]); rstd = sb([1])
        sqt = sb([256]); ssq4 = sb([4]); ln4 = sb([4]); r4 = sb([4])
        t1 = sb([256]); t2 = sb([256]); m1 = sb([256]); m2 = sb([256]); st = sb([256], BF16)
        km = sb([2, 8]); kmb = sb([2, 8], BF16)
        gm = sb([NT, 2, 8]); m8 = sb([NT, 2, 8]); sel = sb([NT, 2, 8])
        bst = sb([NT, 2, 72], BF16)
        PT = [sb([512], BF16) for _ in range(3)]
        rl = [sb([4, 1]), sb([4, 1])]
        of = sb([4, 128]); o2 = sb([4, 128]); tt = sb([4, 128])
        ms4 = sb([4]); ln4b = sb([4]); rr4 = sb([4])
        mT = sb([8, 128], BF16)
        xr = sb([D]); x1 = sb([D])

        for bi_ in range(2):
            dma("pool", QKs[bi_][64:128, 2, :], c_kaug, "QKaug", w=["QK%d" % bi_])
            dma("pool", QKs[bi_][64:128, 3, :], c_kaug, "QKaug", w=["QK%d" % bi_])
            I("pool", "memset", Vbs[bi_][:, :, 128:130], 1.0, w=["Vb%d" % bi_])
        I("pool", "memset", bst, 0.0, w=["bst"])
        I("pool", "memset", m8, 0.0, w=["m8"])

        ptr_h = ptr[:, :].rearrange("p (c t) -> p c t", t=128)

        def group_cols(g):
            if g < 4:
                return (128 * g, 512 + 128 * g, 1024 + 128 * g)
            h = g - 4
            return (1536 + 128 * h, 2048 + 128 * h, 2560 + 128 * h)

        def load_wg(g):
            buf = wg[g % 2]
            key = "wg%d" % (g % 2)
            for i, c0 in enumerate(group_cols(g)):
                dma("pool", buf[:, :, i * 128:(i + 1) * 128],
                    w_in[:, c0:c0 + 128].rearrange("(c p) n -> p c n", p=128), key, w=[key])

        final_ops = []
        pt_i = [0]
        s_i = [0]

        if not do_attn:
            for b in range(B_loc):
                for t in range(NT):
                    rows = slice(t * 128, (t + 1) * 128)
                    dma("sp", xt, x[b, rows, :], "xt", w=["xt"])
                    o = dma("sp", out[b, rows, :], xt, "xto", r=["xt"], w=[("out", b, t)])
                    final_ops.append(o)
        for b in range(B_loc if do_attn else 0):
            for t in range(NT):
                rows = slice(t * 128, (t + 1) * 128)
                dma("sp", xt, x[b, rows, :], "xt", w=["xt"])
                I("act", "activation", out=sqj, in_=xt, func=AF.Square, accum_out=ss,
                     r=["xt"], w=["sqj", "ss"])
                I("act", "activation", out=lnv, in_=ss, func=AF.Ln, scale=1.0 / D, bias=epst,
                     r=["ss", "epst"], w=["lnv"])
                I("act", "activation", out=rstd, in_=lnv, func=AF.Exp, scale=-0.5, r=["lnv"], w=["rstd"])
                I("dve", "scalar_tensor_tensor", out=hb, in0=xt, scalar=rstd, in1=g1,
                                                             op0=ALU.mult, op1=ALU.mult,
                     r=["xt", "rstd", "g1"], w=["hb"])
                for c in range(8):
                    I("pe", "transpose", ptr_h[:, c, :], hb[:, c * 128:(c + 1) * 128], ident_b,
                         r=["hb", "ident_b"], w=["ptr"])
                I("dve", "tensor_copy", hT[:, :, rows], ptr_h, r=["ptr"], w=["hT"])

            def grp_ctx(g):
                moba = g < 4
                return (moba, wg[g % 2], 'wg%d' % (g % 2), gq_m if moba else gq_d, 'gq_m' if moba else 'gq_d',
                        QKs[g % 2], 'QK%d' % (g % 2), Vbs[g % 2], 'Vb%d' % (g % 2))

            def inproj_tile(g, t):
                moba, wgb, wkey, gq, gqk, QK, qkk, Vb, vbk = grp_ctx(g)
                rows = slice(t * 128, (t + 1) * 128)
                for c in range(8):
                    I("pe", "matmul",
                        pj[:, 0:384], lhsT=hT[:, c, rows], rhs=wgb[:, c, :], start=(c == 0), stop=(c == 7),
                        r=["hT", wkey], w=["pj"])
                I("act", "activation", out=sqt, in_=pj[:, 0:256], func=AF.Square, r=["pj"], w=["sqt"])
                I("act", "copy", Vb[:, t, 0:128], pj[:, 256:384], r=["pj"], w=[vbk])
                I("dve", "tensor_reduce", out=ssq4, in_=sqt.rearrange("p (a d) -> p a d", d=64),
                                                      axis=AX.X, op=ALU.add, r=["sqt"], w=["ssq4"])
                I("act", "activation", out=ln4, in_=ssq4, func=AF.Ln, scale=1.0 / 64, bias=epst,
                     r=["ssq4", "epst"], w=["ln4"])
                I("act", "activation", out=r4, in_=ln4, func=AF.Exp, scale=-0.5, r=["ln4"], w=["r4"])
                I("dve", "tensor_tensor",
                    out=t1.rearrange("p (a d) -> p a d", d=64), in0=pj[:, 0:256].rearrange("p (a d) -> p a d", d=64),
                    in1=r4.unsqueeze(2).to_broadcast([128, 4, 64]), op=ALU.mult, r=["pj", "r4"], w=["t1"])
                I("pool", "tensor_tensor", out=t2, in0=t1, in1=gq, op=ALU.mult,
                     r=["t1", gqk], w=["t2"])
                t2v = t2.rearrange("p (a h d) -> p a h d", h=2, d=32)
                m1v = m1.rearrange("p (a h d) -> p a h d", h=2, d=32)
                m2v = m2.rearrange("p (a h d) -> p a h d", h=2, d=32)
                stv = st.rearrange("p (a h d) -> p a h d", h=2, d=32)
                cosb = cos_t[:, t, :].unsqueeze(1).to_broadcast([128, 8, 32])
                sinb = sin_t[:, t, :].unsqueeze(1).to_broadcast([128, 4, 32])
                I("pool", "tensor_tensor",
                    out=m1.rearrange("p (a d) -> p a d", d=32), in0=t2.rearrange("p (a d) -> p a d", d=32),
                    in1=cosb, op=ALU.mult, r=["t2", "cos_t"], w=["m1"])
                I("dve", "tensor_tensor",
                    out=m2v[:, :, 0, :], in0=t2v[:, :, 1, :], in1=sinb, op=ALU.mult, r=["t2", "sin_t"], w=["m2a"])
                I("pool", "tensor_tensor",
                    out=m2v[:, :, 1, :], in0=t2v[:, :, 0, :], in1=sinb, op=ALU.mult, r=["t2", "sin_t"], w=["m2b"])
                I("dve", "tensor_tensor",
                    out=stv[:, :, 0, :], in0=m1v[:, :, 0, :], in1=m2v[:, :, 0, :], op=ALU.subtract,
                    r=["m1", "m2a"], w=["sta"])
                I("pool", "tensor_tensor",
                    out=stv[:, :, 1, :], in0=m1v[:, :, 1, :], in1=m2v[:, :, 1, :], op=ALU.add,
                    r=["m1", "m2b"], w=["stb"])

            def inproj_tr(g, t):
                moba, wgb, wkey, gq, gqk, QK, qkk, Vb, vbk = grp_ctx(g)
                rows = slice(t * 128, (t + 1) * 128)
                for i in range(4):
                    I("pe", "transpose", ptr_h[0:64, i, :], st[:, i * 64:(i + 1) * 64], ident_b,
                         r=["sta", "stb", "ident_b"], w=["ptr"])
                I("dve", "tensor_copy", QK[0:64, :, rows], ptr_h[0:64, 0:4, :],
                     r=["ptr"], w=[qkk])


            def gating(g):
                moba, wgb, wkey, gq, gqk, QK, qkk, Vb, vbk = grp_ctx(g)
                if not moba:
                    return
                I("dve", "tensor_reduce",
                    out=km[0:64], in_=QK[0:64, 2:4, :].rearrange("p m (n k) -> p m n k", k=256),
                    axis=AX.X, op=ALU.add, r=[qkk], w=["km"])
                I("dve", "tensor_copy", kmb[0:64], km[0:64], r=["km"], w=["kmb"])
                pjg = pj[:, 0:256].rearrange("p (t m n) -> p t m n", m=2, n=8)
                for t in range(NT):
                    for m in range(2):
                        I("pe", "matmul",
                            pjg[:, t, m, :], lhsT=QK[0:64, m, t * 128:(t + 1) * 128], rhs=kmb[0:64, m, :],
                            start=True, stop=True, r=[qkk, "kmb"], w=["pj"])
                I("dve", "tensor_tensor",
                    out=gm, in0=pjg, in1=past_t.unsqueeze(2).to_broadcast([128, NT, 2, 8]), op=ALU.add,
                    r=["pj", "past_t"], w=["gm"])
                for t in range(8, NT):
                    for m in range(2):
                        I("dve", "max", out=m8[:, t, m, :], in_=gm[:, t, m, :],
                             r=["gm"], w=["m8"])
                I("dve", "tensor_tensor",
                    out=sel, in0=gm, in1=m8[:, :, :, 2:3].to_broadcast([128, NT, 2, 8]), op=ALU.is_ge,
                    r=["gm", "m8"], w=["sel"])
                I("dve", "tensor_scalar", sel, sel, -NEG, NEG, ALU.mult, ALU.add, r=["sel"], w=["sel"])
                I("dve", "tensor_tensor",
                    out=bst[:, :, :, 64:72], in0=sel, in1=own_t.unsqueeze(2).to_broadcast([128, NT, 2, 8]),
                    op=ALU.max, r=["sel", "own_t"], w=["bst"])
                ptr_a = ptr[:, :].rearrange("p (t m q) -> p t m q", m=2, q=128)
                for t0 in range(0, NT, 4):
                    for tl in range(4):
                        for m in range(2):
                            I("pe", "transpose",
                                ptr_a[0:72, tl, m, :], bst[:, t0 + tl, m, :], ident_b,
                                r=["bst", "ident_b"], w=["ptr"])
                    I("dve", "tensor_copy",
                        QK[64:72, 0:2, t0 * 128:(t0 + 4) * 128].rearrange("p m (t q) -> p t m q", q=128),
                        ptr_a[64:72], r=["ptr"], w=[qkk])


            def attention(g, hooks):
                moba, wgb, wkey, gq, gqk, QK, qkk, Vb, vbk = grp_ctx(g)
                nsc = 0
                K = 72 if moba else 64
                for c in range(4):
                    for m in range(2):
                        ac = acc[m]
                        akey = "acc%d" % m
                        def emit_score(j, c=c, m=m):
                            qlo = max(128 * j, 512 * c)
                            N = 512 * (c + 1) - qlo
                            diag = 128 * j >= 512 * c
                            Sx = Sb[s_i[0] % 2]
                            skey = "S%d" % (s_i[0] % 2)
                            s_i[0] += 1
                            I("pe", "matmul",
                                Sx[:, 0:N], lhsT=QK[0:K, 2 + m, j * 128:(j + 1) * 128], rhs=QK[0:K, m, qlo:qlo + N],
                                start=True, stop=(not diag), r=[qkk], w=[skey])
                            if diag:
                                I("pe", "matmul",
                                    Sx[:, 0:128], lhsT=ident_b, rhs=tri_b, start=False, stop=True,
                                    r=["ident_b", "tri_b"], w=[skey])
                            pti = pt_i[0] % 3
                            pt_i[0] += 1
                            PTx = PT[pti]
                            pkey = "PT%d" % pti
                            I("act", "activation",
                                out=PTx[:, 0:N], in_=Sx[:, 0:N], func=AF.Exp, scale=0.125, r=[skey], w=[pkey])
                            return PTx, pkey, qlo

                        nj = 4 * c + 4
                        cur = emit_score(0)
                        for j in range(nj):
                            nxt = emit_score(j + 1) if j + 1 < nj else None
                            PTx, pkey, qlo = cur
                            nsc += 1
                            hook_now = hooks.get(nsc, ())
                            for i in range(max(j, 4 * c), 4 * c + 4):
                                li = i - 4 * c
                                off = 128 * i - qlo
                                I("pe", "matmul",
                                    ac[:, li, 0:129], lhsT=PTx[:, off:off + 128], rhs=Vb[:, j, 0:129],
                                    start=(j == 0 and li % 2 == 0), stop=(j == i), skip_group_check=True,
                                    r=[pkey, vbk], w=[akey])
                            for fn in hook_now:
                                fn()
                            cur = nxt
                        rlm = rl[m]
                        I("dve", "reciprocal", rlm, ac[:, :, 128:129],
                             r=[akey], w=["rl%d" % m])
                        if moba:
                            h = 2 * g + m
                            I("dve", "tensor_tensor",
                                out=of[:, :, 0:64], in0=ac[:, :, m * 64:(m + 1) * 64],
                                in1=rlm.to_broadcast([128, 4, 64]), op=ALU.mult, r=[akey, "rl%d" % m], w=["of"])
                            W_ = 64
                            gsl = og_m[:, h * 64:(h + 1) * 64]
                            gk = "og_m"
                            col0 = h * 64
                        elif m == 0:
                            I("dve", "tensor_tensor",
                                out=of, in0=ac[:, :, 0:128], in1=rlm.to_broadcast([128, 4, 128]), op=ALU.mult,
                                r=[akey, "rl0"], w=["of"])
                            continue
                        else:
                            h = g - 4
                            I("dve", "tensor_tensor",
                                out=tt, in0=ac[:, :, 0:128], in1=rlm.to_broadcast([128, 4, 128]), op=ALU.mult,
                                r=[akey, "rl1"], w=["tt"])
                            I("dve", "scalar_tensor_tensor",
                                out=of, in0=tt, scalar=neglam, in1=of, op0=ALU.mult, op1=ALU.add,
                                r=["tt", "neglam", "of"], w=["of"])
                            W_ = 128
                            gsl = og_d[:, h * 128:(h + 1) * 128]
                            gk = "og_d"
                            col0 = 512 + h * 128
                        I("pool", "tensor_tensor",
                            out=o2[:, :, 0:W_], in0=of[:, :, 0:W_], in1=of[:, :, 0:W_], op=ALU.mult, r=["of"], w=["o2"])
                        I("dve", "tensor_reduce", out=ms4, in_=o2[:, :, 0:W_], axis=AX.X, op=ALU.add,
                             r=["o2"], w=["ms4"])
                        I("act", "activation", out=ln4b, in_=ms4, func=AF.Ln, scale=1.0 / W_, bias=epst,
                             r=["ms4", "epst"], w=["ln4b"])
                        I("act", "activation", out=rr4, in_=ln4b, func=AF.Exp, scale=-0.5,
                             r=["ln4b"], w=["rr4"])
                        I("dve", "tensor_tensor",
                            out=o2[:, :, 0:W_], in0=of[:, :, 0:W_], in1=rr4.unsqueeze(2).to_broadcast([128, 4, W_]),
                            op=ALU.mult, r=["of", "rr4", "o2"], w=["o2"])
                        I("pool", "tensor_tensor",
                            out=mixed[:, 4 * c:4 * c + 4, col0:col0 + W_], in0=o2[:, :, 0:W_],
                            in1=gsl.unsqueeze(1).to_broadcast([128, 4, W_]), op=ALU.mult,
                            r=["o2", gk], w=["mixed"])


            load_wg(0)
            load_wg(1)
            for t in range(NT):
                inproj_tile(0, t)
                inproj_tr(0, t)
            gating(0)
            for g in range(8):
                hooks = {}
                if g + 1 < 8:
                    for t in range(NT):
                        hk = []
                        if t > 0:
                            hk.append(lambda g=g, t=t: inproj_tr(g + 1, t - 1))
                        hk.append(lambda g=g, t=t: inproj_tile(g + 1, t))
                        hooks[2 + 4 * t] = hk
                    hooks[66] = [lambda g=g: inproj_tr(g + 1, NT - 1)]
                    hooks[70] = [lambda g=g: gating(g + 1)]
                    if g + 2 < 8:
                        hooks[1] = [lambda g=g: load_wg(g + 2)]
                attention(g, hooks)

            for t in range(NT):
                rows = slice(t * 128, (t + 1) * 128)
                dma("sp", xr, x[b, rows, :], "xr", w=["xr"])
                for c in range(8):
                    I("pe", "transpose", ptr_h[:, c, :], mixed[:, t, c * 128:(c + 1) * 128], ident_b,
                         r=["mixed", "ident_b"], w=["ptr"])
                I("act", "copy", mT, ptr_h, r=["ptr"], w=["mT"])
                for hf in range(2):
                    for c in range(8):
                        I("pe", "matmul",
                            Sb[hf][:, 0:512], lhsT=mT[:, c, :], rhs=wout[:, c, hf * 512:(hf + 1) * 512],
                            start=(c == 0), stop=(c == 7), r=["mT", "wout"], w=["S%d" % hf])
                    I("dve", "tensor_tensor",
                        out=x1[:, hf * 512:(hf + 1) * 512], in0=Sb[hf][:, 0:512], in1=xr[:, hf * 512:(hf + 1) * 512],
                        op=ALU.add, r=["S%d" % hf, "xr"], w=["x1"])
                o = dma("sp", out[b, rows, :], x1, "x1", r=["x1"], w=[("out", b, t)])
                final_ops.append(o)

        if do_peer:
            final_ops = []
            P.barrier()
            A.off = mark_shared
            Wq = sb([8, 2048], BF16); keysT = sb([16, 128], BF16); g2 = sb([D])
            iota_b = sb([128], BF16); iota16 = sb([16])
            h2T = sb([8, 256], BF16); qT = sb([16, 256], BF16)
            x1g = sb([2, D]); h2 = sb([D], BF16); sqj2 = sb([D], BF16)
            ss2 = sb([1]); lnv2 = sb([1]); rstd2 = sb([1])
            sc = sb([16, 128]); scr = sb([128]); scr2 = sb([256])
            m16 = sb([16, 16]); i16 = sb([16, 16], U32); i16f = sb([16, 16])
            cand = sb([8, 256]); b16 = sb([8, 16]); p16 = sb([8, 16], U32)
            pa = sb([8, 16], U32); pb = sb([8, 16], U32); paf = sb([8, 16]); pbf = sb([8, 16])
            eb = sb([8, 16]); es = sb([8]); er = sb([8])
            IJG_tm = sb([3, 128]); IJG = sb([3, 256])
            oh = sc.rearrange("p a b -> p (a b)").rearrange("p (h x) -> p h x", x=256)
            A01s = [sb([16, 128], BF16) for _ in range(2)]; Ags = [sb([16, 128], BF16) for _ in range(2)]
            B01s = [sb([16, 128], BF16) for _ in range(2)]
            dTb = [sb([8, 128], BF16) for _ in range(3)]
            upb = [sb([D], BF16) for _ in range(3)]
            yb = sb([D])
            off_WT = A.off
            dn_b = [sb([D], BF16) for _ in range(2)]
            up_b = [sb([D], BF16) for _ in range(2)]
            dT_sb = [sb([8, 128], BF16) for _ in range(2)]
            kst = sb([16, 128], BF16)
            A.off = off_WT
            WT = sb([128, 256], BF16)

            dma("pool", Wq, peer_query.rearrange("(c p) n -> p c n", p=128), "Wq", w=["Wq"])
            dma("pool", kst, peer_sub_keys.rearrange("(hp n) d -> n hp d", n=128), "kst", w=["kst"])
            dma("sp", g2, ffn_norm[0:1, :].to_broadcast([128, D]), "g2", w=["g2"])
            dma("pool", iota_b, c_iota, "iota_b", w=["iota_b"])
            dma("sp", iota16, c_iota[:, 0:16], "iota16", w=["iota16"])
            for h8 in range(2):
                for k in range(8):
                    I("pe", "transpose", ptr_h[:, k, :], kst[:, h8 * 8 + k, :], ident_b, r=["kst", "ident_b"], w=["ptr"])
                I("dve", "tensor_copy", keysT[:, h8 * 8:(h8 + 1) * 8, :], ptr_h, r=["ptr"], w=["keysT"])
            for i in range(128):
                bi = i % 2
                rows = slice(i * 128, (i + 1) * 128)
                dma("pool", dn_b[bi], peer_down[rows, :], "dn_b%d" % bi, w=["dn_b%d" % bi])
                for c in range(8):
                    I("pe", "transpose", ptr_h[:, c, :], dn_b[bi][:, c * 128:(c + 1) * 128], ident_b,
                      r=["dn_b%d" % bi, "ident_b"], w=["ptr"])
                I("act" if i % 2 else "dve", "copy" if i % 2 else "tensor_copy", dT_sb[bi], ptr_h,
                  r=["ptr"], w=["dT_sb%d" % bi])
                dma("sp", downT_s[i], dT_sb[bi], "dT_sbo%d" % bi, r=["dT_sb%d" % bi], w=[("dTs", i)])
                dma("pool", up_b[bi], peer_up[rows, :], "up_bi%d" % bi, w=["up_b%d" % bi])
                dma("sp", up_s[i], up_b[bi], "up_bo%d" % bi, r=["up_b%d" % bi], w=[("ups", i)])
            P.barrier()

            allWT = [("WT", i) for i in range(128)]
            NG = S // 256
            groups = [(b, gi) for b in range(B_loc) for gi in range(NG)]
            if n_groups is not None:
                groups = groups[:n_groups]
            x1gs = [x1g, sb([2, D])]

            def p_load(n, tt):
                b, gi = groups[n]
                xg = x1gs[n % 2]
                xk = "x1g%d" % (n % 2)
                t = gi * 2 + tt
                rows = slice(t * 128, (t + 1) * 128)
                dma("sp", xg[:, tt, :], out[b, rows, :], xk, r=[("out", b, t)], w=[xk])
                I("act", "activation", out=sqj2, in_=xg[:, tt, :], func=AF.Square, accum_out=ss2,
                  r=[xk], w=["sqj2", "ss2"])
                I("act", "activation", out=lnv2, in_=ss2, func=AF.Ln, scale=1.0 / D, bias=epst,
                  r=["ss2", "epst"], w=["lnv2"])
                I("act", "activation", out=rstd2, in_=lnv2, func=AF.Exp, scale=-0.5, r=["lnv2"], w=["rstd2"])
                I("dve", "scalar_tensor_tensor", out=h2, in0=xg[:, tt, :], scalar=rstd2, in1=g2,
                  op0=ALU.mult, op1=ALU.mult, r=[xk, "rstd2", "g2"], w=["h2"])
                for c in range(8):
                    I("pe", "transpose", ptr_h[:, c, :], h2[:, c * 128:(c + 1) * 128], ident_b,
                      r=["h2", "ident_b"], w=["ptr"])
                I("dve", "tensor_copy", h2T[:, :, tt * 128:(tt + 1) * 128], ptr_h, r=["ptr"], w=["h2T"])

            def p_q(n, hp):
                Sx = Sb[hp % 2]
                skey = "S%d" % (hp % 2)
                for c in range(8):
                    I("pe", "matmul", Sx[:, 0:256], lhsT=Wq[:, c, hp * 128:(hp + 1) * 128], rhs=h2T[:, c, :],
                      start=(c == 0), stop=(c == 7), r=["Wq", "h2T"], w=[skey])
                if hp % 2:
                    I("act", "copy", qT[:, hp, :], Sx[:, 0:256], r=[skey], w=["qT"])
                else:
                    I("dve", "tensor_copy", qT[:, hp, :], Sx[:, 0:256], r=[skey], w=["qT"])

            def p_topk(n, tt):
                tsl = slice(tt * 128, (tt + 1) * 128)
                for q4 in range(4):
                    Sx = Sb[q4 % 2]
                    skey = "S%d" % (q4 % 2)
                    for k in range(4):
                        hp = q4 * 4 + k
                        I("pe", "matmul", Sx[:, k * 128:(k + 1) * 128], lhsT=qT[:, hp, tsl], rhs=keysT[:, hp, :],
                          start=True, stop=True, r=["qT", "keysT"], w=[skey])
                    I("act", "copy", sc[:, q4 * 4:(q4 + 1) * 4, :], Sx[:, :].rearrange("p (a n) -> p a n", n=128),
                      r=[skey], w=["sc"])
                for hp in range(16):
                    I("dve", "max", out=m16[:, hp, 0:8], in_=sc[:, hp, :], r=["sc"], w=["m16"])
                    I("dve", "max_index", out=i16[:, hp, 0:8], in_max=m16[:, hp, 0:8], in_values=sc[:, hp, :],
                      r=["sc", "m16"], w=["i16"])
                    I("dve", "match_replace", out=scr, in_to_replace=m16[:, hp, 0:8], in_values=sc[:, hp, :],
                      imm_value=-1e30, r=["sc", "m16"], w=["scr"])
                    I("dve", "max", out=m16[:, hp, 8:16], in_=scr, r=["scr"], w=["m16"])
                    I("dve", "max_index", out=i16[:, hp, 8:16], in_max=m16[:, hp, 8:16], in_values=scr,
                      r=["scr", "m16"], w=["i16"])
                m16v = m16.rearrange("p (h two) k -> p h two k", two=2)
                candv = cand.rearrange("p h (a b) -> p h a b", b=16)
                I("dve", "tensor_tensor", out=candv,
                  in0=m16v[:, :, 0, :].unsqueeze(3).to_broadcast([128, 8, 16, 16]),
                  in1=m16v[:, :, 1, :].unsqueeze(2).to_broadcast([128, 8, 16, 16]), op=ALU.add,
                  r=["m16"], w=["cand"])
                for h in range(8):
                    I("dve", "max", out=b16[:, h, 0:8], in_=cand[:, h, :], r=["cand"], w=["b16"])
                    I("dve", "max_index", out=p16[:, h, 0:8], in_max=b16[:, h, 0:8], in_values=cand[:, h, :],
                      r=["cand", "b16"], w=["p16"])
                    I("dve", "match_replace", out=scr2, in_to_replace=b16[:, h, 0:8], in_values=cand[:, h, :],
                      imm_value=-1e30, r=["cand", "b16"], w=["scr2"])
                    I("dve", "max", out=b16[:, h, 8:16], in_=scr2, r=["scr2"], w=["b16"])
                    I("dve", "max_index", out=p16[:, h, 8:16], in_max=b16[:, h, 8:16], in_values=scr2,
                      r=["scr2", "b16"], w=["p16"])
                I("dve", "tensor_tensor", out=eb, in0=b16, in1=b16[:, :, 0:1].to_broadcast([128, 8, 16]),
                  op=ALU.subtract, r=["b16"], w=["eb"])
                I("act", "activation", out=eb, in_=eb, func=AF.Exp, r=["eb"], w=["eb"])
                I("dve", "tensor_reduce", out=es, in_=eb, axis=AX.X, op=ALU.add, r=["eb"], w=["es"])
                I("dve", "reciprocal", er, es, r=["es"], w=["er"])
                I("dve", "tensor_tensor", out=IJG_tm[:, 2, :].rearrange("p (h k) -> p h k", k=16), in0=eb,
                  in1=er.unsqueeze(2).to_broadcast([128, 8, 16]), op=ALU.mult, r=["eb", "er"], w=["IJG_tm"])
                I("dve", "tensor_single_scalar", pa, p16, 4, ALU.logical_shift_right, r=["p16"], w=["pa"])
                I("dve", "tensor_single_scalar", pb, p16, 15, ALU.bitwise_and, r=["p16"], w=["pb"])
                I("dve", "tensor_copy", paf, pa, r=["pa"], w=["paf"])
                I("dve", "tensor_copy", pbf, pb, r=["pb"], w=["pbf"])
                I("dve", "tensor_copy", i16f, i16, r=["i16"], w=["i16f"])
                i16v = i16f.rearrange("p (h two) k -> p h two k", two=2)
                ohv = oh.rearrange("p h (k a) -> p h k a", a=16)
                for pf, pfk, which in ((paf, "paf", 0), (pbf, "pbf", 1)):
                    I("dve", "tensor_tensor", out=ohv, in0=pf.unsqueeze(3).to_broadcast([128, 8, 16, 16]),
                      in1=iota16.unsqueeze(1).unsqueeze(1).to_broadcast([128, 8, 16, 16]), op=ALU.is_equal,
                      r=[pfk, "iota16"], w=["sc"])
                    I("pool", "tensor_tensor", out=ohv, in0=ohv,
                      in1=i16v[:, :, which, :].unsqueeze(2).to_broadcast([128, 8, 16, 16]), op=ALU.mult,
                      r=["sc", "i16f"], w=["sc"])
                    I("dve", "tensor_reduce", out=IJG_tm[:, which, :].rearrange("p (h k) -> p h k", k=16),
                      in_=ohv, axis=AX.X, op=ALU.add, r=["sc"], w=["IJG_tm"])

            def p_tr(n, tt):
                tsl = slice(tt * 128, (tt + 1) * 128)
                for k3 in range(3):
                    I("pe", "transpose", pj[:, k3 * 128:(k3 + 1) * 128], IJG_tm[:, k3, :], ident_f,
                      r=["IJG_tm", "ident_f"], w=["pj"])
                I("dve", "tensor_copy", IJG[:, :, tsl], pj[:, 0:384].rearrange("p (a t) -> p a t", t=128),
                  r=["pj"], w=["IJG"])

            def prologue_sched(n):
                sch = {}
                sch[2] = [lambda: p_load(n, 0)]
                sch[6] = [lambda: p_load(n, 1)]
                for hp in range(16):
                    sch[12 + 2 * hp] = [lambda hp=hp: p_q(n, hp)]
                sch[46] = [lambda: p_topk(n, 0)]
                sch[84] = [lambda: p_tr(n, 0), lambda: p_topk(n, 1)]
                sch[126] = [lambda: p_tr(n, 1)]
                return sch

            def run_prologue(n):
                sch = prologue_sched(n)
                for k in sorted(sch):
                    for fn in sch[k]:
                        fn()

            def U_phase(n):
                for i in range(128):
                    bi = i % 3
                    dkey = "dTb%d" % bi
                    dma("sp" if i % 2 == 0 else "act", dTb[bi], downT_s[i], dkey, r=[("dTs", i)], w=[dkey])
                    Sx = Sb[i % 2]
                    skey = "S%d" % (i % 2)
                    for c in range(8):
                        I("pe", "matmul", Sx[:, 0:256], lhsT=dTb[bi][:, c, :], rhs=h2T[:, c, :],
                          start=(c == 0), stop=(c == 7), r=[dkey, "h2T"], w=[skey])
                    I("act", "activation", out=WT[:, i, :], in_=Sx[:, 0:256], func=AF.Gelu, r=[skey], w=[("WT", i)])

            def G_phase(n):
                for ci, t0 in enumerate(range(0, 256, 16)):
                    pb_ = ci % 2
                    A01 = A01s[pb_]; Ag = Ags[pb_]; B01 = B01s[pb_]
                    ka = "A01_%d" % pb_; kg = "Ag_%d" % pb_; kb = "B01_%d" % pb_
                    iob = iota_b.unsqueeze(1).to_broadcast([128, 16, 128])
                    I("dve", "tensor_tensor", out=A01, in0=iob,
                      in1=IJG[:, 0, t0:t0 + 16].unsqueeze(2).to_broadcast([128, 16, 128]), op=ALU.is_equal,
                      r=["iota_b", "IJG"], w=[ka])
                    I("dve", "tensor_tensor", out=B01, in0=iob,
                      in1=IJG[:, 1, t0:t0 + 16].unsqueeze(2).to_broadcast([128, 16, 128]), op=ALU.is_equal,
                      r=["iota_b", "IJG"], w=[kb])
                    I("pool", "tensor_tensor", out=Ag, in0=A01,
                      in1=IJG[:, 2, t0:t0 + 16].unsqueeze(2).to_broadcast([128, 16, 128]), op=ALU.mult,
                      r=[ka, "IJG"], w=[kg])
                    for half in range(2):
                        akey = "acc%d" % half
                        accv = acc[half][:, :, :].rearrange("p a (b i) -> p (a b) i", i=128)
                        for tl in range(8):
                            tk = half * 8 + tl
                            I("pe", "matmul", accv[:, tl, :], lhsT=B01[:, tk, :], rhs=Ag[:, tk, :],
                              start=True, stop=True, skip_group_check=True, r=[kb, kg], w=[akey])
                        ts8 = slice(t0 + half * 8, t0 + half * 8 + 8)
                        I("dve", "tensor_tensor", out=WT[:, :, ts8], in0=accv.rearrange("p t i -> p i t"),
                          in1=WT[:, :, ts8], op=ALU.mult, r=[akey] + allWT, w=allWT)

            def up_phase(n, sch):
                b, gi = groups[n]
                xg = x1gs[n % 2]
                xk = "x1g%d" % (n % 2)
                for i in range(128):
                    bi = i % 3
                    ukey = "upb%d" % bi
                    dma("sp" if i % 2 == 0 else "act", upb[bi], up_s[i], ukey, r=[("ups", i)], w=[ukey])
                    for tt in range(2):
                        accf = acc[tt][:, :, :].rearrange("p a b -> p (a b)")
                        for hf in range(2):
                            I("pe", "matmul", accf[:, hf * 512:(hf + 1) * 512], lhsT=WT[:, i, tt * 128:(tt + 1) * 128],
                              rhs=upb[bi][:, hf * 512:(hf + 1) * 512], start=(i == 0), stop=(i == 127),
                              r=[("WT", i), ukey], w=["acc%d" % tt])
                    for fn in sch.get(i, ()):
                        fn()
                for tt in range(2):
                    t = gi * 2 + tt
                    rows = slice(t * 128, (t + 1) * 128)
                    accf = acc[tt][:, :, :].rearrange("p a b -> p (a b)")
                    I("dve", "tensor_tensor", out=yb, in0=accf, in1=xg[:, tt, :], op=ALU.add,
                      r=["acc%d" % tt, xk], w=["yb"])
                    o = dma("sp", out[b, rows, :], yb, "yb", r=["yb"], w=[("out", b, t)])
                    final_ops.append(o)

            run_prologue(0)
            for n in range(len(groups)):
                U_phase(n)
                G_phase(n)
                up_phase(n, prologue_sched(n + 1) if n + 1 < len(groups) else {})

        P.emit(final_ops)
    return nc


_NC_CACHE = {}


def kernel(**inputs):
    B = inputs["x"].shape[0]
    B_loc = B // N_CORES
    if B_loc not in _NC_CACHE:
        _NC_CACHE[B_loc] = build(B_loc)
    nc = _NC_CACHE[B_loc]
    consts = host_consts()
    f = lambda a: np.ascontiguousarray(np.asarray(a, dtype=np.float32))
    shared = {
        "attn_norm": f(inputs["attn_norm"]).reshape(1, D),
        "w_in": f(inputs["w_in"]).reshape(D, 3072),
        "q_norm_moba": f(inputs["q_norm_moba"]).reshape(1, 64),
        "k_norm_moba": f(inputs["k_norm_moba"]).reshape(1, 64),
        "q_norm_diff": f(inputs["q_norm_diff"]).reshape(2, 64),
        "k_norm_diff": f(inputs["k_norm_diff"]).reshape(2, 64),
        "lambda_q1": f(inputs["lambda_q1"]).reshape(1, 64),
        "lambda_k1": f(inputs["lambda_k1"]).reshape(1, 64),
        "lambda_q2": f(inputs["lambda_q2"]).reshape(1, 64),
        "lambda_k2": f(inputs["lambda_k2"]).reshape(1, 64),
        "moba_out_gain": f(inputs["moba_out_gain"]).reshape(1, 512),
        "diff_out_gain": f(inputs["diff_out_gain"]).reshape(1, 512),
        "w_out": f(inputs["w_out"]).reshape(D, D),
        "ffn_norm": f(inputs["ffn_norm"]).reshape(1, D),
        "peer_query": f(inputs["peer_query"]).reshape(D, 2048),
        "peer_sub_keys": f(inputs["peer_sub_keys"]).reshape(2048, 128),
        "peer_down": f(inputs["peer_down"]).reshape(16384, D),
        "peer_up": f(inputs["peer_up"]).reshape(16384, D),
    }
    shared.update(consts)
    xs = f(inputs["x"])
    in_maps = []
    for c in range(N_CORES):
        m = dict(shared)
        m["x"] = xs[c * B_loc:(c + 1) * B_loc]
        in_maps.append(m)
    res = run_bass_kernel_spmd(nc, in_maps, core_ids=list(range(N_CORES)))
    return np.concatenate([r["out"] for r in res.results], axis=0)
```

```python
import math
from contextlib import ExitStack

import numpy as np
import concourse.bass as bass
import concourse.mybir as mybir
from concourse.bass_utils import run_bass_kernel_spmd

F32 = mybir.dt.float32
BF16 = mybir.dt.bfloat16
U32 = mybir.dt.uint32
U8 = mybir.dt.uint8
AF = mybir.ActivationFunctionType
ALU = mybir.AluOpType
AX = mybir.AxisListType

ENGS = ["pe", "dve", "act", "pool", "sp"]
N_CORES = 8
D = 1024
S = 2048
NT = S // 128
EPS = 1e-6
NEG = -30000.0


class Op:
    __slots__ = ("eng", "fn", "idx", "deps", "is_dma", "dsem", "dval", "signal", "sval")

    def __init__(self, eng, fn, idx, is_dma):
        self.eng = eng
        self.fn = fn
        self.idx = idx
        self.deps = []
        self.is_dma = is_dma
        self.dsem = None
        self.dval = 0
        self.signal = False
        self.sval = 0


class Prog:
    def __init__(self, nc):
        self.nc = nc
        self.ops = {e: [] for e in ENGS}
        self.last_w = {}
        self.readers = {}
        self.seen = {e: {} for e in ENGS}
        self.dma_sems = {}
        self.barrier_deps = []
        self.dma_ops = []

    def _skey(self, dep):
        if dep.is_dma:
            return ("d", dep.dsem), dep.dval
        return ("e", dep.eng), dep.idx

    def op(self, eng, fn, r=(), w=(), dma=None):
        lst = self.ops[eng]
        o = Op(eng, fn, len(lst), dma is not None)
        if dma is not None:
            ent = self.dma_sems.setdefault(dma, [len(self.dma_sems), 0])
            ent[1] += 16
            o.dsem = dma
            o.dval = ent[1]
            self.dma_ops.append(o)
        cand = {}

        def add(dep):
            if dep is None:
                return
            if (not dep.is_dma) and dep.eng == eng and eng == "pe":
                return
            k, v = self._skey(dep)
            if k not in cand or cand[k][0] < v:
                cand[k] = (v, dep)

        for d in self.barrier_deps:
            add(d)
        for k in r:
            add(self.last_w.get(k))
        for k in w:
            add(self.last_w.get(k))
            for rd in self.readers.get(k, ()):
                add(rd)
        seen = self.seen[eng]
        for k, (v, dep) in cand.items():
            if seen.get(k, -1) >= v:
                continue
            seen[k] = v
            o.deps.append(dep)
            if not dep.is_dma:
                dep.signal = True
        for k in r:
            self.readers.setdefault(k, []).append(o)
        for k in w:
            self.last_w[k] = o
            self.readers[k] = []
        lst.append(o)
        return o

    def barrier(self):
        deps = [lst[-1] for lst in self.ops.values() if lst]
        deps = [d for d in deps if not d.is_dma]
        self.barrier_deps = deps + list(self.dma_ops)
        self.dma_ops = []

    def emit(self, final_ops):
        nc = self.nc
        with ExitStack() as es:
            esem = {e: es.enter_context(nc.semaphore("s_" + e)) for e in ENGS}
            dsem = {k: es.enter_context(nc.semaphore("d%d" % v[0])) for k, v in self.dma_sems.items()}
            for o in final_ops:
                if not o.is_dma:
                    o.signal = True
            for e in ENGS:
                c = 0
                for o in self.ops[e]:
                    if o.signal and not o.is_dma:
                        c += 1
                        o.sval = c
            block = es.enter_context(nc.Block())
            reg = {"pe": block.tensor, "dve": block.vector, "act": block.scalar,
                   "pool": block.gpsimd, "sp": block.sync}

            def make(e):
                ops = self.ops[e]

                def body(eng):
                    def wait(d):
                        if d.is_dma:
                            eng.wait_ge(dsem[d.dsem], d.dval)
                        else:
                            eng.wait_ge(esem[d.eng], d.sval)
                    for o in ops:
                        for d in o.deps:
                            wait(d)
                        meth, args, kw = o.fn
                        ins = getattr(eng, meth)(*args, **kw)
                        if o.is_dma:
                            ins.then_inc(dsem[o.dsem], 16)
                        elif o.signal:
                            ins.then_inc(esem[e], 1)
                    if e == "sp":
                        for d in final_ops:
                            wait(d)
                return body

            for e in ENGS:
                reg[e](make(e))


class Arena:
    def __init__(self, tile, size):
        self.t = tile
        self.size = size
        self.off = 0

    def alloc(self, shape, dt, nbytes_el):
        n = 1
        for s in shape:
            n *= s
        nb = n * nbytes_el
        nb_al = (nb + 63) // 64 * 64
        assert self.off + nb_al <= self.size, ("arena overflow", self.off, nb_al, self.size)
        ap = self.t[:, self.off:self.off + nb]
        self.off += nb_al
        if dt is not U8:
            ap = ap.bitcast(dt)
        if len(shape) == 2:
            ap = ap.rearrange("p (a b) -> p a b", b=shape[1])
        elif len(shape) == 3:
            ap = ap.rearrange("p (a b c) -> p a b c", b=shape[1], c=shape[2])
        return ap


def rope_consts():
    pos = np.arange(S, dtype=np.float32)
    inv = (1.0 / (np.float32(10000.0) ** (np.arange(0, 64, 2, dtype=np.float32) / np.float32(64)))).astype(np.float32)
    ang = (pos[:, None] * inv[None, :]).astype(np.float32)
    cos = np.cos(ang).astype(np.float32).reshape(NT, 128, 32).transpose(1, 0, 2)
    sin = np.sin(ang).astype(np.float32).reshape(NT, 128, 32).transpose(1, 0, 2)
    return np.ascontiguousarray(cos), np.ascontiguousarray(sin)


def host_consts():
    cos, sin = rope_consts()
    k = np.arange(128)[:, None]
    q = np.arange(128)[None, :]
    tri = np.where(k <= q, 0.0, NEG).astype(np.float32)
    kaug = np.zeros((64, S), np.float32)
    for n in range(8):
        kaug[n, n * 256:(n + 1) * 256] = 1.0
    past = np.zeros((NT, 8), np.float32)
    own = np.full((NT, 8), -1e30, np.float32)
    for t in range(NT):
        qb = t // 2
        for n in range(8):
            if n >= qb:
                past[t, n] = -1e30
            if n == qb or (qb <= 3 and n <= qb):
                own[t, n] = 0.0
    past = np.ascontiguousarray(np.broadcast_to(past[None], (128, NT, 8))).astype(np.float32)
    own = np.ascontiguousarray(np.broadcast_to(own[None], (128, NT, 8))).astype(np.float32)
    ident = np.eye(128, dtype=np.float32)
    iota = np.ascontiguousarray(np.broadcast_to(np.arange(128, dtype=np.float32)[None], (128, 128)))
    return {"c_iota": iota, "c_cos": cos, "c_sin": sin, "c_tri": tri, "c_kaug": kaug, "c_past": past,
            "c_own": own, "c_ident": ident}


def build(B_loc, do_peer=True, do_attn=True, n_groups=None):
    nc = bass.Bass("TRN2", target_bir_lowering=False)

    def din(name, shape, dt=F32):
        return nc.dram_tensor(name, list(shape), dt, kind="ExternalInput").ap()

    x = din("x", [B_loc, S, D])
    attn_norm = din("attn_norm", [1, D])
    w_in = din("w_in", [D, 3072])
    qnm = din("q_norm_moba", [1, 64])
    knm = din("k_norm_moba", [1, 64])
    qnd = din("q_norm_diff", [2, 64])
    knd = din("k_norm_diff", [2, 64])
    lq1 = din("lambda_q1", [1, 64])
    lk1 = din("lambda_k1", [1, 64])
    lq2 = din("lambda_q2", [1, 64])
    lk2 = din("lambda_k2", [1, 64])
    mog = din("moba_out_gain", [1, 512])
    dog = din("diff_out_gain", [1, 512])
    w_out = din("w_out", [D, D])
    c_cos = din("c_cos", [128, NT, 32])
    c_sin = din("c_sin", [128, NT, 32])
    c_tri = din("c_tri", [128, 128])
    c_kaug = din("c_kaug", [64, S])
    c_past = din("c_past", [128, NT, 8])
    c_own = din("c_own", [128, NT, 8])
    c_ident = din("c_ident", [128, 128])
    ffn_norm = din("ffn_norm", [1, D])
    peer_query = din("peer_query", [D, 2048])
    peer_sub_keys = din("peer_sub_keys", [2048, 128])
    peer_down = din("peer_down", [16384, D])
    peer_up = din("peer_up", [16384, D])
    c_iota = din("c_iota", [128, 128])
    downT_s = nc.dram_tensor("downT_s", [128, 128, 8, 128], BF16).ap()
    up_s = nc.dram_tensor("up_s", [128, 128, D], BF16).ap()
    out = nc.dram_tensor("out", [B_loc, S, D], F32, kind="ExternalOutput").ap()

    P = Prog(nc)
    with ExitStack() as es:
        ARENA_BYTES = 212000
        arena_t = es.enter_context(nc.sbuf_tensor("arena", [128, ARENA_BYTES], U8))
        A = Arena(arena_t, ARENA_BYTES)

        def sb(shape, dt=F32):
            return A.alloc(shape, dt, 2 if dt is BF16 else 4)

        def ps(name, shape, dt=F32):
            return es.enter_context(nc.psum_tensor(name, shape, dt))

        S0 = ps("S0", [128, 512])
        S1 = ps("S1", [128, 512])
        acc = [ps("acc0", [128, 4, 256]), ps("acc1", [128, 4, 256])]
        pj = ps("pj", [128, 512])
        ptr = ps("ptr", [128, 1024], BF16)
        Sb = [S0, S1]

        ident_f = sb([128]); ident_b = sb([128], BF16)
        epst = sb([1])
        mark_shared = A.off
        tri_b = sb([128], BF16)
        cos_t = sb([NT, 32]); sin_t = sb([NT, 32])
        past_t = sb([NT, 8]); own_t = sb([NT, 8])
        g1 = sb([D])
        gq_m = sb([256]); gq_d = sb([256])
        og_m = sb([512]); og_d = sb([512])
        lam4 = sb([4, 64]); lamt = sb([2, 64]); lams = sb([2]); lame = sb([2]); neglam = sb([1])
        wout = sb([8, D], BF16)

        def I(eng, meth, *args, r=(), w=(), dma=None, **kw):
            return P.op(eng, (meth, args, kw), r=r, w=w, dma=dma)

        def dma(eng, out_ap, in_ap, key, r=(), w=()):
            return I(eng, "dma_start", out=out_ap, in_=in_ap, r=r, w=w, dma=key)

        dma("sp", ident_f, c_ident, "ident_f", w=["ident_f"])
        dma("pool", ident_b, c_ident, "ident_b", w=["ident_b"])
        dma("pool", tri_b, c_tri, "tri_b", w=["tri_b"])
        dma("sp", cos_t, c_cos, "cos_t", w=["cos_t"])
        dma("sp", sin_t, c_sin, "sin_t", w=["sin_t"])
        dma("sp", past_t, c_past, "past_t", w=["past_t"])
        dma("sp", own_t, c_own, "own_t", w=["own_t"])
        dma("sp", g1, attn_norm[0:1, :].to_broadcast([128, D]), "g1", w=["g1"])
        for i in range(2):
            dma("sp", gq_m[:, i * 64:(i + 1) * 64], qnm[0:1, :].to_broadcast([128, 64]), "gq_m", w=["gq_m"])
            dma("sp", gq_m[:, 128 + i * 64:128 + (i + 1) * 64], knm[0:1, :].to_broadcast([128, 64]), "gq_m", w=["gq_m"])
            dma("sp", gq_d[:, i * 64:(i + 1) * 64], qnd[i:i + 1, :].to_broadcast([128, 64]), "gq_d", w=["gq_d"])
            dma("sp", gq_d[:, 128 + i * 64:128 + (i + 1) * 64], knd[i:i + 1, :].to_broadcast([128, 64]), "gq_d", w=["gq_d"])
        dma("sp", og_m, mog[0:1, :].to_broadcast([128, 512]), "og_m", w=["og_m"])
        dma("sp", og_d, dog[0:1, :].to_broadcast([128, 512]), "og_d", w=["og_d"])
        for i, v in enumerate((lq1, lk1, lq2, lk2)):
            dma("sp", lam4[:, i, :], v[0:1, :].to_broadcast([128, 64]), "lam4", w=["lam4"])
        dma("pool", wout, w_out.rearrange("(c p) n -> p c n", p=128), "wout", w=["wout"])
        I("pool", "memset", epst, EPS, w=["epst"])
        I("dve", "tensor_tensor", out=lamt, in0=lam4[:, 0:4:2, :], in1=lam4[:, 1:4:2, :], op=ALU.mult,
             r=["lam4"], w=["lamt"])
        I("dve", "tensor_reduce", out=lams, in_=lamt, axis=AX.X, op=ALU.add, r=["lamt"], w=["lams"])
        I("act", "activation", out=lame, in_=lams, func=AF.Exp, r=["lams"], w=["lame"])
        I("dve", "tensor_tensor", out=neglam, in0=lame[:, 1:2], in1=lame[:, 0:1], op=ALU.subtract,
             r=["lame"], w=["neglam"])
        I("dve", "tensor_scalar_add", neglam, neglam, -0.2, r=["neglam"], w=["neglam"])
        I("dve", "tensor_scalar_mul", og_d, og_d, 0.8, r=["og_d"], w=["og_d"])

        mark_A = A.off
        hT = sb([8, S], BF16)
        mixed = sb([NT, D], BF16)
        QKs = [sb([4, S], BF16), sb([4, S], BF16)]
        Vbs = [sb([NT, 130], BF16), sb([NT, 130], BF16)]
        wg = [sb([8, 384], BF16), sb([8, 384], BF16)]
        xt = sb([D]); hb = sb([D], BF16); sqj = sb([D], BF16)
        ss = sb([1]); lnv = sb([1]); rstd = sb([1])
        sqt = sb([256]); ssq4 = sb([4]); ln4 = sb([4]); r4 = sb([4])
        t1 = sb([256]); t2 = sb([256]); m1 = sb([256]); m2 = sb([256]); st = sb([256], BF16)
        km = sb([2, 8]); kmb = sb([2, 8], BF16)
        gm = sb([NT, 2, 8]); m8 = sb([NT, 2, 8]); sel = sb([NT, 2, 8])
        bst = sb([NT, 2, 72], BF16)
        PT = [sb([512], BF16) for _ in range(3)]
        rl = [sb([4, 1]), sb([4, 1])]
        of = sb([4, 128]); o2 = sb([4, 128]); tt = sb([4, 128])
        ms4 = sb([4]); ln4b = sb([4]); rr4 = sb([4])
        mT = sb([8, 128], BF16)
        xr = sb([D]); x1 = sb([D])

        for bi_ in range(2):
            dma("pool", QKs[bi_][64:128, 2, :], c_kaug, "QKaug", w=["QK%d" % bi_])
            dma("pool", QKs[bi_][64:128, 3, :], c_kaug, "QKaug", w=["QK%d" % bi_])
            I("pool", "memset", Vbs[bi_][:, :, 128:130], 1.0, w=["Vb%d" % bi_])
        I("pool", "memset", bst, 0.0, w=["bst"])
        I("pool", "memset", m8, 0.0, w=["m8"])

        ptr_h = ptr[:, :].rearrange("p (c t) -> p c t", t=128)

        def group_cols(g):
            if g < 4:
                return (128 * g, 512 + 128 * g, 1024 + 128 * g)
            h = g - 4
            return (1536 + 128 * h, 2048 + 128 * h, 2560 + 128 * h)

        def load_wg(g):
            buf = wg[g % 2]
            key = "wg%d" % (g % 2)
            for i, c0 in enumerate(group_cols(g)):
                dma("pool", buf[:, :, i * 128:(i + 1) * 128],
                    w_in[:, c0:c0 + 128].rearrange("(c p) n -> p c n", p=128), key, w=[key])

        final_ops = []
        pt_i = [0]
        s_i = [0]

        if not do_attn:
            for b in range(B_loc):
                for t in range(NT):
                    rows = slice(t * 128, (t + 1) * 128)
                    dma("sp", xt, x[b, rows, :], "xt", w=["xt"])
                    o = dma("sp", out[b, rows, :], xt, "xto", r=["xt"], w=[("out", b, t)])
                    final_ops.append(o)
        for b in range(B_loc if do_attn else 0):
            for t in range(NT):
                rows = slice(t * 128, (t + 1) * 128)
                dma("sp", xt, x[b, rows, :], "xt", w=["xt"])
                I("act", "activation", out=sqj, in_=xt, func=AF.Square, accum_out=ss,
                     r=["xt"], w=["sqj", "ss"])
                I("act", "activation", out=lnv, in_=ss, func=AF.Ln, scale=1.0 / D, bias=epst,
                     r=["ss", "epst"], w=["lnv"])
                I("act", "activation", out=rstd, in_=lnv, func=AF.Exp, scale=-0.5, r=["lnv"], w=["rstd"])
                I("dve", "scalar_tensor_tensor", out=hb, in0=xt, scalar=rstd, in1=g1,
                                                             op0=ALU.mult, op1=ALU.mult,
                     r=["xt", "rstd", "g1"], w=["hb"])
                for c in range(8):
                    I("pe", "transpose", ptr_h[:, c, :], hb[:, c * 128:(c + 1) * 128], ident_b,
                         r=["hb", "ident_b"], w=["ptr"])
                I("dve", "tensor_copy", hT[:, :, rows], ptr_h, r=["ptr"], w=["hT"])

            def grp_ctx(g):
                moba = g < 4
                return (moba, wg[g % 2], 'wg%d' % (g % 2), gq_m if moba else gq_d, 'gq_m' if moba else 'gq_d',
                        QKs[g % 2], 'QK%d' % (g % 2), Vbs[g % 2], 'Vb%d' % (g % 2))

            def inproj_tile(g, t):
                moba, wgb, wkey, gq, gqk, QK, qkk, Vb, vbk = grp_ctx(g)
                rows = slice(t * 128, (t + 1) * 128)
                for c in range(8):
                    I("pe", "matmul",
                        pj[:, 0:384], lhsT=hT[:, c, rows], rhs=wgb[:, c, :], start=(c == 0), stop=(c == 7),
                        r=["hT", wkey], w=["pj"])
                I("act", "activation", out=sqt, in_=pj[:, 0:256], func=AF.Square, r=["pj"], w=["sqt"])
                I("act", "copy", Vb[:, t, 0:128], pj[:, 256:384], r=["pj"], w=[vbk])
                I("dve", "tensor_reduce", out=ssq4, in_=sqt.rearrange("p (a d) -> p a d", d=64),
                                                      axis=AX.X, op=ALU.add, r=["sqt"], w=["ssq4"])
                I("act", "activation", out=ln4, in_=ssq4, func=AF.Ln, scale=1.0 / 64, bias=epst,
                     r=["ssq4", "epst"], w=["ln4"])
                I("act", "activation", out=r4, in_=ln4, func=AF.Exp, scale=-0.5, r=["ln4"], w=["r4"])
                I("dve", "tensor_tensor",
                    out=t1.rearrange("p (a d) -> p a d", d=64), in0=pj[:, 0:256].rearrange("p (a d) -> p a d", d=64),
                    in1=r4.unsqueeze(2).to_broadcast([128, 4, 64]), op=ALU.mult, r=["pj", "r4"], w=["t1"])
                I("pool", "tensor_tensor", out=t2, in0=t1, in1=gq, op=ALU.mult,
                     r=["t1", gqk], w=["t2"])
                t2v = t2.rearrange("p (a h d) -> p a h d", h=2, d=32)
                m1v = m1.rearrange("p (a h d) -> p a h d", h=2, d=32)
                m2v = m2.rearrange("p (a h d) -> p a h d", h=2, d=32)
                stv = st.rearrange("p (a h d) -> p a h d", h=2, d=32)
                cosb = cos_t[:, t, :].unsqueeze(1).to_broadcast([128, 8, 32])
                sinb = sin_t[:, t, :].unsqueeze(1).to_broadcast([128, 4, 32])
                I("pool", "tensor_tensor",
                    out=m1.rearrange("p (a d) -> p a d", d=32), in0=t2.rearrange("p (a d) -> p a d", d=32),
                    in1=cosb, op=ALU.mult, r=["t2", "cos_t"], w=["m1"])
                I("dve", "tensor_tensor",
                    out=m2v[:, :, 0, :], in0=t2v[:, :, 1, :], in1=sinb, op=ALU.mult, r=["t2", "sin_t"], w=["m2a"])
                I("pool", "tensor_tensor",
                    out=m2v[:, :, 1, :], in0=t2v[:, :, 0, :], in1=sinb, op=ALU.mult, r=["t2", "sin_t"], w=["m2b"])
                I("dve", "tensor_tensor",
                    out=stv[:, :, 0, :], in0=m1v[:, :, 0, :], in1=m2v[:, :, 0, :], op=ALU.subtract,
                    r=["m1", "m2a"], w=["sta"])
                I("pool", "tensor_tensor",
                    out=stv[:, :, 1, :], in0=m1v[:, :, 1, :], in1=m2v[:, :, 1, :], op=ALU.add,
                    r=["m1", "m2b"], w=["stb"])

            def inproj_tr(g, t):
                moba, wgb, wkey, gq, gqk, QK, qkk, Vb, vbk = grp_ctx(g)
                rows = slice(t * 128, (t + 1) * 128)
                for i in range(4):
                    I("pe", "transpose", ptr_h[0:64, i, :], st[:, i * 64:(i + 1) * 64], ident_b,
                         r=["sta", "stb", "ident_b"], w=["ptr"])
                I("dve", "tensor_copy", QK[0:64, :, rows], ptr_h[0:64, 0:4, :],
                     r=["ptr"], w=[qkk])


            def gating(g):
                moba, wgb, wkey, gq, gqk, QK, qkk, Vb, vbk = grp_ctx(g)
                if not moba:
                    return
                I("dve", "tensor_reduce",
                    out=km[0:64], in_=QK[0:64, 2:4, :].rearrange("p m (n k) -> p m n k", k=256),
                    axis=AX.X, op=ALU.add, r=[qkk], w=["km"])
                I("dve", "tensor_copy", kmb[0:64], km[0:64], r=["km"], w=["kmb"])
                pjg = pj[:, 0:256].rearrange("p (t m n) -> p t m n", m=2, n=8)
                for t in range(NT):
                    for m in range(2):
                        I("pe", "matmul",
                            pjg[:, t, m, :], lhsT=QK[0:64, m, t * 128:(t + 1) * 128], rhs=kmb[0:64, m, :],
                            start=True, stop=True, r=[qkk, "kmb"], w=["pj"])
                I("dve", "tensor_tensor",
                    out=gm, in0=pjg, in1=past_t.unsqueeze(2).to_broadcast([128, NT, 2, 8]), op=ALU.add,
                    r=["pj", "past_t"], w=["gm"])
                for t in range(8, NT):
                    for m in range(2):
                        I("dve", "max", out=m8[:, t, m, :], in_=gm[:, t, m, :],
                             r=["gm"], w=["m8"])
                I("dve", "tensor_tensor",
                    out=sel, in0=gm, in1=m8[:, :, :, 2:3].to_broadcast([128, NT, 2, 8]), op=ALU.is_ge,
                    r=["gm", "m8"], w=["sel"])
                I("dve", "tensor_scalar", sel, sel, -NEG, NEG, ALU.mult, ALU.add, r=["sel"], w=["sel"])
                I("dve", "tensor_tensor",
                    out=bst[:, :, :, 64:72], in0=sel, in1=own_t.unsqueeze(2).to_broadcast([128, NT, 2, 8]),
                    op=ALU.max, r=["sel", "own_t"], w=["bst"])
                ptr_a = ptr[:, :].rearrange("p (t m q) -> p t m q", m=2, q=128)
                for t0 in range(0, NT, 4):
                    for tl in range(4):
                        for m in range(2):
                            I("pe", "transpose",
                                ptr_a[0:72, tl, m, :], bst[:, t0 + tl, m, :], ident_b,
                                r=["bst", "ident_b"], w=["ptr"])
                    I("dve", "tensor_copy",
                        QK[64:72, 0:2, t0 * 128:(t0 + 4) * 128].rearrange("p m (t q) -> p t m q", q=128),
                        ptr_a[64:72], r=["ptr"], w=[qkk])


            def attention(g, hooks):
                moba, wgb, wkey, gq, gqk, QK, qkk, Vb, vbk = grp_ctx(g)
                nsc = 0
                K = 72 if moba else 64
                for c in range(4):
                    for m in range(2):
                        ac = acc[m]
                        akey = "acc%d" % m
                        def emit_score(j, c=c, m=m):
                            qlo = max(128 * j, 512 * c)
                            N = 512 * (c + 1) - qlo
                            diag = 128 * j >= 512 * c
                            Sx = Sb[s_i[0] % 2]
                            skey = "S%d" % (s_i[0] % 2)
                            s_i[0] += 1
                            I("pe", "matmul",
                                Sx[:, 0:N], lhsT=QK[0:K, 2 + m, j * 128:(j + 1) * 128], rhs=QK[0:K, m, qlo:qlo + N],
                                start=True, stop=(not diag), r=[qkk], w=[skey])
                            if diag:
                                I("pe", "matmul",
                                    Sx[:, 0:128], lhsT=ident_b, rhs=tri_b, start=False, stop=True,
                                    r=["ident_b", "tri_b"], w=[skey])
                            pti = pt_i[0] % 3
                            pt_i[0] += 1
                            PTx = PT[pti]
                            pkey = "PT%d" % pti
                            I("act", "activation",
                                out=PTx[:, 0:N], in_=Sx[:, 0:N], func=AF.Exp, scale=0.125, r=[skey], w=[pkey])
                            return PTx, pkey, qlo

                        nj = 4 * c + 4
                        cur = emit_score(0)
                        for j in range(nj):
                            nxt = emit_score(j + 1) if j + 1 < nj else None
                            PTx, pkey, qlo = cur
                            nsc += 1
                            hook_now = hooks.get(nsc, ())
                            for i in range(max(j, 4 * c), 4 * c + 4):
                                li = i - 4 * c
                                off = 128 * i - qlo
                                I("pe", "matmul",
                                    ac[:, li, 0:129], lhsT=PTx[:, off:off + 128], rhs=Vb[:, j, 0:129],
                                    start=(j == 0 and li % 2 == 0), stop=(j == i), skip_group_check=True,
                                    r=[pkey, vbk], w=[akey])
                            for fn in hook_now:
                                fn()
                            cur = nxt
                        rlm = rl[m]
                        I("dve", "reciprocal", rlm, ac[:, :, 128:129],
                             r=[akey], w=["rl%d" % m])
                        if moba:
                            h = 2 * g + m
                            I("dve", "tensor_tensor",
                                out=of[:, :, 0:64], in0=ac[:, :, m * 64:(m + 1) * 64],
                                in1=rlm.to_broadcast([128, 4, 64]), op=ALU.mult, r=[akey, "rl%d" % m], w=["of"])
                            W_ = 64
                            gsl = og_m[:, h * 64:(h + 1) * 64]
                            gk = "og_m"
                            col0 = h * 64
                        elif m == 0:
                            I("dve", "tensor_tensor",
                                out=of, in0=ac[:, :, 0:128], in1=rlm.to_broadcast([128, 4, 128]), op=ALU.mult,
                                r=[akey, "rl0"], w=["of"])
                            continue
                        else:
                            h = g - 4
                            I("dve", "tensor_tensor",
                                out=tt, in0=ac[:, :, 0:128], in1=rlm.to_broadcast([128, 4, 128]), op=ALU.mult,
                                r=[akey, "rl1"], w=["tt"])
                            I("dve", "scalar_tensor_tensor",
                                out=of, in0=tt, scalar=neglam, in1=of, op0=ALU.mult, op1=ALU.add,
                                r=["tt", "neglam", "of"], w=["of"])
                            W_ = 128
                            gsl = og_d[:, h * 128:(h + 1) * 128]
                            gk = "og_d"
                            col0 = 512 + h * 128
                        I("pool", "tensor_tensor",
                            out=o2[:, :, 0:W_], in0=of[:, :, 0:W_], in1=of[:, :, 0:W_], op=ALU.mult, r=["of"], w=["o2"])
                        I("dve", "tensor_reduce", out=ms4, in_=o2[:, :, 0:W_], axis=AX.X, op=ALU.add,
                             r=["o2"], w=["ms4"])
                        I("act", "activation", out=ln4b, in_=ms4, func=AF.Ln, scale=1.0 / W_, bias=epst,
                             r=["ms4", "epst"], w=["ln4b"])
                        I("act", "activation", out=rr4, in_=ln4b, func=AF.Exp, scale=-0.5,
                             r=["ln4b"], w=["rr4"])
                        I("dve", "tensor_tensor",
                            out=o2[:, :, 0:W_], in0=of[:, :, 0:W_], in1=rr4.unsqueeze(2).to_broadcast([128, 4, W_]),
                            op=ALU.mult, r=["of", "rr4", "o2"], w=["o2"])
                        I("pool", "tensor_tensor",
                            out=mixed[:, 4 * c:4 * c + 4, col0:col0 + W_], in0=o2[:, :, 0:W_],
                            in1=gsl.unsqueeze(1).to_broadcast([128, 4, W_]), op=ALU.mult,
                            r=["o2", gk], w=["mixed"])


            load_wg(0)
            load_wg(1)
            for t in range(NT):
                inproj_tile(0, t)
                inproj_tr(0, t)
            gating(0)
            for g in range(8):
                hooks = {}
                if g + 1 < 8:
                    for t in range(NT):
                        hk = []
                        if t > 0:
                            hk.append(lambda g=g, t=t: inproj_tr(g + 1, t - 1))
                        hk.append(lambda g=g, t=t: inproj_tile(g + 1, t))
                        hooks[2 + 4 * t] = hk
                    hooks[66] = [lambda g=g: inproj_tr(g + 1, NT - 1)]
                    hooks[70] = [lambda g=g: gating(g + 1)]
                    if g + 2 < 8:
                        hooks[1] = [lambda g=g: load_wg(g + 2)]
                attention(g, hooks)

            for t in range(NT):
                rows = slice(t * 128, (t + 1) * 128)
                dma("sp", xr, x[b, rows, :], "xr", w=["xr"])
                for c in range(8):
                    I("pe", "transpose", ptr_h[:, c, :], mixed[:, t, c * 128:(c + 1) * 128], ident_b,
                         r=["mixed", "ident_b"], w=["ptr"])
                I("act", "copy", mT, ptr_h, r=["ptr"], w=["mT"])
                for hf in range(2):
                    for c in range(8):
                        I("pe", "matmul",
                            Sb[hf][:, 0:512], lhsT=mT[:, c, :], rhs=wout[:, c, hf * 512:(hf + 1) * 512],
                            start=(c == 0), stop=(c == 7), r=["mT", "wout"], w=["S%d" % hf])
                    I("dve", "tensor_tensor",
                        out=x1[:, hf * 512:(hf + 1) * 512], in0=Sb[hf][:, 0:512], in1=xr[:, hf * 512:(hf + 1) * 512],
                        op=ALU.add, r=["S%d" % hf, "xr"], w=["x1"])
                o = dma("sp", out[b, rows, :], x1, "x1", r=["x1"], w=[("out", b, t)])
                final_ops.append(o)

        if do_peer:
            final_ops = []
            P.barrier()
            A.off = mark_shared
            Wq = sb([8, 2048], BF16); keysT = sb([16, 128], BF16); g2 = sb([D])
            iota_b = sb([128], BF16); iota16 = sb([16])
            h2Ts = [sb([8, 256], BF16), sb([8, 256], BF16)]; qT = sb([16, 256], BF16)
            x1g = sb([2, D]); h2 = sb([D], BF16)
            ss2 = sb([1]); lnv2 = sb([1]); rstd2 = sb([1])
            sc = sb([16, 128]); scr = sb([128]); scr2 = sb([256])
            m16 = sb([16, 16]); i16 = sb([16, 16], U32); i16f = sb([16, 16])
            cand = sb([8, 256]); b16 = sb([8, 16]); p16 = sb([8, 16], U32)
            pa = sb([8, 16], U32); pb = sb([8, 16], U32); paf = sb([8, 16]); pbf = sb([8, 16])
            eb = sb([8, 16]); es = sb([8]); er = sb([8])
            IJG_tms = [sb([3, 128]), sb([3, 128])]; IJG = sb([3, 256])
            oh = sc.rearrange("p a b -> p (a b)").rearrange("p (h x) -> p h x", x=256)
            A01s = [sb([16, 128], BF16) for _ in range(2)]; Ags = [sb([16, 128], BF16) for _ in range(2)]
            B01s = [sb([16, 128], BF16) for _ in range(2)]
            dTb = [sb([8, 128], BF16) for _ in range(3)]
            upb = [sb([D], BF16) for _ in range(3)]
            off_WT = A.off
            dn_b = [sb([D], BF16) for _ in range(2)]
            up_b = [sb([D], BF16) for _ in range(2)]
            dT_sb = [sb([8, 128], BF16) for _ in range(2)]
            kst = sb([16, 128], BF16)
            A.off = off_WT
            WT = sb([128, 256], BF16)

            dma("pool", Wq, peer_query.rearrange("(c p) n -> p c n", p=128), "Wq", w=["Wq"])
            dma("pool", kst, peer_sub_keys.rearrange("(hp n) d -> n hp d", n=128), "kst", w=["kst"])
            dma("sp", g2, ffn_norm[0:1, :].to_broadcast([128, D]), "g2", w=["g2"])
            dma("pool", iota_b, c_iota, "iota_b", w=["iota_b"])
            dma("sp", iota16, c_iota[:, 0:16], "iota16", w=["iota16"])
            for h8 in range(2):
                for k in range(8):
                    I("pe", "transpose", ptr_h[:, k, :], kst[:, h8 * 8 + k, :], ident_b, r=["kst", "ident_b"], w=["ptr"])
                I("dve", "tensor_copy", keysT[:, h8 * 8:(h8 + 1) * 8, :], ptr_h, r=["ptr"], w=["keysT"])
            for i in range(128):
                bi = i % 2
                rows = slice(i * 128, (i + 1) * 128)
                dma("pool", dn_b[bi], peer_down[rows, :], "dn_b%d" % bi, w=["dn_b%d" % bi])
                for c in range(8):
                    I("pe", "transpose", ptr_h[:, c, :], dn_b[bi][:, c * 128:(c + 1) * 128], ident_b,
                      r=["dn_b%d" % bi, "ident_b"], w=["ptr"])
                I("act" if i % 2 else "dve", "copy" if i % 2 else "tensor_copy", dT_sb[bi], ptr_h,
                  r=["ptr"], w=["dT_sb%d" % bi])
                dma("sp", downT_s[i], dT_sb[bi], "dT_sbo%d" % bi, r=["dT_sb%d" % bi], w=[("dTs", i)])
                dma("pool", up_b[bi], peer_up[rows, :], "up_bi%d" % bi, w=["up_b%d" % bi])
                dma("sp", up_s[i], up_b[bi], "up_bo%d" % bi, r=["up_b%d" % bi], w=[("ups", i)])
            P.barrier()

            allWT = [("WT", i) for i in range(128)]
            NG = S // 256
            groups = [(b, gi) for b in range(B_loc) for gi in range(NG)]
            if n_groups is not None:
                groups = groups[:n_groups]
            x1gs = [x1g, sb([2, D])]

            def p_load(n, tt):
                b, gi = groups[n]
                xg = x1gs[n % 2]
                xk = "x1g%d" % (n % 2)
                t = gi * 2 + tt
                rows = slice(t * 128, (t + 1) * 128)
                dma("sp", xg[:, tt, :], out[b, rows, :], xk, r=[("out", b, t)], w=[xk])
                I("act", "activation", out=h2, in_=xg[:, tt, :], func=AF.Square, accum_out=ss2,
                  r=[xk], w=["h2", "ss2"])
                I("act", "activation", out=lnv2, in_=ss2, func=AF.Ln, scale=1.0 / D, bias=epst,
                  r=["ss2", "epst"], w=["lnv2"])
                I("act", "activation", out=rstd2, in_=lnv2, func=AF.Exp, scale=-0.5, r=["lnv2"], w=["rstd2"])
                I("dve", "scalar_tensor_tensor", out=h2, in0=xg[:, tt, :], scalar=rstd2, in1=g2,
                  op0=ALU.mult, op1=ALU.mult, r=[xk, "rstd2", "g2"], w=["h2"])
                for c in range(8):
                    I("pe", "transpose", ptr_h[:, c, :], h2[:, c * 128:(c + 1) * 128], ident_b,
                      r=["h2", "ident_b"], w=["ptr"])
                I("dve", "tensor_copy", h2Ts[n % 2][:, :, tt * 128:(tt + 1) * 128], ptr_h, r=["ptr"], w=["h2T%d" % (n % 2)])

            def p_q(n, hp):
                Sx = pj
                skey = "pj"
                h2T = h2Ts[n % 2]
                for c in range(8):
                    I("pe", "matmul", Sx[:, 0:256], lhsT=Wq[:, c, hp * 128:(hp + 1) * 128], rhs=h2T[:, c, :],
                      start=(c == 0), stop=(c == 7), r=["Wq", "h2T%d" % (n % 2)], w=[skey])
                if hp % 2:
                    I("act", "copy", qT[:, hp, :], Sx[:, 0:256], r=[skey], w=["qT"])
                else:
                    I("dve", "tensor_copy", qT[:, hp, :], Sx[:, 0:256], r=[skey], w=["qT"])

            def p_topk(n, tt):
                tsl = slice(tt * 128, (tt + 1) * 128)
                IJG_tm = IJG_tms[tt]
                ijk = "IJG_tm%d" % tt
                for q4 in range(4):
                    Sx = pj
                    skey = "pj"
                    for k in range(4):
                        hp = q4 * 4 + k
                        I("pe", "matmul", Sx[:, k * 128:(k + 1) * 128], lhsT=qT[:, hp, tsl], rhs=keysT[:, hp, :],
                          start=True, stop=True, r=["qT", "keysT"], w=[skey])
                    I("act", "copy", sc[:, q4 * 4:(q4 + 1) * 4, :], Sx[:, :].rearrange("p (a n) -> p a n", n=128),
                      r=[skey], w=["sc"])
                for hp in range(16):
                    I("dve", "max", out=m16[:, hp, 0:8], in_=sc[:, hp, :], r=["sc"], w=["m16"])
                    I("dve", "max_index", out=i16[:, hp, 0:8], in_max=m16[:, hp, 0:8], in_values=sc[:, hp, :],
                      r=["sc", "m16"], w=["i16"])
                    I("dve", "match_replace", out=scr, in_to_replace=m16[:, hp, 0:8], in_values=sc[:, hp, :],
                      imm_value=-1e30, r=["sc", "m16"], w=["scr"])
                    I("dve", "max", out=m16[:, hp, 8:16], in_=scr, r=["scr"], w=["m16"])
                    I("dve", "max_index", out=i16[:, hp, 8:16], in_max=m16[:, hp, 8:16], in_values=scr,
                      r=["scr", "m16"], w=["i16"])
                m16v = m16.rearrange("p (h two) k -> p h two k", two=2)
                candv = cand.rearrange("p h (a b) -> p h a b", b=16)
                I("dve", "tensor_tensor", out=candv,
                  in0=m16v[:, :, 0, :].unsqueeze(3).to_broadcast([128, 8, 16, 16]),
                  in1=m16v[:, :, 1, :].unsqueeze(2).to_broadcast([128, 8, 16, 16]), op=ALU.add,
                  r=["m16"], w=["cand"])
                for h in range(8):
                    I("dve", "max", out=b16[:, h, 0:8], in_=cand[:, h, :], r=["cand"], w=["b16"])
                    I("dve", "max_index", out=p16[:, h, 0:8], in_max=b16[:, h, 0:8], in_values=cand[:, h, :],
                      r=["cand", "b16"], w=["p16"])
                    I("dve", "match_replace", out=scr2, in_to_replace=b16[:, h, 0:8], in_values=cand[:, h, :],
                      imm_value=-1e30, r=["cand", "b16"], w=["scr2"])
                    I("dve", "max", out=b16[:, h, 8:16], in_=scr2, r=["scr2"], w=["b16"])
                    I("dve", "max_index", out=p16[:, h, 8:16], in_max=b16[:, h, 8:16], in_values=scr2,
                      r=["scr2", "b16"], w=["p16"])
                I("dve", "tensor_tensor", out=eb, in0=b16, in1=b16[:, :, 0:1].to_broadcast([128, 8, 16]),
                  op=ALU.subtract, r=["b16"], w=["eb"])
                I("act", "activation", out=eb, in_=eb, func=AF.Exp, r=["eb"], w=["eb"])
                I("dve", "tensor_reduce", out=es, in_=eb, axis=AX.X, op=ALU.add, r=["eb"], w=["es"])
                I("dve", "reciprocal", er, es, r=["es"], w=["er"])
                I("dve", "tensor_tensor", out=IJG_tm[:, 2, :].rearrange("p (h k) -> p h k", k=16), in0=eb,
                  in1=er.unsqueeze(2).to_broadcast([128, 8, 16]), op=ALU.mult, r=["eb", "er"], w=[ijk])
                I("dve", "tensor_single_scalar", pa, p16, 4, ALU.logical_shift_right, r=["p16"], w=["pa"])
                I("dve", "tensor_single_scalar", pb, p16, 15, ALU.bitwise_and, r=["p16"], w=["pb"])
                I("dve", "tensor_copy", paf, pa, r=["pa"], w=["paf"])
                I("dve", "tensor_copy", pbf, pb, r=["pb"], w=["pbf"])
                I("dve", "tensor_copy", i16f, i16, r=["i16"], w=["i16f"])
                i16v = i16f.rearrange("p (h two) k -> p h two k", two=2)
                ohv = oh.rearrange("p h (k a) -> p h k a", a=16)
                for pf, pfk, which in ((paf, "paf", 0), (pbf, "pbf", 1)):
                    I("dve", "tensor_tensor", out=ohv, in0=pf.unsqueeze(3).to_broadcast([128, 8, 16, 16]),
                      in1=iota16.unsqueeze(1).unsqueeze(1).to_broadcast([128, 8, 16, 16]), op=ALU.is_equal,
                      r=[pfk, "iota16"], w=["sc"])
                    I("pool", "tensor_tensor", out=ohv, in0=ohv,
                      in1=i16v[:, :, which, :].unsqueeze(2).to_broadcast([128, 8, 16, 16]), op=ALU.mult,
                      r=["sc", "i16f"], w=["sc"])
                    I("dve", "tensor_reduce", out=IJG_tm[:, which, :].rearrange("p (h k) -> p h k", k=16),
                      in_=ohv, axis=AX.X, op=ALU.add, r=["sc"], w=[ijk])

            def p_tr(n, tt):
                tsl = slice(tt * 128, (tt + 1) * 128)
                IJG_tm = IJG_tms[tt]
                for k3 in range(3):
                    I("pe", "transpose", pj[:, k3 * 128:(k3 + 1) * 128], IJG_tm[:, k3, :], ident_f,
                      r=["IJG_tm%d" % tt, "ident_f"], w=["pj"])
                I("dve", "tensor_copy", IJG[:, :, tsl], pj[:, 0:384].rearrange("p (a t) -> p a t", t=128),
                  r=["pj"], w=["IJG"])

            def prologue_sched(n):
                sch = {}
                sch[2] = [lambda: p_load(n, 0)]
                sch[6] = [lambda: p_load(n, 1)]
                for hp in range(16):
                    sch[10 + 2 * hp] = [lambda hp=hp: p_q(n, hp)]
                sch[44] = [lambda: p_topk(n, 0)]
                sch[48] = [lambda: p_topk(n, 1)]
                return sch

            def tr_sched(n):
                return {2: [lambda: p_tr(n, 0)], 6: [lambda: p_tr(n, 1)]}

            def run_prologue(n):
                for sch in (prologue_sched(n), tr_sched(n)):
                    for k in sorted(sch):
                        for fn in sch[k]:
                            fn()

            def U_phase(n, sch):
                h2T = h2Ts[n % 2]
                for i in range(128):
                    bi = i % 3
                    dkey = "dTb%d" % bi
                    dma("sp" if i % 2 == 0 else "pool", dTb[bi], downT_s[i], dkey, r=[("dTs", i)], w=[dkey])
                    Sx = Sb[i % 2]
                    skey = "S%d" % (i % 2)
                    for c in range(8):
                        I("pe", "matmul", Sx[:, 0:256], lhsT=dTb[bi][:, c, :], rhs=h2T[:, c, :],
                          start=(c == 0), stop=(c == 7), r=[dkey, "h2T%d" % (n % 2)], w=[skey])
                    I("act", "activation", out=WT[:, i, :], in_=Sx[:, 0:256], func=AF.Gelu, r=[skey], w=[("WT", i)])
                    for fn in sch.get(i, ()):
                        fn()

            def G_phase(n):
                for ci, t0 in enumerate(range(0, 256, 16)):
                    pb_ = ci % 2
                    A01 = A01s[pb_]; Ag = Ags[pb_]; B01 = B01s[pb_]
                    ka = "A01_%d" % pb_; kg = "Ag_%d" % pb_; kb = "B01_%d" % pb_
                    iob = iota_b.unsqueeze(1).to_broadcast([128, 16, 128])
                    I("dve", "tensor_tensor", out=A01, in0=iob,
                      in1=IJG[:, 0, t0:t0 + 16].unsqueeze(2).to_broadcast([128, 16, 128]), op=ALU.is_equal,
                      r=["iota_b", "IJG"], w=[ka])
                    I("dve", "tensor_tensor", out=B01, in0=iob,
                      in1=IJG[:, 1, t0:t0 + 16].unsqueeze(2).to_broadcast([128, 16, 128]), op=ALU.is_equal,
                      r=["iota_b", "IJG"], w=[kb])
                    I("pool", "tensor_tensor", out=Ag, in0=A01,
                      in1=IJG[:, 2, t0:t0 + 16].unsqueeze(2).to_broadcast([128, 16, 128]), op=ALU.mult,
                      r=[ka, "IJG"], w=[kg])
                    for half in range(2):
                        akey = "acc%d" % half
                        accv = acc[half][:, :, :].rearrange("p a (b i) -> p (a b) i", i=128)
                        for tl in range(8):
                            tk = half * 8 + tl
                            I("pe", "matmul", accv[:, tl, :], lhsT=B01[:, tk, :], rhs=Ag[:, tk, :],
                              start=True, stop=True, skip_group_check=True, r=[kb, kg], w=[akey])
                        ts8 = slice(t0 + half * 8, t0 + half * 8 + 8)
                        I("dve", "tensor_tensor", out=WT[:, :, ts8], in0=accv.rearrange("p t i -> p i t"),
                          in1=WT[:, :, ts8], op=ALU.mult, r=[akey] + allWT, w=allWT)

            def up_phase(n, sch):
                b, gi = groups[n]
                xg = x1gs[n % 2]
                xk = "x1g%d" % (n % 2)
                for i in range(128):
                    bi = i % 3
                    ukey = "upb%d" % bi
                    dma("sp" if i % 2 == 0 else "pool", upb[bi], up_s[i], ukey, r=[("ups", i)], w=[ukey])
                    for tt in range(2):
                        accf = acc[tt][:, :, :].rearrange("p a b -> p (a b)")
                        for hf in range(2):
                            I("pe", "matmul", accf[:, hf * 512:(hf + 1) * 512], lhsT=WT[:, i, tt * 128:(tt + 1) * 128],
                              rhs=upb[bi][:, hf * 512:(hf + 1) * 512], start=(i == 0), stop=(i == 127),
                              r=[("WT", i), ukey], w=["acc%d" % tt])
                    for fn in sch.get(i, ()):
                        fn()
                for tt in range(2):
                    t = gi * 2 + tt
                    rows = slice(t * 128, (t + 1) * 128)
                    accf = acc[tt][:, :, :].rearrange("p a b -> p (a b)")
                    I("dve", "tensor_tensor", out=xg[:, tt, :], in0=accf, in1=xg[:, tt, :], op=ALU.add,
                      r=["acc%d" % tt, xk], w=[xk])
                    o = dma("sp", out[b, rows, :], xg[:, tt, :], "yo%d" % (n % 2), r=[xk], w=[("out", b, t)])
                    final_ops.append(o)

            run_prologue(0)
            for n in range(len(groups)):
                more = n + 1 < len(groups)
                U_phase(n, prologue_sched(n + 1) if more else {})
                G_phase(n)
                up_phase(n, tr_sched(n + 1) if more else {})

        P.emit(final_ops)
    return nc


_NC_CACHE = {}


def kernel(**inputs):
    B = inputs["x"].shape[0]
    B_loc = B // N_CORES
    if B_loc not in _NC_CACHE:
        _NC_CACHE[B_loc] = build(B_loc)
    nc = _NC_CACHE[B_loc]
    consts = host_consts()
    f = lambda a: np.ascontiguousarray(np.asarray(a, dtype=np.float32))
    shared = {
        "attn_norm": f(inputs["attn_norm"]).reshape(1, D),
        "w_in": f(inputs["w_in"]).reshape(D, 3072),
        "q_norm_moba": f(inputs["q_norm_moba"]).reshape(1, 64),
        "k_norm_moba": f(inputs["k_norm_moba"]).reshape(1, 64),
        "q_norm_diff": f(inputs["q_norm_diff"]).reshape(2, 64),
        "k_norm_diff": f(inputs["k_norm_diff"]).reshape(2, 64),
        "lambda_q1": f(inputs["lambda_q1"]).reshape(1, 64),
        "lambda_k1": f(inputs["lambda_k1"]).reshape(1, 64),
        "lambda_q2": f(inputs["lambda_q2"]).reshape(1, 64),
        "lambda_k2": f(inputs["lambda_k2"]).reshape(1, 64),
        "moba_out_gain": f(inputs["moba_out_gain"]).reshape(1, 512),
        "diff_out_gain": f(inputs["diff_out_gain"]).reshape(1, 512),
        "w_out": f(inputs["w_out"]).reshape(D, D),
        "ffn_norm": f(inputs["ffn_norm"]).reshape(1, D),
        "peer_query": f(inputs["peer_query"]).reshape(D, 2048),
        "peer_sub_keys": f(inputs["peer_sub_keys"]).reshape(2048, 128),
        "peer_down": f(inputs["peer_down"]).reshape(16384, D),
        "peer_up": f(inputs["peer_up"]).reshape(16384, D),
    }
    shared.update(consts)
    xs = f(inputs["x"])
    in_maps = []
    for c in range(N_CORES):
        m = dict(shared)
        m["x"] = xs[c * B_loc:(c + 1) * B_loc]
        in_maps.append(m)
    res = run_bass_kernel_spmd(nc, in_maps, core_ids=list(range(N_CORES)))
    return np.concatenate([r["out"] for r in res.results], axis=0)
```

```python
import math
from contextlib import ExitStack

import numpy as np
import concourse.bass as bass
import concourse.mybir as mybir
from concourse.bass_utils import run_bass_kernel_spmd

F32 = mybir.dt.float32
BF16 = mybir.dt.bfloat16
U32 = mybir.dt.uint32
U8 = mybir.dt.uint8
AF = mybir.ActivationFunctionType
ALU = mybir.AluOpType
AX = mybir.AxisListType

ENGS = ["pe", "dve", "act", "pool", "sp"]
N_CORES = 8
D = 1024
S = 2048
NT = S // 128
EPS = 1e-6
NEG = -30000.0


class Op:
    __slots__ = ("eng", "fn", "idx", "deps", "is_dma", "dsem", "dval", "signal", "sval")

    def __init__(self, eng, fn, idx, is_dma):
        self.eng = eng
        self.fn = fn
        self.idx = idx
        self.deps = []
        self.is_dma = is_dma
        self.dsem = None
        self.dval = 0
        self.signal = False
        self.sval = 0


class Prog:
    def __init__(self, nc):
        self.nc = nc
        self.ops = {e: [] for e in ENGS}
        self.last_w = {}
        self.readers = {}
        self.seen = {e: {} for e in ENGS}
        self.dma_sems = {}
        self.barrier_deps = []
        self.dma_ops = []

    def _skey(self, dep):
        if dep.is_dma:
            return ("d", dep.dsem), dep.dval
        return ("e", dep.eng), dep.idx

    def op(self, eng, fn, r=(), w=(), dma=None):
        lst = self.ops[eng]
        o = Op(eng, fn, len(lst), dma is not None)
        if dma is not None:
            ent = self.dma_sems.setdefault(dma, [len(self.dma_sems), 0])
            ent[1] += 16
            o.dsem = dma
            o.dval = ent[1]
            self.dma_ops.append(o)
        cand = {}

        def add(dep):
            if dep is None:
                return
            if (not dep.is_dma) and dep.eng == eng and eng == "pe":
                return
            k, v = self._skey(dep)
            if k not in cand or cand[k][0] < v:
                cand[k] = (v, dep)

        for d in self.barrier_deps:
            add(d)
        for k in r:
            add(self.last_w.get(k))
        for k in w:
            add(self.last_w.get(k))
            for rd in self.readers.get(k, ()):
                add(rd)
        seen = self.seen[eng]
        for k, (v, dep) in cand.items():
            if seen.get(k, -1) >= v:
                continue
            seen[k] = v
            o.deps.append(dep)
            if not dep.is_dma:
                dep.signal = True
        for k in r:
            self.readers.setdefault(k, []).append(o)
        for k in w:
            self.last_w[k] = o
            self.readers[k] = []
        lst.append(o)
        return o

    def barrier(self):
        deps = [lst[-1] for lst in self.ops.values() if lst]
        deps = [d for d in deps if not d.is_dma]
        self.barrier_deps = deps + list(self.dma_ops)
        self.dma_ops = []

    def emit(self, final_ops):
        nc = self.nc
        with ExitStack() as es:
            esem = {e: es.enter_context(nc.semaphore("s_" + e)) for e in ENGS}
            dsem = {k: es.enter_context(nc.semaphore("d%d" % v[0])) for k, v in self.dma_sems.items()}
            for o in final_ops:
                if not o.is_dma:
                    o.signal = True
            for e in ENGS:
                c = 0
                for o in self.ops[e]:
                    if o.signal and not o.is_dma:
                        c += 1
                        o.sval = c
            block = es.enter_context(nc.Block())
            reg = {"pe": block.tensor, "dve": block.vector, "act": block.scalar,
                   "pool": block.gpsimd, "sp": block.sync}

            def make(e):
                ops = self.ops[e]

                def body(eng):
                    def wait(d):
                        if d.is_dma:
                            eng.wait_ge(dsem[d.dsem], d.dval)
                        else:
                            eng.wait_ge(esem[d.eng], d.sval)
                    for o in ops:
                        for d in o.deps:
                            wait(d)
                        meth, args, kw = o.fn
                        ins = getattr(eng, meth)(*args, **kw)
                        if o.is_dma:
                            ins.then_inc(dsem[o.dsem], 16)
                        elif o.signal:
                            ins.then_inc(esem[e], 1)
                    if e == "sp":
                        for d in final_ops:
                            wait(d)
                return body

            for e in ENGS:
                reg[e](make(e))


class Arena:
    def __init__(self, tile, size):
        self.t = tile
        self.size = size
        self.off = 0

    def alloc(self, shape, dt, nbytes_el):
        n = 1
        for s in shape:
            n *= s
        nb = n * nbytes_el
        nb_al = (nb + 63) // 64 * 64
        assert self.off + nb_al <= self.size, ("arena overflow", self.off, nb_al, self.size)
        ap = self.t[:, self.off:self.off + nb]
        self.off += nb_al
        if dt is not U8:
            ap = ap.bitcast(dt)
        if len(shape) == 2:
            ap = ap.rearrange("p (a b) -> p a b", b=shape[1])
        elif len(shape) == 3:
            ap = ap.rearrange("p (a b c) -> p a b c", b=shape[1], c=shape[2])
        return ap


def rope_consts():
    pos = np.arange(S, dtype=np.float32)
    inv = (1.0 / (np.float32(10000.0) ** (np.arange(0, 64, 2, dtype=np.float32) / np.float32(64)))).astype(np.float32)
    ang = (pos[:, None] * inv[None, :]).astype(np.float32)
    cos = np.cos(ang).astype(np.float32).reshape(NT, 128, 32).transpose(1, 0, 2)
    sin = np.sin(ang).astype(np.float32).reshape(NT, 128, 32).transpose(1, 0, 2)
    return np.ascontiguousarray(cos), np.ascontiguousarray(sin)


def host_consts():
    cos, sin = rope_consts()
    k = np.arange(128)[:, None]
    q = np.arange(128)[None, :]
    tri = np.where(k <= q, 0.0, NEG).astype(np.float32)
    kaug = np.zeros((64, S), np.float32)
    for n in range(8):
        kaug[n, n * 256:(n + 1) * 256] = 1.0
    past = np.zeros((NT, 8), np.float32)
    own = np.full((NT, 8), -1e30, np.float32)
    for t in range(NT):
        qb = t // 2
        for n in range(8):
            if n >= qb:
                past[t, n] = -1e30
            if n == qb or (qb <= 3 and n <= qb):
                own[t, n] = 0.0
    past = np.ascontiguousarray(np.broadcast_to(past[None], (128, NT, 8))).astype(np.float32)
    own = np.ascontiguousarray(np.broadcast_to(own[None], (128, NT, 8))).astype(np.float32)
    ident = np.eye(128, dtype=np.float32)
    iota = np.ascontiguousarray(np.broadcast_to(np.arange(128, dtype=np.float32)[None], (128, 128)))
    return {"c_iota": iota, "c_cos": cos, "c_sin": sin, "c_tri": tri, "c_kaug": kaug, "c_past": past,
            "c_own": own, "c_ident": ident}


def build(B_loc, do_peer=True, do_attn=True, n_groups=None):
    nc = bass.Bass("TRN2", target_bir_lowering=False)

    def din(name, shape, dt=F32):
        return nc.dram_tensor(name, list(shape), dt, kind="ExternalInput").ap()

    x = din("x", [B_loc, S, D])
    attn_norm = din("attn_norm", [1, D])
    w_in = din("w_in", [D, 3072])
    qnm = din("q_norm_moba", [1, 64])
    knm = din("k_norm_moba", [1, 64])
    qnd = din("q_norm_diff", [2, 64])
    knd = din("k_norm_diff", [2, 64])
    lq1 = din("lambda_q1", [1, 64])
    lk1 = din("lambda_k1", [1, 64])
    lq2 = din("lambda_q2", [1, 64])
    lk2 = din("lambda_k2", [1, 64])
    mog = din("moba_out_gain", [1, 512])
    dog = din("diff_out_gain", [1, 512])
    w_out = din("w_out", [D, D])
    c_cos = din("c_cos", [128, NT, 32])
    c_sin = din("c_sin", [128, NT, 32])
    c_tri = din("c_tri", [128, 128])
    c_kaug = din("c_kaug", [64, S])
    c_past = din("c_past", [128, NT, 8])
    c_own = din("c_own", [128, NT, 8])
    c_ident = din("c_ident", [128, 128])
    ffn_norm = din("ffn_norm", [1, D])
    peer_query = din("peer_query", [D, 2048])
    peer_sub_keys = din("peer_sub_keys", [2048, 128])
    peer_down = din("peer_down", [16384, D])
    peer_up = din("peer_up", [16384, D])
    c_iota = din("c_iota", [128, 128])
    downT_s = nc.dram_tensor("downT_s", [128, 128, 8, 128], BF16).ap()
    up_s = nc.dram_tensor("up_s", [128, 128, D], BF16).ap()
    out = nc.dram_tensor("out", [B_loc, S, D], F32, kind="ExternalOutput").ap()

    P = Prog(nc)
    with ExitStack() as es:
        ARENA_BYTES = 212000
        arena_t = es.enter_context(nc.sbuf_tensor("arena", [128, ARENA_BYTES], U8))
        A = Arena(arena_t, ARENA_BYTES)

        def sb(shape, dt=F32):
            return A.alloc(shape, dt, 2 if dt is BF16 else 4)

        def ps(name, shape, dt=F32):
            return es.enter_context(nc.psum_tensor(name, shape, dt))

        S0 = ps("S0", [128, 512])
        S1 = ps("S1", [128, 512])
        acc = [ps("acc0", [128, 4, 256]), ps("acc1", [128, 4, 256])]
        pj = ps("pj", [128, 512])
        ptr = ps("ptr", [128, 1024], BF16)
        Sb = [S0, S1]

        ident_f = sb([128]); ident_b = sb([128], BF16)
        epst = sb([1])
        mark_shared = A.off
        tri_b = sb([128], BF16)
        cos_t = sb([NT, 32]); sin_t = sb([NT, 32])
        past_t = sb([NT, 8]); own_t = sb([NT, 8])
        g1 = sb([D])
        gq_m = sb([256]); gq_d = sb([256])
        og_m = sb([512]); og_d = sb([512])
        lam4 = sb([4, 64]); lamt = sb([2, 64]); lams = sb([2]); lame = sb([2]); neglam = sb([1])
        wout = sb([8, D], BF16)

        def I(eng, meth, *args, r=(), w=(), dma=None, **kw):
            return P.op(eng, (meth, args, kw), r=r, w=w, dma=dma)

        def dma(eng, out_ap, in_ap, key, r=(), w=()):
            return I(eng, "dma_start", out=out_ap, in_=in_ap, r=r, w=w, dma=key)

        dma("sp", ident_f, c_ident, "ident_f", w=["ident_f"])
        dma("pool", ident_b, c_ident, "ident_b", w=["ident_b"])
        dma("pool", tri_b, c_tri, "tri_b", w=["tri_b"])
        dma("sp", cos_t, c_cos, "cos_t", w=["cos_t"])
        dma("sp", sin_t, c_sin, "sin_t", w=["sin_t"])
        dma("sp", past_t, c_past, "past_t", w=["past_t"])
        dma("sp", own_t, c_own, "own_t", w=["own_t"])
        dma("sp", g1, attn_norm[0:1, :].to_broadcast([128, D]), "g1", w=["g1"])
        for i in range(2):
            dma("sp", gq_m[:, i * 64:(i + 1) * 64], qnm[0:1, :].to_broadcast([128, 64]), "gq_m", w=["gq_m"])
            dma("sp", gq_m[:, 128 + i * 64:128 + (i + 1) * 64], knm[0:1, :].to_broadcast([128, 64]), "gq_m", w=["gq_m"])
            dma("sp", gq_d[:, i * 64:(i + 1) * 64], qnd[i:i + 1, :].to_broadcast([128, 64]), "gq_d", w=["gq_d"])
            dma("sp", gq_d[:, 128 + i * 64:128 + (i + 1) * 64], knd[i:i + 1, :].to_broadcast([128, 64]), "gq_d", w=["gq_d"])
        dma("sp", og_m, mog[0:1, :].to_broadcast([128, 512]), "og_m", w=["og_m"])
        dma("sp", og_d, dog[0:1, :].to_broadcast([128, 512]), "og_d", w=["og_d"])
        for i, v in enumerate((lq1, lk1, lq2, lk2)):
            dma("sp", lam4[:, i, :], v[0:1, :].to_broadcast([128, 64]), "lam4", w=["lam4"])
        dma("pool", wout, w_out.rearrange("(c p) n -> p c n", p=128), "wout", w=["wout"])
        I("pool", "memset", epst, EPS, w=["epst"])
        I("dve", "tensor_tensor", out=lamt, in0=lam4[:, 0:4:2, :], in1=lam4[:, 1:4:2, :], op=ALU.mult,
             r=["lam4"], w=["lamt"])
        I("dve", "tensor_reduce", out=lams, in_=lamt, axis=AX.X, op=ALU.add, r=["lamt"], w=["lams"])
        I("act", "activation", out=lame, in_=lams, func=AF.Exp, r=["lams"], w=["lame"])
        I("dve", "tensor_tensor", out=neglam, in0=lame[:, 1:2], in1=lame[:, 0:1], op=ALU.subtract,
             r=["lame"], w=["neglam"])
        I("dve", "tensor_scalar_add", neglam, neglam, -0.2, r=["neglam"], w=["neglam"])
        I("dve", "tensor_scalar_mul", og_d, og_d, 0.8, r=["og_d"], w=["og_d"])

        mark_A = A.off
        hT = sb([8, S], BF16)
        mixed = sb([NT, D], BF16)
        QKs = [sb([4, S], BF16), sb([4, S], BF16)]
        Vbs = [sb([NT, 130], BF16), sb([NT, 130], BF16)]
        wg = [sb([8, 384], BF16), sb([8, 384], BF16)]
        xt = sb([D]); hb = sb([D], BF16); sqj = sb([D], BF16)
        ss = sb([1]); lnv = sb([1]); rstd = sb([1])
        sqt = sb([256]); ssq4 = sb([4]); ln4 = sb([4]); r4 = sb([4])
        t1 = sb([256]); t2 = sb([256]); m1 = sb([256]); m2 = sb([256]); st = sb([256], BF16)
        km = sb([2, 8]); kmb = sb([2, 8], BF16)
        gm = sb([NT, 2, 8]); m8 = sb([NT, 2, 8]); sel = sb([NT, 2, 8])
        bst = sb([NT, 2, 72], BF16)
        PT = [sb([512], BF16) for _ in range(3)]
        rl = [sb([4, 1]), sb([4, 1])]
        of = sb([4, 128]); o2 = sb([4, 128]); tt = sb([4, 128])
        ms4 = sb([4]); ln4b = sb([4]); rr4 = sb([4])
        mT = sb([8, 128], BF16)
        xr = sb([D]); x1 = sb([D])

        for bi_ in range(2):
            dma("pool", QKs[bi_][64:128, 2, :], c_kaug, "QKaug", w=["QK%d" % bi_])
            dma("pool", QKs[bi_][64:128, 3, :], c_kaug, "QKaug", w=["QK%d" % bi_])
            I("pool", "memset", Vbs[bi_][:, :, 128:130], 1.0, w=["Vb%d" % bi_])
        I("pool", "memset", bst, 0.0, w=["bst"])
        I("pool", "memset", m8, 0.0, w=["m8"])

        ptr_h = ptr[:, :].rearrange("p (c t) -> p c t", t=128)

        def group_cols(g):
            if g < 4:
                return (128 * g, 512 + 128 * g, 1024 + 128 * g)
            h = g - 4
            return (1536 + 128 * h, 2048 + 128 * h, 2560 + 128 * h)

        def load_wg(g):
            buf = wg[g % 2]
            key = "wg%d" % (g % 2)
            for i, c0 in enumerate(group_cols(g)):
                dma("pool", buf[:, :, i * 128:(i + 1) * 128],
                    w_in[:, c0:c0 + 128].rearrange("(c p) n -> p c n", p=128), key, w=[key])

        final_ops = []
        pt_i = [0]
        s_i = [0]

        if not do_attn:
            for b in range(B_loc):
                for t in range(NT):
                    rows = slice(t * 128, (t + 1) * 128)
                    dma("sp", xt, x[b, rows, :], "xt", w=["xt"])
                    o = dma("sp", out[b, rows, :], xt, "xto", r=["xt"], w=[("out", b, t)])
                    final_ops.append(o)
        for b in range(B_loc if do_attn else 0):
            for t in range(NT):
                rows = slice(t * 128, (t + 1) * 128)
                dma("sp", xt, x[b, rows, :], "xt", w=["xt"])
                I("act", "activation", out=sqj, in_=xt, func=AF.Square, accum_out=ss,
                     r=["xt"], w=["sqj", "ss"])
                I("act", "activation", out=lnv, in_=ss, func=AF.Ln, scale=1.0 / D, bias=epst,
                     r=["ss", "epst"], w=["lnv"])
                I("act", "activation", out=rstd, in_=lnv, func=AF.Exp, scale=-0.5, r=["lnv"], w=["rstd"])
                I("dve", "scalar_tensor_tensor", out=hb, in0=xt, scalar=rstd, in1=g1,
                                                             op0=ALU.mult, op1=ALU.mult,
                     r=["xt", "rstd", "g1"], w=["hb"])
                for c in range(8):
                    I("pe", "transpose", ptr_h[:, c, :], hb[:, c * 128:(c + 1) * 128], ident_b,
                         r=["hb", "ident_b"], w=["ptr"])
                I("dve", "tensor_copy", hT[:, :, rows], ptr_h, r=["ptr"], w=["hT"])

            def grp_ctx(g):
                moba = g < 4
                return (moba, wg[g % 2], 'wg%d' % (g % 2), gq_m if moba else gq_d, 'gq_m' if moba else 'gq_d',
                        QKs[g % 2], 'QK%d' % (g % 2), Vbs[g % 2], 'Vb%d' % (g % 2))

            def inproj_tile(g, t):
                moba, wgb, wkey, gq, gqk, QK, qkk, Vb, vbk = grp_ctx(g)
                rows = slice(t * 128, (t + 1) * 128)
                for c in range(8):
                    I("pe", "matmul",
                        pj[:, 0:384], lhsT=hT[:, c, rows], rhs=wgb[:, c, :], start=(c == 0), stop=(c == 7),
                        r=["hT", wkey], w=["pj"])
                I("act", "activation", out=sqt, in_=pj[:, 0:256], func=AF.Square, r=["pj"], w=["sqt"])
                I("act", "copy", Vb[:, t, 0:128], pj[:, 256:384], r=["pj"], w=[vbk])
                I("dve", "tensor_reduce", out=ssq4, in_=sqt.rearrange("p (a d) -> p a d", d=64),
                                                      axis=AX.X, op=ALU.add, r=["sqt"], w=["ssq4"])
                I("act", "activation", out=ln4, in_=ssq4, func=AF.Ln, scale=1.0 / 64, bias=epst,
                     r=["ssq4", "epst"], w=["ln4"])
                I("act", "activation", out=r4, in_=ln4, func=AF.Exp, scale=-0.5, r=["ln4"], w=["r4"])
                I("dve", "tensor_tensor",
                    out=t1.rearrange("p (a d) -> p a d", d=64), in0=pj[:, 0:256].rearrange("p (a d) -> p a d", d=64),
                    in1=r4.unsqueeze(2).to_broadcast([128, 4, 64]), op=ALU.mult, r=["pj", "r4"], w=["t1"])
                I("pool", "tensor_tensor", out=t2, in0=t1, in1=gq, op=ALU.mult,
                     r=["t1", gqk], w=["t2"])
                t2v = t2.rearrange("p (a h d) -> p a h d", h=2, d=32)
                m1v = m1.rearrange("p (a h d) -> p a h d", h=2, d=32)
                m2v = m2.rearrange("p (a h d) -> p a h d", h=2, d=32)
                stv = st.rearrange("p (a h d) -> p a h d", h=2, d=32)
                cosb = cos_t[:, t, :].unsqueeze(1).to_broadcast([128, 8, 32])
                sinb = sin_t[:, t, :].unsqueeze(1).to_broadcast([128, 4, 32])
                I("pool", "tensor_tensor",
                    out=m1.rearrange("p (a d) -> p a d", d=32), in0=t2.rearrange("p (a d) -> p a d", d=32),
                    in1=cosb, op=ALU.mult, r=["t2", "cos_t"], w=["m1"])
                I("dve", "tensor_tensor",
                    out=m2v[:, :, 0, :], in0=t2v[:, :, 1, :], in1=sinb, op=ALU.mult, r=["t2", "sin_t"], w=["m2a"])
                I("pool", "tensor_tensor",
                    out=m2v[:, :, 1, :], in0=t2v[:, :, 0, :], in1=sinb, op=ALU.mult, r=["t2", "sin_t"], w=["m2b"])
                I("dve", "tensor_tensor",
                    out=stv[:, :, 0, :], in0=m1v[:, :, 0, :], in1=m2v[:, :, 0, :], op=ALU.subtract,
                    r=["m1", "m2a"], w=["sta"])
                I("pool", "tensor_tensor",
                    out=stv[:, :, 1, :], in0=m1v[:, :, 1, :], in1=m2v[:, :, 1, :], op=ALU.add,
                    r=["m1", "m2b"], w=["stb"])

            def inproj_tr(g, t):
                moba, wgb, wkey, gq, gqk, QK, qkk, Vb, vbk = grp_ctx(g)
                rows = slice(t * 128, (t + 1) * 128)
                for i in range(4):
                    I("pe", "transpose", ptr_h[0:64, i, :], st[:, i * 64:(i + 1) * 64], ident_b,
                         r=["sta", "stb", "ident_b"], w=["ptr"])
                I("dve", "tensor_copy", QK[0:64, :, rows], ptr_h[0:64, 0:4, :],
                     r=["ptr"], w=[qkk])


            def gating(g):
                moba, wgb, wkey, gq, gqk, QK, qkk, Vb, vbk = grp_ctx(g)
                if not moba:
                    return
                I("dve", "tensor_reduce",
                    out=km[0:64], in_=QK[0:64, 2:4, :].rearrange("p m (n k) -> p m n k", k=256),
                    axis=AX.X, op=ALU.add, r=[qkk], w=["km"])
                I("dve", "tensor_copy", kmb[0:64], km[0:64], r=["km"], w=["kmb"])
                pjg = pj[:, 0:256].rearrange("p (t m n) -> p t m n", m=2, n=8)
                for t in range(NT):
                    for m in range(2):
                        I("pe", "matmul",
                            pjg[:, t, m, :], lhsT=QK[0:64, m, t * 128:(t + 1) * 128], rhs=kmb[0:64, m, :],
                            start=True, stop=True, r=[qkk, "kmb"], w=["pj"])
                I("dve", "tensor_tensor",
                    out=gm, in0=pjg, in1=past_t.unsqueeze(2).to_broadcast([128, NT, 2, 8]), op=ALU.add,
                    r=["pj", "past_t"], w=["gm"])
                for t in range(8, NT):
                    for m in range(2):
                        I("dve", "max", out=m8[:, t, m, :], in_=gm[:, t, m, :],
                             r=["gm"], w=["m8"])
                I("dve", "tensor_tensor",
                    out=sel, in0=gm, in1=m8[:, :, :, 2:3].to_broadcast([128, NT, 2, 8]), op=ALU.is_ge,
                    r=["gm", "m8"], w=["sel"])
                I("dve", "tensor_scalar", sel, sel, -NEG, NEG, ALU.mult, ALU.add, r=["sel"], w=["sel"])
                I("dve", "tensor_tensor",
                    out=bst[:, :, :, 64:72], in0=sel, in1=own_t.unsqueeze(2).to_broadcast([128, NT, 2, 8]),
                    op=ALU.max, r=["sel", "own_t"], w=["bst"])
                ptr_a = ptr[:, :].rearrange("p (t m q) -> p t m q", m=2, q=128)
                for t0 in range(0, NT, 4):
                    for tl in range(4):
                        for m in range(2):
                            I("pe", "transpose",
                                ptr_a[0:72, tl, m, :], bst[:, t0 + tl, m, :], ident_b,
                                r=["bst", "ident_b"], w=["ptr"])
                    I("dve", "tensor_copy",
                        QK[64:72, 0:2, t0 * 128:(t0 + 4) * 128].rearrange("p m (t q) -> p t m q", q=128),
                        ptr_a[64:72], r=["ptr"], w=[qkk])


            def attention(g, hooks):
                moba, wgb, wkey, gq, gqk, QK, qkk, Vb, vbk = grp_ctx(g)
                nsc = 0
                K = 72 if moba else 64
                for c in range(4):
                    for m in range(2):
                        ac = acc[m]
                        akey = "acc%d" % m
                        def emit_score(j, c=c, m=m):
                            qlo = max(128 * j, 512 * c)
                            N = 512 * (c + 1) - qlo
                            diag = 128 * j >= 512 * c
                            Sx = Sb[s_i[0] % 2]
                            skey = "S%d" % (s_i[0] % 2)
                            s_i[0] += 1
                            I("pe", "matmul",
                                Sx[:, 0:N], lhsT=QK[0:K, 2 + m, j * 128:(j + 1) * 128], rhs=QK[0:K, m, qlo:qlo + N],
                                start=True, stop=(not diag), r=[qkk], w=[skey])
                            if diag:
                                I("pe", "matmul",
                                    Sx[:, 0:128], lhsT=ident_b, rhs=tri_b, start=False, stop=True,
                                    r=["ident_b", "tri_b"], w=[skey])
                            pti = pt_i[0] % 3
                            pt_i[0] += 1
                            PTx = PT[pti]
                            pkey = "PT%d" % pti
                            I("act", "activation",
                                out=PTx[:, 0:N], in_=Sx[:, 0:N], func=AF.Exp, scale=0.125, r=[skey], w=[pkey])
                            return PTx, pkey, qlo

                        nj = 4 * c + 4
                        cur = emit_score(0)
                        for j in range(nj):
                            nxt = emit_score(j + 1) if j + 1 < nj else None
                            PTx, pkey, qlo = cur
                            nsc += 1
                            hook_now = hooks.get(nsc, ())
                            for i in range(max(j, 4 * c), 4 * c + 4):
                                li = i - 4 * c
                                off = 128 * i - qlo
                                I("pe", "matmul",
                                    ac[:, li, 0:129], lhsT=PTx[:, off:off + 128], rhs=Vb[:, j, 0:129],
                                    start=(j == 0 and li % 2 == 0), stop=(j == i), skip_group_check=True,
                                    r=[pkey, vbk], w=[akey])
                            for fn in hook_now:
                                fn()
                            cur = nxt
                        rlm = rl[m]
                        I("dve", "reciprocal", rlm, ac[:, :, 128:129],
                             r=[akey], w=["rl%d" % m])
                        if moba:
                            h = 2 * g + m
                            I("dve", "tensor_tensor",
                                out=of[:, :, 0:64], in0=ac[:, :, m * 64:(m + 1) * 64],
                                in1=rlm.to_broadcast([128, 4, 64]), op=ALU.mult, r=[akey, "rl%d" % m], w=["of"])
                            W_ = 64
                            gsl = og_m[:, h * 64:(h + 1) * 64]
                            gk = "og_m"
                            col0 = h * 64
                        elif m == 0:
                            I("dve", "tensor_tensor",
                                out=of, in0=ac[:, :, 0:128], in1=rlm.to_broadcast([128, 4, 128]), op=ALU.mult,
                                r=[akey, "rl0"], w=["of"])
                            continue
                        else:
                            h = g - 4
                            I("dve", "tensor_tensor",
                                out=tt, in0=ac[:, :, 0:128], in1=rlm.to_broadcast([128, 4, 128]), op=ALU.mult,
                                r=[akey, "rl1"], w=["tt"])
                            I("dve", "scalar_tensor_tensor",
                                out=of, in0=tt, scalar=neglam, in1=of, op0=ALU.mult, op1=ALU.add,
                                r=["tt", "neglam", "of"], w=["of"])
                            W_ = 128
                            gsl = og_d[:, h * 128:(h + 1) * 128]
                            gk = "og_d"
                            col0 = 512 + h * 128
                        I("pool", "tensor_tensor",
                            out=o2[:, :, 0:W_], in0=of[:, :, 0:W_], in1=of[:, :, 0:W_], op=ALU.mult, r=["of"], w=["o2"])
                        I("dve", "tensor_reduce", out=ms4, in_=o2[:, :, 0:W_], axis=AX.X, op=ALU.add,
                             r=["o2"], w=["ms4"])
                        I("act", "activation", out=ln4b, in_=ms4, func=AF.Ln, scale=1.0 / W_, bias=epst,
                             r=["ms4", "epst"], w=["ln4b"])
                        I("act", "activation", out=rr4, in_=ln4b, func=AF.Exp, scale=-0.5,
                             r=["ln4b"], w=["rr4"])
                        I("dve", "tensor_tensor",
                            out=o2[:, :, 0:W_], in0=of[:, :, 0:W_], in1=rr4.unsqueeze(2).to_broadcast([128, 4, W_]),
                            op=ALU.mult, r=["of", "rr4", "o2"], w=["o2"])
                        I("pool", "tensor_tensor",
                            out=mixed[:, 4 * c:4 * c + 4, col0:col0 + W_], in0=o2[:, :, 0:W_],
                            in1=gsl.unsqueeze(1).to_broadcast([128, 4, W_]), op=ALU.mult,
                            r=["o2", gk], w=["mixed"])


            load_wg(0)
            load_wg(1)
            for t in range(NT):
                inproj_tile(0, t)
                inproj_tr(0, t)
            gating(0)
            for g in range(8):
                hooks = {}
                if g + 1 < 8:
                    for t in range(NT):
                        hk = []
                        if t > 0:
                            hk.append(lambda g=g, t=t: inproj_tr(g + 1, t - 1))
                        hk.append(lambda g=g, t=t: inproj_tile(g + 1, t))
                        hooks[2 + 4 * t] = hk
                    hooks[66] = [lambda g=g: inproj_tr(g + 1, NT - 1)]
                    hooks[70] = [lambda g=g: gating(g + 1)]
                    if g + 2 < 8:
                        hooks[1] = [lambda g=g: load_wg(g + 2)]
                attention(g, hooks)

            for t in range(NT):
                rows = slice(t * 128, (t + 1) * 128)
                dma("sp", xr, x[b, rows, :], "xr", w=["xr"])
                for c in range(8):
                    I("pe", "transpose", ptr_h[:, c, :], mixed[:, t, c * 128:(c + 1) * 128], ident_b,
                         r=["mixed", "ident_b"], w=["ptr"])
                I("act", "copy", mT, ptr_h, r=["ptr"], w=["mT"])
                for hf in range(2):
                    for c in range(8):
                        I("pe", "matmul",
                            Sb[hf][:, 0:512], lhsT=mT[:, c, :], rhs=wout[:, c, hf * 512:(hf + 1) * 512],
                            start=(c == 0), stop=(c == 7), r=["mT", "wout"], w=["S%d" % hf])
                    I("dve", "tensor_tensor",
                        out=x1[:, hf * 512:(hf + 1) * 512], in0=Sb[hf][:, 0:512], in1=xr[:, hf * 512:(hf + 1) * 512],
                        op=ALU.add, r=["S%d" % hf, "xr"], w=["x1"])
                o = dma("sp", out[b, rows, :], x1, "x1", r=["x1"], w=[("out", b, t)])
                final_ops.append(o)

        if do_peer:
            final_ops = []
            P.barrier()
            A.off = mark_shared
            Wq = sb([8, 2048], BF16); keysT = sb([16, 128], BF16); g2 = sb([D])
            iota_b = sb([128], BF16); iota16 = sb([16])
            h2Ts = [sb([8, 256], BF16), sb([8, 256], BF16)]; qT = sb([16, 256], BF16)
            x1g = sb([2, D]); h2 = sb([D], BF16)
            ss2 = sb([1]); lnv2 = sb([1]); rstd2 = sb([1])
            sc = sb([16, 128]); scr = sb([128]); scr2 = sb([256])
            m16 = sb([16, 16]); i16 = sb([16, 16], U32); i16f = sb([16, 16])
            cand = sb([8, 256]); b16 = sb([8, 16]); p16 = sb([8, 16], U32)
            pa = sb([8, 16], U32); pb = sb([8, 16], U32); paf = sb([8, 16]); pbf = sb([8, 16])
            eb = sb([8, 16]); es = sb([8]); er = sb([8])
            IJG_tms = [sb([3, 128]), sb([3, 128])]; IJG = sb([3, 256])
            oh = sc.rearrange("p a b -> p (a b)").rearrange("p (h x) -> p h x", x=256)
            A01s = [sb([16, 128], BF16) for _ in range(2)]; Ags = [sb([16, 128], BF16) for _ in range(2)]
            B01s = [sb([16, 128], BF16) for _ in range(2)]
            dTb = [sb([8, 128], BF16) for _ in range(3)]
            upb = [sb([D], BF16) for _ in range(3)]
            off_WT = A.off
            dn_b = [sb([D], BF16) for _ in range(2)]
            up_b = [sb([D], BF16) for _ in range(2)]
            dT_sb = [sb([8, 128], BF16) for _ in range(2)]
            kst = sb([16, 128], BF16)
            A.off = off_WT
            WT = sb([128, 256], BF16)

            dma("pool", Wq, peer_query.rearrange("(c p) n -> p c n", p=128), "Wq", w=["Wq"])
            dma("pool", kst, peer_sub_keys.rearrange("(hp n) d -> n hp d", n=128), "kst", w=["kst"])
            dma("sp", g2, ffn_norm[0:1, :].to_broadcast([128, D]), "g2", w=["g2"])
            dma("pool", iota_b, c_iota, "iota_b", w=["iota_b"])
            dma("sp", iota16, c_iota[:, 0:16], "iota16", w=["iota16"])
            for h8 in range(2):
                for k in range(8):
                    I("pe", "transpose", ptr_h[:, k, :], kst[:, h8 * 8 + k, :], ident_b, r=["kst", "ident_b"], w=["ptr"])
                I("dve", "tensor_copy", keysT[:, h8 * 8:(h8 + 1) * 8, :], ptr_h, r=["ptr"], w=["keysT"])
            for i in range(128):
                bi = i % 2
                rows = slice(i * 128, (i + 1) * 128)
                dma("pool", dn_b[bi], peer_down[rows, :], "dn_b%d" % bi, w=["dn_b%d" % bi])
                for c in range(8):
                    I("pe", "transpose", ptr_h[:, c, :], dn_b[bi][:, c * 128:(c + 1) * 128], ident_b,
                      r=["dn_b%d" % bi, "ident_b"], w=["ptr"])
                I("act" if i % 2 else "dve", "copy" if i % 2 else "tensor_copy", dT_sb[bi], ptr_h,
                  r=["ptr"], w=["dT_sb%d" % bi])
                dma("sp", downT_s[i], dT_sb[bi], "dT_sbo%d" % bi, r=["dT_sb%d" % bi], w=[("dTs", i)])
                dma("pool", up_b[bi], peer_up[rows, :], "up_bi%d" % bi, w=["up_b%d" % bi])
                dma("sp", up_s[i], up_b[bi], "up_bo%d" % bi, r=["up_b%d" % bi], w=[("ups", i)])
            P.barrier()

            allWT = [("WT", i) for i in range(128)]
            NG = S // 256
            groups = [(b, gi) for b in range(B_loc) for gi in range(NG)]
            if n_groups is not None:
                groups = groups[:n_groups]
            x1gs = [x1g, sb([2, D])]

            def p_load(n, tt):
                b, gi = groups[n]
                xg = x1gs[n % 2]
                xk = "x1g%d" % (n % 2)
                t = gi * 2 + tt
                rows = slice(t * 128, (t + 1) * 128)
                dma("sp", xg[:, tt, :], out[b, rows, :], xk, r=[("out", b, t)], w=[xk])
                I("act", "activation", out=h2, in_=xg[:, tt, :], func=AF.Square, accum_out=ss2,
                  r=[xk], w=["h2", "ss2"])
                I("act", "activation", out=lnv2, in_=ss2, func=AF.Ln, scale=1.0 / D, bias=epst,
                  r=["ss2", "epst"], w=["lnv2"])
                I("act", "activation", out=rstd2, in_=lnv2, func=AF.Exp, scale=-0.5, r=["lnv2"], w=["rstd2"])
                I("dve", "scalar_tensor_tensor", out=h2, in0=xg[:, tt, :], scalar=rstd2, in1=g2,
                  op0=ALU.mult, op1=ALU.mult, r=[xk, "rstd2", "g2"], w=["h2"])

            def p_loadB(n, tt):
                for c in range(8):
                    I("pe", "transpose", ptr_h[:, c, :], h2[:, c * 128:(c + 1) * 128], ident_b,
                      r=["h2", "ident_b"], w=["ptr"])
                I("dve", "tensor_copy", h2Ts[n % 2][:, :, tt * 128:(tt + 1) * 128], ptr_h, r=["ptr"], w=["h2T%d" % (n % 2)])

            def p_q(n, hp):
                Sx = pj[:, (hp % 2) * 256:(hp % 2) * 256 + 256]
                skey = "pj"
                h2T = h2Ts[n % 2]
                for c in range(8):
                    I("pe", "matmul", Sx, lhsT=Wq[:, c, hp * 128:(hp + 1) * 128], rhs=h2T[:, c, :],
                      start=(c == 0), stop=(c == 7), skip_group_check=True,
                      r=["Wq", "h2T%d" % (n % 2)], w=["pj0", "pj1"])
                if hp % 2:
                    I("act", "copy", qT[:, hp, :], Sx, r=["pj0", "pj1"], w=[("qT", hp)])
                else:
                    I("dve", "tensor_copy", qT[:, hp, :], Sx, r=["pj0", "pj1"], w=[("qT", hp)])

            def p_topk(n, tt):
                tsl = slice(tt * 128, (tt + 1) * 128)
                IJG_tm = IJG_tms[tt]
                ijk = "IJG_tm%d" % tt
                for q4 in range(4):
                    Sx = pj
                    skey = "pj"
                    for k in range(4):
                        hp = q4 * 4 + k
                        I("pe", "matmul", Sx[:, k * 128:(k + 1) * 128], lhsT=qT[:, hp, tsl], rhs=keysT[:, hp, :],
                          start=True, stop=True, skip_group_check=True, r=[("qT", hp), "keysT"], w=["pj0", "pj1"])
                    I("act", "copy", sc[:, q4 * 4:(q4 + 1) * 4, :], Sx[:, :].rearrange("p (a n) -> p a n", n=128),
                      r=["pj0", "pj1"], w=["sc"])
                for hp in range(16):
                    I("dve", "max", out=m16[:, hp, 0:8], in_=sc[:, hp, :], r=["sc"], w=["m16"])
                    I("dve", "max_index", out=i16[:, hp, 0:8], in_max=m16[:, hp, 0:8], in_values=sc[:, hp, :],
                      r=["sc", "m16"], w=["i16"])
                    I("dve", "match_replace", out=scr, in_to_replace=m16[:, hp, 0:8], in_values=sc[:, hp, :],
                      imm_value=-1e30, r=["sc", "m16"], w=["scr"])
                    I("dve", "max", out=m16[:, hp, 8:16], in_=scr, r=["scr"], w=["m16"])
                    I("dve", "max_index", out=i16[:, hp, 8:16], in_max=m16[:, hp, 8:16], in_values=scr,
                      r=["scr", "m16"], w=["i16"])
                m16v = m16.rearrange("p (h two) k -> p h two k", two=2)
                candv = cand.rearrange("p h (a b) -> p h a b", b=16)
                I("dve", "tensor_tensor", out=candv,
                  in0=m16v[:, :, 0, :].unsqueeze(3).to_broadcast([128, 8, 16, 16]),
                  in1=m16v[:, :, 1, :].unsqueeze(2).to_broadcast([128, 8, 16, 16]), op=ALU.add,
                  r=["m16"], w=["cand"])
                for h in range(8):
                    I("dve", "max", out=b16[:, h, 0:8], in_=cand[:, h, :], r=["cand"], w=["b16"])
                    I("dve", "max_index", out=p16[:, h, 0:8], in_max=b16[:, h, 0:8], in_values=cand[:, h, :],
                      r=["cand", "b16"], w=["p16"])
                    I("dve", "match_replace", out=scr2, in_to_replace=b16[:, h, 0:8], in_values=cand[:, h, :],
                      imm_value=-1e30, r=["cand", "b16"], w=["scr2"])
                    I("dve", "max", out=b16[:, h, 8:16], in_=scr2, r=["scr2"], w=["b16"])
                    I("dve", "max_index", out=p16[:, h, 8:16], in_max=b16[:, h, 8:16], in_values=scr2,
                      r=["scr2", "b16"], w=["p16"])
                I("dve", "tensor_tensor", out=eb, in0=b16, in1=b16[:, :, 0:1].to_broadcast([128, 8, 16]),
                  op=ALU.subtract, r=["b16"], w=["eb"])
                I("act", "activation", out=eb, in_=eb, func=AF.Exp, r=["eb"], w=["eb"])
                I("dve", "tensor_reduce", out=es, in_=eb, axis=AX.X, op=ALU.add, r=["eb"], w=["es"])
                I("dve", "reciprocal", er, es, r=["es"], w=["er"])
                I("dve", "tensor_tensor", out=IJG_tm[:, 2, :].rearrange("p (h k) -> p h k", k=16), in0=eb,
                  in1=er.unsqueeze(2).to_broadcast([128, 8, 16]), op=ALU.mult, r=["eb", "er"], w=[ijk])
                I("dve", "tensor_single_scalar", pa, p16, 4, ALU.logical_shift_right, r=["p16"], w=["pa"])
                I("dve", "tensor_single_scalar", pb, p16, 15, ALU.bitwise_and, r=["p16"], w=["pb"])
                I("dve", "tensor_copy", paf, pa, r=["pa"], w=["paf"])
                I("dve", "tensor_copy", pbf, pb, r=["pb"], w=["pbf"])
                I("dve", "tensor_copy", i16f, i16, r=["i16"], w=["i16f"])
                i16v = i16f.rearrange("p (h two) k -> p h two k", two=2)
                ohv = oh.rearrange("p h (k a) -> p h k a", a=16)
                for pf, pfk, which in ((paf, "paf", 0), (pbf, "pbf", 1)):
                    I("dve", "tensor_tensor", out=ohv, in0=pf.unsqueeze(3).to_broadcast([128, 8, 16, 16]),
                      in1=iota16.unsqueeze(1).unsqueeze(1).to_broadcast([128, 8, 16, 16]), op=ALU.is_equal,
                      r=[pfk, "iota16"], w=["sc"])
                    I("pool", "tensor_tensor", out=ohv, in0=ohv,
                      in1=i16v[:, :, which, :].unsqueeze(2).to_broadcast([128, 8, 16, 16]), op=ALU.mult,
                      r=["sc", "i16f"], w=["sc"])
                    I("dve", "tensor_reduce", out=IJG_tm[:, which, :].rearrange("p (h k) -> p h k", k=16),
                      in_=ohv, axis=AX.X, op=ALU.add, r=["sc"], w=[ijk])

            def p_tr(n, tt):
                tsl = slice(tt * 128, (tt + 1) * 128)
                IJG_tm = IJG_tms[tt]
                for k3 in range(3):
                    I("pe", "transpose", pj[:, k3 * 128:(k3 + 1) * 128], IJG_tm[:, k3, :], ident_f,
                      r=["IJG_tm%d" % tt, "ident_f"], w=["pj0", "pj1"])
                I("dve", "tensor_copy", IJG[:, :, tsl], pj[:, 0:384].rearrange("p (a t) -> p a t", t=128),
                  r=["pj0", "pj1"], w=["IJG"])

            def prologue_sched(n):
                sch = {}
                sch[0] = [lambda: p_load(n, 0)]
                sch[4] = [lambda: p_loadB(n, 0), lambda: p_load(n, 1)]
                sch[8] = [lambda: p_loadB(n, 1)]
                for hp in range(16):
                    sch[10 + hp] = [lambda hp=hp: p_q(n, hp)]
                sch[28] = [lambda: p_topk(n, 0)]
                sch[84] = [lambda: p_topk(n, 1)]
                return sch

            def tr_sched(n):
                return {2: [lambda: p_tr(n, 0)], 6: [lambda: p_tr(n, 1)]}

            def run_prologue(n):
                for sch in (prologue_sched(n), tr_sched(n)):
                    for k in sorted(sch):
                        for fn in sch[k]:
                            fn()

            def U_phase(n, sch):
                h2T = h2Ts[n % 2]
                for i in range(128):
                    bi = i % 3
                    dkey = "dTb%d" % bi
                    dma("sp" if i % 2 == 0 else "pool", dTb[bi], downT_s[i], dkey, r=[("dTs", i)], w=[dkey])
                    Sx = Sb[i % 2]
                    skey = "S%d" % (i % 2)
                    for c in range(8):
                        I("pe", "matmul", Sx[:, 0:256], lhsT=dTb[bi][:, c, :], rhs=h2T[:, c, :],
                          start=(c == 0), stop=(c == 7), r=[dkey, "h2T%d" % (n % 2)], w=[skey])
                    I("act", "activation", out=WT[:, i, :], in_=Sx[:, 0:256], func=AF.Gelu, r=[skey], w=[("WT", i)])
                    for fn in sch.get(i, ()):
                        fn()

            def G_phase(n):
                iob = iota_b.unsqueeze(1).to_broadcast([128, 16, 128])

                def build_oh(ci):
                    t0 = ci * 16
                    pb_ = ci % 2
                    I("dve", "tensor_tensor", out=A01s[pb_], in0=iob,
                      in1=IJG[:, 0, t0:t0 + 16].unsqueeze(2).to_broadcast([128, 16, 128]), op=ALU.is_equal,
                      r=["iota_b", "IJG"], w=["A01_%d" % pb_])
                    I("pool", "tensor_tensor", out=Ags[pb_], in0=A01s[pb_],
                      in1=IJG[:, 2, t0:t0 + 16].unsqueeze(2).to_broadcast([128, 16, 128]), op=ALU.mult,
                      r=["A01_%d" % pb_, "IJG"], w=["Ag_%d" % pb_])
                    I("dve", "tensor_tensor", out=B01s[pb_], in0=iob,
                      in1=IJG[:, 1, t0:t0 + 16].unsqueeze(2).to_broadcast([128, 16, 128]), op=ALU.is_equal,
                      r=["iota_b", "IJG"], w=["B01_%d" % pb_])

                def mm_evac(ci):
                    t0 = ci * 16
                    pb_ = ci % 2
                    for half in range(2):
                        akey = "acc%d" % half
                        accv = acc[half][:, :, :].rearrange("p a (b i) -> p (a b) i", i=128)
                        for tl in range(8):
                            tk = half * 8 + tl
                            I("pe", "matmul", accv[:, tl, :], lhsT=B01s[pb_][:, tk, :], rhs=Ags[pb_][:, tk, :],
                              start=True, stop=True, skip_group_check=True,
                              r=["B01_%d" % pb_, "Ag_%d" % pb_], w=[akey])
                        ts8 = slice(t0 + half * 8, t0 + half * 8 + 8)
                        I("dve", "tensor_tensor", out=WT[:, :, ts8], in0=accv.rearrange("p t i -> p i t"),
                          in1=WT[:, :, ts8], op=ALU.mult, r=[akey] + allWT, w=allWT)

                build_oh(0)
                for ci in range(16):
                    if ci + 1 < 16:
                        build_oh(ci + 1)
                    mm_evac(ci)

            def up_phase(n, sch):
                b, gi = groups[n]
                xg = x1gs[n % 2]
                xk = "x1g%d" % (n % 2)
                for i in range(128):
                    bi = i % 3
                    ukey = "upb%d" % bi
                    dma("sp" if i % 2 == 0 else "pool", upb[bi], up_s[i], ukey, r=[("ups", i)], w=[ukey])
                    for tt in range(2):
                        accf = acc[tt][:, :, :].rearrange("p a b -> p (a b)")
                        for hf in range(2):
                            I("pe", "matmul", accf[:, hf * 512:(hf + 1) * 512], lhsT=WT[:, i, tt * 128:(tt + 1) * 128],
                              rhs=upb[bi][:, hf * 512:(hf + 1) * 512], start=(i == 0), stop=(i == 127),
                              r=[("WT", i), ukey], w=["acc%d" % tt])
                    for fn in sch.get(i, ()):
                        fn()
                for tt in range(2):
                    t = gi * 2 + tt
                    rows = slice(t * 128, (t + 1) * 128)
                    accf = acc[tt][:, :, :].rearrange("p a b -> p (a b)")
                    I("dve", "tensor_tensor", out=xg[:, tt, :], in0=accf, in1=xg[:, tt, :], op=ALU.add,
                      r=["acc%d" % tt, xk], w=[xk])
                    o = dma("sp", out[b, rows, :], xg[:, tt, :], "yo%d" % (n % 2), r=[xk], w=[("out", b, t)])
                    final_ops.append(o)

            run_prologue(0)
            for n in range(len(groups)):
                more = n + 1 < len(groups)
                U_phase(n, prologue_sched(n + 1) if more else {})
                G_phase(n)
                up_phase(n, tr_sched(n + 1) if more else {})

        P.emit(final_ops)
    return nc


_NC_CACHE = {}


def kernel(**inputs):
    B = inputs["x"].shape[0]
    B_loc = B // N_CORES
    if B_loc not in _NC_CACHE:
        _NC_CACHE[B_loc] = build(B_loc)
    nc = _NC_CACHE[B_loc]
    consts = host_consts()
    f = lambda a: np.ascontiguousarray(np.asarray(a, dtype=np.float32))
    shared = {
        "attn_norm": f(inputs["attn_norm"]).reshape(1, D),
        "w_in": f(inputs["w_in"]).reshape(D, 3072),
        "q_norm_moba": f(inputs["q_norm_moba"]).reshape(1, 64),
        "k_norm_moba": f(inputs["k_norm_moba"]).reshape(1, 64),
        "q_norm_diff": f(inputs["q_norm_diff"]).reshape(2, 64),
        "k_norm_diff": f(inputs["k_norm_diff"]).reshape(2, 64),
        "lambda_q1": f(inputs["lambda_q1"]).reshape(1, 64),
        "lambda_k1": f(inputs["lambda_k1"]).reshape(1, 64),
        "lambda_q2": f(inputs["lambda_q2"]).reshape(1, 64),
        "lambda_k2": f(inputs["lambda_k2"]).reshape(1, 64),
        "moba_out_gain": f(inputs["moba_out_gain"]).reshape(1, 512),
        "diff_out_gain": f(inputs["diff_out_gain"]).reshape(1, 512),
        "w_out": f(inputs["w_out"]).reshape(D, D),
        "ffn_norm": f(inputs["ffn_norm"]).reshape(1, D),
        "peer_query": f(inputs["peer_query"]).reshape(D, 2048),
        "peer_sub_keys": f(inputs["peer_sub_keys"]).reshape(2048, 128),
        "peer_down": f(inputs["peer_down"]).reshape(16384, D),
        "peer_up": f(inputs["peer_up"]).reshape(16384, D),
    }
    shared.update(consts)
    xs = f(inputs["x"])
    in_maps = []
    for c in range(N_CORES):
        m = dict(shared)
        m["x"] = xs[c * B_loc:(c + 1) * B_loc]
        in_maps.append(m)
    res = run_bass_kernel_spmd(nc, in_maps, core_ids=list(range(N_CORES)))
    return np.concatenate([r["out"] for r in res.results], axis=0)
```

```python
import math
from contextlib import ExitStack

import numpy as np
import concourse.bass as bass
import concourse.mybir as mybir
from concourse.bass_utils import run_bass_kernel_spmd

F32 = mybir.dt.float32
BF16 = mybir.dt.bfloat16
U32 = mybir.dt.uint32
U8 = mybir.dt.uint8
AF = mybir.ActivationFunctionType
ALU = mybir.AluOpType
AX = mybir.AxisListType

ENGS = ["pe", "dve", "act", "pool", "sp"]
N_CORES = 8
D = 1024
S = 2048
NT = S // 128
EPS = 1e-6
NEG = -30000.0


class Op:
    __slots__ = ("eng", "fn", "idx", "deps", "is_dma", "dsem", "dval", "signal", "sval")

    def __init__(self, eng, fn, idx, is_dma):
        self.eng = eng
        self.fn = fn
        self.idx = idx
        self.deps = []
        self.is_dma = is_dma
        self.dsem = None
        self.dval = 0
        self.signal = False
        self.sval = 0


class Prog:
    def __init__(self, nc):
        self.nc = nc
        self.ops = {e: [] for e in ENGS}
        self.last_w = {}
        self.readers = {}
        self.seen = {e: {} for e in ENGS}
        self.dma_sems = {}
        self.barrier_deps = []
        self.dma_ops = []

    def _skey(self, dep):
        if dep.is_dma:
            return ("d", dep.dsem), dep.dval
        return ("e", dep.eng), dep.idx

    def op(self, eng, fn, r=(), w=(), dma=None):
        lst = self.ops[eng]
        o = Op(eng, fn, len(lst), dma is not None)
        if dma is not None:
            ent = self.dma_sems.setdefault(dma, [len(self.dma_sems), 0])
            ent[1] += 16
            o.dsem = dma
            o.dval = ent[1]
            self.dma_ops.append(o)
        cand = {}

        def add(dep):
            if dep is None:
                return
            if (not dep.is_dma) and dep.eng == eng and eng == "pe":
                return
            k, v = self._skey(dep)
            if k not in cand or cand[k][0] < v:
                cand[k] = (v, dep)

        for d in self.barrier_deps:
            add(d)
        for k in r:
            add(self.last_w.get(k))
        for k in w:
            add(self.last_w.get(k))
            for rd in self.readers.get(k, ()):
                add(rd)
        seen = self.seen[eng]
        for k, (v, dep) in cand.items():
            if seen.get(k, -1) >= v:
                continue
            seen[k] = v
            o.deps.append(dep)
            if not dep.is_dma:
                dep.signal = True
        for k in r:
            self.readers.setdefault(k, []).append(o)
        for k in w:
            self.last_w[k] = o
            self.readers[k] = []
        lst.append(o)
        return o

    def barrier(self):
        deps = [lst[-1] for lst in self.ops.values() if lst]
        deps = [d for d in deps if not d.is_dma]
        self.barrier_deps = deps + list(self.dma_ops)
        self.dma_ops = []

    def emit(self, final_ops):
        nc = self.nc
        with ExitStack() as es:
            esem = {e: es.enter_context(nc.semaphore("s_" + e)) for e in ENGS}
            dsem = {k: es.enter_context(nc.semaphore("d%d" % v[0])) for k, v in self.dma_sems.items()}
            for o in final_ops:
                if not o.is_dma:
                    o.signal = True
            for e in ENGS:
                c = 0
                for o in self.ops[e]:
                    if o.signal and not o.is_dma:
                        c += 1
                        o.sval = c
            block = es.enter_context(nc.Block())
            reg = {"pe": block.tensor, "dve": block.vector, "act": block.scalar,
                   "pool": block.gpsimd, "sp": block.sync}

            def make(e):
                ops = self.ops[e]

                def body(eng):
                    def wait(d):
                        if d.is_dma:
                            eng.wait_ge(dsem[d.dsem], d.dval)
                        else:
                            eng.wait_ge(esem[d.eng], d.sval)
                    for o in ops:
                        for d in o.deps:
                            wait(d)
                        meth, args, kw = o.fn
                        ins = getattr(eng, meth)(*args, **kw)
                        if o.is_dma:
                            ins.then_inc(dsem[o.dsem], 16)
                        elif o.signal:
                            ins.then_inc(esem[e], 1)
                    if e == "sp":
                        for d in final_ops:
                            wait(d)
                return body

            for e in ENGS:
                reg[e](make(e))


class Arena:
    def __init__(self, tile, size):
        self.t = tile
        self.size = size
        self.off = 0

    def alloc(self, shape, dt, nbytes_el):
        n = 1
        for s in shape:
            n *= s
        nb = n * nbytes_el
        nb_al = (nb + 63) // 64 * 64
        assert self.off + nb_al <= self.size, ("arena overflow", self.off, nb_al, self.size)
        ap = self.t[:, self.off:self.off + nb]
        self.off += nb_al
        if dt is not U8:
            ap = ap.bitcast(dt)
        if len(shape) == 2:
            ap = ap.rearrange("p (a b) -> p a b", b=shape[1])
        elif len(shape) == 3:
            ap = ap.rearrange("p (a b c) -> p a b c", b=shape[1], c=shape[2])
        return ap


def rope_consts():
    pos = np.arange(S, dtype=np.float32)
    inv = (1.0 / (np.float32(10000.0) ** (np.arange(0, 64, 2, dtype=np.float32) / np.float32(64)))).astype(np.float32)
    ang = (pos[:, None] * inv[None, :]).astype(np.float32)
    cos = np.cos(ang).astype(np.float32).reshape(NT, 128, 32).transpose(1, 0, 2)
    sin = np.sin(ang).astype(np.float32).reshape(NT, 128, 32).transpose(1, 0, 2)
    return np.ascontiguousarray(cos), np.ascontiguousarray(sin)


def host_consts():
    cos, sin = rope_consts()
    k = np.arange(128)[:, None]
    q = np.arange(128)[None, :]
    tri = np.where(k <= q, 0.0, NEG).astype(np.float32)
    kaug = np.zeros((64, S), np.float32)
    for n in range(8):
        kaug[n, n * 256:(n + 1) * 256] = 1.0
    past = np.zeros((NT, 8), np.float32)
    own = np.full((NT, 8), -1e30, np.float32)
    for t in range(NT):
        qb = t // 2
        for n in range(8):
            if n >= qb:
                past[t, n] = -1e30
            if n == qb or (qb <= 3 and n <= qb):
                own[t, n] = 0.0
    past = np.ascontiguousarray(np.broadcast_to(past[None], (128, NT, 8))).astype(np.float32)
    own = np.ascontiguousarray(np.broadcast_to(own[None], (128, NT, 8))).astype(np.float32)
    ident = np.eye(128, dtype=np.float32)
    iota = np.ascontiguousarray(np.broadcast_to(np.arange(128, dtype=np.float32)[None], (128, 128)))
    return {"c_iota": iota, "c_cos": cos, "c_sin": sin, "c_tri": tri, "c_kaug": kaug, "c_past": past,
            "c_own": own, "c_ident": ident}


def build(B_loc, do_peer=True, do_attn=True, n_groups=None):
    nc = bass.Bass("TRN2", target_bir_lowering=False)

    def din(name, shape, dt=F32):
        return nc.dram_tensor(name, list(shape), dt, kind="ExternalInput").ap()

    x = din("x", [B_loc, S, D])
    attn_norm = din("attn_norm", [1, D])
    w_in = din("w_in", [D, 3072])
    qnm = din("q_norm_moba", [1, 64])
    knm = din("k_norm_moba", [1, 64])
    qnd = din("q_norm_diff", [2, 64])
    knd = din("k_norm_diff", [2, 64])
    lq1 = din("lambda_q1", [1, 64])
    lk1 = din("lambda_k1", [1, 64])
    lq2 = din("lambda_q2", [1, 64])
    lk2 = din("lambda_k2", [1, 64])
    mog = din("moba_out_gain", [1, 512])
    dog = din("diff_out_gain", [1, 512])
    w_out = din("w_out", [D, D])
    c_cos = din("c_cos", [128, NT, 32])
    c_sin = din("c_sin", [128, NT, 32])
    c_tri = din("c_tri", [128, 128])
    c_kaug = din("c_kaug", [64, S])
    c_past = din("c_past", [128, NT, 8])
    c_own = din("c_own", [128, NT, 8])
    c_ident = din("c_ident", [128, 128])
    ffn_norm = din("ffn_norm", [1, D])
    peer_query = din("peer_query", [D, 2048])
    peer_sub_keys = din("peer_sub_keys", [2048, 128])
    peer_down = din("peer_down", [16384, D])
    peer_up = din("peer_up", [16384, D])
    c_iota = din("c_iota", [128, 128])
    downT_s = nc.dram_tensor("downT_s", [128, 128, 8, 128], BF16).ap()
    up_s = nc.dram_tensor("up_s", [128, 128, D], BF16).ap()
    out = nc.dram_tensor("out", [B_loc, S, D], F32, kind="ExternalOutput").ap()

    P = Prog(nc)
    with ExitStack() as es:
        ARENA_BYTES = 212000
        arena_t = es.enter_context(nc.sbuf_tensor("arena", [128, ARENA_BYTES], U8))
        A = Arena(arena_t, ARENA_BYTES)

        def sb(shape, dt=F32):
            return A.alloc(shape, dt, 2 if dt is BF16 else 4)

        def ps(name, shape, dt=F32):
            return es.enter_context(nc.psum_tensor(name, shape, dt))

        S0 = ps("S0", [128, 512])
        S1 = ps("S1", [128, 512])
        acc = [ps("acc0", [128, 4, 256]), ps("acc1", [128, 4, 256])]
        pj = ps("pj", [128, 512])
        ptr = ps("ptr", [128, 1024], BF16)
        Sb = [S0, S1]

        ident_f = sb([128]); ident_b = sb([128], BF16)
        epst = sb([1])
        mark_shared = A.off
        tri_b = sb([128], BF16)
        cos_t = sb([NT, 32]); sin_t = sb([NT, 32])
        past_t = sb([NT, 8]); own_t = sb([NT, 8])
        g1 = sb([D])
        gq_m = sb([256]); gq_d = sb([256])
        og_m = sb([512]); og_d = sb([512])
        lam4 = sb([4, 64]); lamt = sb([2, 64]); lams = sb([2]); lame = sb([2]); neglam = sb([1])
        wout = sb([8, D], BF16)

        def I(eng, meth, *args, r=(), w=(), dma=None, **kw):
            return P.op(eng, (meth, args, kw), r=r, w=w, dma=dma)

        def dma(eng, out_ap, in_ap, key, r=(), w=()):
            return I(eng, "dma_start", out=out_ap, in_=in_ap, r=r, w=w, dma=key)

        dma("sp", ident_f, c_ident, "ident_f", w=["ident_f"])
        dma("pool", ident_b, c_ident, "ident_b", w=["ident_b"])
        dma("pool", tri_b, c_tri, "tri_b", w=["tri_b"])
        dma("sp", cos_t, c_cos, "cos_t", w=["cos_t"])
        dma("sp", sin_t, c_sin, "sin_t", w=["sin_t"])
        dma("sp", past_t, c_past, "past_t", w=["past_t"])
        dma("sp", own_t, c_own, "own_t", w=["own_t"])
        dma("sp", g1, attn_norm[0:1, :].to_broadcast([128, D]), "g1", w=["g1"])
        for i in range(2):
            dma("sp", gq_m[:, i * 64:(i + 1) * 64], qnm[0:1, :].to_broadcast([128, 64]), "gq_m", w=["gq_m"])
            dma("sp", gq_m[:, 128 + i * 64:128 + (i + 1) * 64], knm[0:1, :].to_broadcast([128, 64]), "gq_m", w=["gq_m"])
            dma("sp", gq_d[:, i * 64:(i + 1) * 64], qnd[i:i + 1, :].to_broadcast([128, 64]), "gq_d", w=["gq_d"])
            dma("sp", gq_d[:, 128 + i * 64:128 + (i + 1) * 64], knd[i:i + 1, :].to_broadcast([128, 64]), "gq_d", w=["gq_d"])
        dma("sp", og_m, mog[0:1, :].to_broadcast([128, 512]), "og_m", w=["og_m"])
        dma("sp", og_d, dog[0:1, :].to_broadcast([128, 512]), "og_d", w=["og_d"])
        for i, v in enumerate((lq1, lk1, lq2, lk2)):
            dma("sp", lam4[:, i, :], v[0:1, :].to_broadcast([128, 64]), "lam4", w=["lam4"])
        dma("pool", wout, w_out.rearrange("(c p) n -> p c n", p=128), "wout", w=["wout"])
        I("pool", "memset", epst, EPS, w=["epst"])
        I("dve", "tensor_tensor", out=lamt, in0=lam4[:, 0:4:2, :], in1=lam4[:, 1:4:2, :], op=ALU.mult,
             r=["lam4"], w=["lamt"])
        I("dve", "tensor_reduce", out=lams, in_=lamt, axis=AX.X, op=ALU.add, r=["lamt"], w=["lams"])
        I("act", "activation", out=lame, in_=lams, func=AF.Exp, r=["lams"], w=["lame"])
        I("dve", "tensor_tensor", out=neglam, in0=lame[:, 1:2], in1=lame[:, 0:1], op=ALU.subtract,
             r=["lame"], w=["neglam"])
        I("dve", "tensor_scalar_add", neglam, neglam, -0.2, r=["neglam"], w=["neglam"])
        I("dve", "tensor_scalar_mul", og_d, og_d, 0.8, r=["og_d"], w=["og_d"])

        mark_A = A.off
        hT = sb([8, S], BF16)
        mixed = sb([NT, D], BF16)
        QKs = [sb([4, S], BF16), sb([4, S], BF16)]
        Vbs = [sb([NT, 130], BF16), sb([NT, 130], BF16)]
        wg = [sb([8, 384], BF16), sb([8, 384], BF16)]
        xt = sb([D]); hb = sb([D], BF16); sqj = sb([D], BF16)
        ss = sb([1]); lnv = sb([1]); rstd = sb([1])
        sqt = sb([256]); ssq4 = sb([4]); ln4 = sb([4]); r4 = sb([4])
        t1 = sb([256]); t2 = sb([256]); m1 = sb([256]); m2 = sb([256]); st = sb([256], BF16)
        km = sb([2, 8]); kmb = sb([2, 8], BF16)
        gm = sb([NT, 2, 8]); m8 = sb([NT, 2, 8]); sel = sb([NT, 2, 8])
        bst = sb([NT, 2, 72], BF16)
        PT = [sb([512], BF16) for _ in range(3)]
        rl = [sb([4, 1]), sb([4, 1])]
        of = sb([4, 128]); o2 = sb([4, 128]); tt = sb([4, 128])
        ms4 = sb([4]); ln4b = sb([4]); rr4 = sb([4])
        mT = sb([8, 128], BF16)
        xr = sb([D]); x1 = sb([D])

        for bi_ in range(2):
            dma("pool", QKs[bi_][64:128, 2, :], c_kaug, "QKaug", w=["QK%d" % bi_])
            dma("pool", QKs[bi_][64:128, 3, :], c_kaug, "QKaug", w=["QK%d" % bi_])
            I("pool", "memset", Vbs[bi_][:, :, 128:130], 1.0, w=["Vb%d" % bi_])
        I("pool", "memset", bst, 0.0, w=["bst"])
        I("pool", "memset", m8, 0.0, w=["m8"])

        ptr_h = ptr[:, :].rearrange("p (c t) -> p c t", t=128)

        def group_cols(g):
            if g < 4:
                return (128 * g, 512 + 128 * g, 1024 + 128 * g)
            h = g - 4
            return (1536 + 128 * h, 2048 + 128 * h, 2560 + 128 * h)

        def load_wg(g):
            buf = wg[g % 2]
            key = "wg%d" % (g % 2)
            for i, c0 in enumerate(group_cols(g)):
                dma("pool", buf[:, :, i * 128:(i + 1) * 128],
                    w_in[:, c0:c0 + 128].rearrange("(c p) n -> p c n", p=128), key, w=[key])

        final_ops = []
        pt_i = [0]
        s_i = [0]

        if not do_attn:
            for b in range(B_loc):
                for t in range(NT):
                    rows = slice(t * 128, (t + 1) * 128)
                    dma("sp", xt, x[b, rows, :], "xt", w=["xt"])
                    o = dma("sp", out[b, rows, :], xt, "xto", r=["xt"], w=[("out", b, t)])
                    final_ops.append(o)
        for b in range(B_loc if do_attn else 0):
            for t in range(NT):
                rows = slice(t * 128, (t + 1) * 128)
                dma("sp", xt, x[b, rows, :], "xt", w=["xt"])
                I("act", "activation", out=sqj, in_=xt, func=AF.Square, accum_out=ss,
                     r=["xt"], w=["sqj", "ss"])
                I("act", "activation", out=lnv, in_=ss, func=AF.Ln, scale=1.0 / D, bias=epst,
                     r=["ss", "epst"], w=["lnv"])
                I("act", "activation", out=rstd, in_=lnv, func=AF.Exp, scale=-0.5, r=["lnv"], w=["rstd"])
                I("dve", "scalar_tensor_tensor", out=hb, in0=xt, scalar=rstd, in1=g1,
                                                             op0=ALU.mult, op1=ALU.mult,
                     r=["xt", "rstd", "g1"], w=["hb"])
                for c in range(8):
                    I("pe", "transpose", ptr_h[:, c, :], hb[:, c * 128:(c + 1) * 128], ident_b,
                         r=["hb", "ident_b"], w=["ptr"])
                I("dve", "tensor_copy", hT[:, :, rows], ptr_h, r=["ptr"], w=["hT"])

            def grp_ctx(g):
                moba = g < 4
                return (moba, wg[g % 2], 'wg%d' % (g % 2), gq_m if moba else gq_d, 'gq_m' if moba else 'gq_d',
                        QKs[g % 2], 'QK%d' % (g % 2), Vbs[g % 2], 'Vb%d' % (g % 2))

            def inproj_tile(g, t):
                moba, wgb, wkey, gq, gqk, QK, qkk, Vb, vbk = grp_ctx(g)
                rows = slice(t * 128, (t + 1) * 128)
                for c in range(8):
                    I("pe", "matmul",
                        pj[:, 0:384], lhsT=hT[:, c, rows], rhs=wgb[:, c, :], start=(c == 0), stop=(c == 7),
                        r=["hT", wkey], w=["pj"])
                I("act", "activation", out=sqt, in_=pj[:, 0:256], func=AF.Square, r=["pj"], w=["sqt"])
                I("act", "copy", Vb[:, t, 0:128], pj[:, 256:384], r=["pj"], w=[vbk])
                I("dve", "tensor_reduce", out=ssq4, in_=sqt.rearrange("p (a d) -> p a d", d=64),
                                                      axis=AX.X, op=ALU.add, r=["sqt"], w=["ssq4"])
                I("act", "activation", out=ln4, in_=ssq4, func=AF.Ln, scale=1.0 / 64, bias=epst,
                     r=["ssq4", "epst"], w=["ln4"])
                I("act", "activation", out=r4, in_=ln4, func=AF.Exp, scale=-0.5, r=["ln4"], w=["r4"])
                I("dve", "tensor_tensor",
                    out=t1.rearrange("p (a d) -> p a d", d=64), in0=pj[:, 0:256].rearrange("p (a d) -> p a d", d=64),
                    in1=r4.unsqueeze(2).to_broadcast([128, 4, 64]), op=ALU.mult, r=["pj", "r4"], w=["t1"])
                I("pool", "tensor_tensor", out=t2, in0=t1, in1=gq, op=ALU.mult,
                     r=["t1", gqk], w=["t2"])
                t2v = t2.rearrange("p (a h d) -> p a h d", h=2, d=32)
                m1v = m1.rearrange("p (a h d) -> p a h d", h=2, d=32)
                m2v = m2.rearrange("p (a h d) -> p a h d", h=2, d=32)
                stv = st.rearrange("p (a h d) -> p a h d", h=2, d=32)
                cosb = cos_t[:, t, :].unsqueeze(1).to_broadcast([128, 8, 32])
                sinb = sin_t[:, t, :].unsqueeze(1).to_broadcast([128, 4, 32])
                I("pool", "tensor_tensor",
                    out=m1.rearrange("p (a d) -> p a d", d=32), in0=t2.rearrange("p (a d) -> p a d", d=32),
                    in1=cosb, op=ALU.mult, r=["t2", "cos_t"], w=["m1"])
                I("dve", "tensor_tensor",
                    out=m2v[:, :, 0, :], in0=t2v[:, :, 1, :], in1=sinb, op=ALU.mult, r=["t2", "sin_t"], w=["m2a"])
                I("pool", "tensor_tensor",
                    out=m2v[:, :, 1, :], in0=t2v[:, :, 0, :], in1=sinb, op=ALU.mult, r=["t2", "sin_t"], w=["m2b"])
                I("dve", "tensor_tensor",
                    out=stv[:, :, 0, :], in0=m1v[:, :, 0, :], in1=m2v[:, :, 0, :], op=ALU.subtract,
                    r=["m1", "m2a"], w=["sta"])
                I("pool", "tensor_tensor",
                    out=stv[:, :, 1, :], in0=m1v[:, :, 1, :], in1=m2v[:, :, 1, :], op=ALU.add,
                    r=["m1", "m2b"], w=["stb"])

            def inproj_tr(g, t):
                moba, wgb, wkey, gq, gqk, QK, qkk, Vb, vbk = grp_ctx(g)
                rows = slice(t * 128, (t + 1) * 128)
                for i in range(4):
                    I("pe", "transpose", ptr_h[0:64, i, :], st[:, i * 64:(i + 1) * 64], ident_b,
                         r=["sta", "stb", "ident_b"], w=["ptr"])
                I("dve", "tensor_copy", QK[0:64, :, rows], ptr_h[0:64, 0:4, :],
                     r=["ptr"], w=[qkk])


            def gating(g):
                moba, wgb, wkey, gq, gqk, QK, qkk, Vb, vbk = grp_ctx(g)
                if not moba:
                    return
                I("dve", "tensor_reduce",
                    out=km[0:64], in_=QK[0:64, 2:4, :].rearrange("p m (n k) -> p m n k", k=256),
                    axis=AX.X, op=ALU.add, r=[qkk], w=["km"])
                I("dve", "tensor_copy", kmb[0:64], km[0:64], r=["km"], w=["kmb"])
                pjg = pj[:, 0:256].rearrange("p (t m n) -> p t m n", m=2, n=8)
                for t in range(NT):
                    for m in range(2):
                        I("pe", "matmul",
                            pjg[:, t, m, :], lhsT=QK[0:64, m, t * 128:(t + 1) * 128], rhs=kmb[0:64, m, :],
                            start=True, stop=True, r=[qkk, "kmb"], w=["pj"])
                I("dve", "tensor_tensor",
                    out=gm, in0=pjg, in1=past_t.unsqueeze(2).to_broadcast([128, NT, 2, 8]), op=ALU.add,
                    r=["pj", "past_t"], w=["gm"])
                for t in range(8, NT):
                    for m in range(2):
                        I("dve", "max", out=m8[:, t, m, :], in_=gm[:, t, m, :],
                             r=["gm"], w=["m8"])
                I("dve", "tensor_tensor",
                    out=sel, in0=gm, in1=m8[:, :, :, 2:3].to_broadcast([128, NT, 2, 8]), op=ALU.is_ge,
                    r=["gm", "m8"], w=["sel"])
                I("dve", "tensor_scalar", sel, sel, -NEG, NEG, ALU.mult, ALU.add, r=["sel"], w=["sel"])
                I("dve", "tensor_tensor",
                    out=bst[:, :, :, 64:72], in0=sel, in1=own_t.unsqueeze(2).to_broadcast([128, NT, 2, 8]),
                    op=ALU.max, r=["sel", "own_t"], w=["bst"])
                ptr_a = ptr[:, :].rearrange("p (t m q) -> p t m q", m=2, q=128)
                for t0 in range(0, NT, 4):
                    for tl in range(4):
                        for m in range(2):
                            I("pe", "transpose",
                                ptr_a[0:72, tl, m, :], bst[:, t0 + tl, m, :], ident_b,
                                r=["bst", "ident_b"], w=["ptr"])
                    I("dve", "tensor_copy",
                        QK[64:72, 0:2, t0 * 128:(t0 + 4) * 128].rearrange("p m (t q) -> p t m q", q=128),
                        ptr_a[64:72], r=["ptr"], w=[qkk])


            def attention(g, hooks):
                moba, wgb, wkey, gq, gqk, QK, qkk, Vb, vbk = grp_ctx(g)
                nsc = 0
                K = 72 if moba else 64
                for c in range(4):
                    for m in range(2):
                        ac = acc[m]
                        akey = "acc%d" % m
                        def emit_score(j, c=c, m=m):
                            qlo = max(128 * j, 512 * c)
                            N = 512 * (c + 1) - qlo
                            diag = 128 * j >= 512 * c
                            Sx = Sb[s_i[0] % 2]
                            skey = "S%d" % (s_i[0] % 2)
                            s_i[0] += 1
                            I("pe", "matmul",
                                Sx[:, 0:N], lhsT=QK[0:K, 2 + m, j * 128:(j + 1) * 128], rhs=QK[0:K, m, qlo:qlo + N],
                                start=True, stop=(not diag), r=[qkk], w=[skey])
                            if diag:
                                I("pe", "matmul",
                                    Sx[:, 0:128], lhsT=ident_b, rhs=tri_b, start=False, stop=True,
                                    r=["ident_b", "tri_b"], w=[skey])
                            pti = pt_i[0] % 3
                            pt_i[0] += 1
                            PTx = PT[pti]
                            pkey = "PT%d" % pti
                            I("act", "activation",
                                out=PTx[:, 0:N], in_=Sx[:, 0:N], func=AF.Exp, scale=0.125, r=[skey], w=[pkey])
                            return PTx, pkey, qlo

                        nj = 4 * c + 4
                        cur = emit_score(0)
                        for j in range(nj):
                            nxt = emit_score(j + 1) if j + 1 < nj else None
                            PTx, pkey, qlo = cur
                            nsc += 1
                            hook_now = hooks.get(nsc, ())
                            for i in range(max(j, 4 * c), 4 * c + 4):
                                li = i - 4 * c
                                off = 128 * i - qlo
                                I("pe", "matmul",
                                    ac[:, li, 0:129], lhsT=PTx[:, off:off + 128], rhs=Vb[:, j, 0:129],
                                    start=(j == 0 and li % 2 == 0), stop=(j == i), skip_group_check=True,
                                    r=[pkey, vbk], w=[akey])
                            for fn in hook_now:
                                fn()
                            cur = nxt
                        rlm = rl[m]
                        I("dve", "reciprocal", rlm, ac[:, :, 128:129],
                             r=[akey], w=["rl%d" % m])
                        if moba:
                            h = 2 * g + m
                            I("dve", "tensor_tensor",
                                out=of[:, :, 0:64], in0=ac[:, :, m * 64:(m + 1) * 64],
                                in1=rlm.to_broadcast([128, 4, 64]), op=ALU.mult, r=[akey, "rl%d" % m], w=["of"])
                            W_ = 64
                            gsl = og_m[:, h * 64:(h + 1) * 64]
                            gk = "og_m"
                            col0 = h * 64
                        elif m == 0:
                            I("dve", "tensor_tensor",
                                out=of, in0=ac[:, :, 0:128], in1=rlm.to_broadcast([128, 4, 128]), op=ALU.mult,
                                r=[akey, "rl0"], w=["of"])
                            continue
                        else:
                            h = g - 4
                            I("dve", "tensor_tensor",
                                out=tt, in0=ac[:, :, 0:128], in1=rlm.to_broadcast([128, 4, 128]), op=ALU.mult,
                                r=[akey, "rl1"], w=["tt"])
                            I("dve", "scalar_tensor_tensor",
                                out=of, in0=tt, scalar=neglam, in1=of, op0=ALU.mult, op1=ALU.add,
                                r=["tt", "neglam", "of"], w=["of"])
                            W_ = 128
                            gsl = og_d[:, h * 128:(h + 1) * 128]
                            gk = "og_d"
                            col0 = 512 + h * 128
                        I("pool", "tensor_tensor",
                            out=o2[:, :, 0:W_], in0=of[:, :, 0:W_], in1=of[:, :, 0:W_], op=ALU.mult, r=["of"], w=["o2"])
                        I("dve", "tensor_reduce", out=ms4, in_=o2[:, :, 0:W_], axis=AX.X, op=ALU.add,
                             r=["o2"], w=["ms4"])
                        I("act", "activation", out=ln4b, in_=ms4, func=AF.Ln, scale=1.0 / W_, bias=epst,
                             r=["ms4", "epst"], w=["ln4b"])
                        I("act", "activation", out=rr4, in_=ln4b, func=AF.Exp, scale=-0.5,
                             r=["ln4b"], w=["rr4"])
                        I("dve", "tensor_tensor",
                            out=o2[:, :, 0:W_], in0=of[:, :, 0:W_], in1=rr4.unsqueeze(2).to_broadcast([128, 4, W_]),
                            op=ALU.mult, r=["of", "rr4", "o2"], w=["o2"])
                        I("pool", "tensor_tensor",
                            out=mixed[:, 4 * c:4 * c + 4, col0:col0 + W_], in0=o2[:, :, 0:W_],
                            in1=gsl.unsqueeze(1).to_broadcast([128, 4, W_]), op=ALU.mult,
                            r=["o2", gk], w=["mixed"])


            load_wg(0)
            load_wg(1)
            for t in range(NT):
                inproj_tile(0, t)
                inproj_tr(0, t)
            gating(0)
            for g in range(8):
                hooks = {}
                if g + 1 < 8:
                    for t in range(NT):
                        hk = []
                        if t > 0:
                            hk.append(lambda g=g, t=t: inproj_tr(g + 1, t - 1))
                        hk.append(lambda g=g, t=t: inproj_tile(g + 1, t))
                        hooks[2 + 4 * t] = hk
                    hooks[66] = [lambda g=g: inproj_tr(g + 1, NT - 1)]
                    hooks[70] = [lambda g=g: gating(g + 1)]
                    if g + 2 < 8:
                        hooks[1] = [lambda g=g: load_wg(g + 2)]
                attention(g, hooks)

            for t in range(NT):
                rows = slice(t * 128, (t + 1) * 128)
                dma("sp", xr, x[b, rows, :], "xr", w=["xr"])
                for c in range(8):
                    I("pe", "transpose", ptr_h[:, c, :], mixed[:, t, c * 128:(c + 1) * 128], ident_b,
                         r=["mixed", "ident_b"], w=["ptr"])
                I("act", "copy", mT, ptr_h, r=["ptr"], w=["mT"])
                for hf in range(2):
                    for c in range(8):
                        I("pe", "matmul",
                            Sb[hf][:, 0:512], lhsT=mT[:, c, :], rhs=wout[:, c, hf * 512:(hf + 1) * 512],
                            start=(c == 0), stop=(c == 7), r=["mT", "wout"], w=["S%d" % hf])
                    I("dve", "tensor_tensor",
                        out=x1[:, hf * 512:(hf + 1) * 512], in0=Sb[hf][:, 0:512], in1=xr[:, hf * 512:(hf + 1) * 512],
                        op=ALU.add, r=["S%d" % hf, "xr"], w=["x1"])
                o = dma("sp", out[b, rows, :], x1, "x1", r=["x1"], w=[("out", b, t)])
                final_ops.append(o)

        if do_peer:
            final_ops = []
            P.barrier()
            A.off = mark_shared
            Wq = sb([8, 2048], BF16); keysT = sb([16, 128], BF16); g2 = sb([D])
            iota_b = sb([128], BF16); iota16 = sb([16])
            h2Ts = [sb([8, 256], BF16), sb([8, 256], BF16)]; qT = sb([16, 256], BF16)
            x1g = sb([2, D]); h2 = sb([D], BF16)
            ss2 = sb([1]); lnv2 = sb([1]); rstd2 = sb([1])
            sc = sb([16, 128]); scr = sb([128]); scr2 = sb([256])
            m16 = sb([16, 16]); i16 = sb([16, 16], U32); i16f = sb([16, 16])
            cand = sb([8, 256]); b16 = sb([8, 16]); p16 = sb([8, 16], U32)
            pa = sb([8, 16], U32); pb = sb([8, 16], U32); paf = sb([8, 16]); pbf = sb([8, 16])
            eb = sb([8, 16]); es = sb([8]); er = sb([8])
            IJG_tms = [sb([3, 128]), sb([3, 128])]; IJG = sb([3, 256])
            oh = sc.rearrange("p a b -> p (a b)").rearrange("p (h x) -> p h x", x=256)
            A01s = [sb([16, 128], BF16) for _ in range(2)]; Ags = [sb([16, 128], BF16) for _ in range(2)]
            B01s = [sb([16, 128], BF16) for _ in range(2)]
            dTb = [sb([8, 128], BF16) for _ in range(3)]
            upb = [sb([D], BF16) for _ in range(3)]
            off_WT = A.off
            dn_b = [sb([D], BF16) for _ in range(2)]
            up_b = [sb([D], BF16) for _ in range(2)]
            dT_sb = [sb([8, 128], BF16) for _ in range(2)]
            kst = sb([16, 128], BF16)
            A.off = off_WT
            WT = sb([128, 256], BF16)

            dma("pool", Wq, peer_query.rearrange("(c p) n -> p c n", p=128), "Wq", w=["Wq"])
            dma("pool", kst, peer_sub_keys.rearrange("(hp n) d -> n hp d", n=128), "kst", w=["kst"])
            dma("sp", g2, ffn_norm[0:1, :].to_broadcast([128, D]), "g2", w=["g2"])
            dma("pool", iota_b, c_iota, "iota_b", w=["iota_b"])
            dma("sp", iota16, c_iota[:, 0:16], "iota16", w=["iota16"])
            for h8 in range(2):
                for k in range(8):
                    I("pe", "transpose", ptr_h[:, k, :], kst[:, h8 * 8 + k, :], ident_b, r=["kst", "ident_b"], w=["ptr"])
                I("dve", "tensor_copy", keysT[:, h8 * 8:(h8 + 1) * 8, :], ptr_h, r=["ptr"], w=["keysT"])
            for i in range(128):
                bi = i % 2
                rows = slice(i * 128, (i + 1) * 128)
                dma("pool", dn_b[bi], peer_down[rows, :], "dn_b%d" % bi, w=["dn_b%d" % bi])
                for c in range(8):
                    I("pe", "transpose", ptr_h[:, c, :], dn_b[bi][:, c * 128:(c + 1) * 128], ident_b,
                      r=["dn_b%d" % bi, "ident_b"], w=["ptr"])
                I("act" if i % 2 else "dve", "copy" if i % 2 else "tensor_copy", dT_sb[bi], ptr_h,
                  r=["ptr"], w=["dT_sb%d" % bi])
                dma("sp", downT_s[i], dT_sb[bi], "dT_sbo%d" % bi, r=["dT_sb%d" % bi], w=[("dTs", i)])
                dma("pool", up_b[bi], peer_up[rows, :], "up_bi%d" % bi, w=["up_b%d" % bi])
                dma("sp", up_s[i], up_b[bi], "up_bo%d" % bi, r=["up_b%d" % bi], w=[("ups", i)])
            P.barrier()

            allWT = [("WT", i) for i in range(128)]
            NG = S // 256
            groups = [(b, gi) for b in range(B_loc) for gi in range(NG)]
            if n_groups is not None:
                groups = groups[:n_groups]
            x1gs = [x1g, sb([2, D])]

            def p_load(n, tt):
                b, gi = groups[n]
                xg = x1gs[n % 2]
                xk = "x1g%d" % (n % 2)
                t = gi * 2 + tt
                rows = slice(t * 128, (t + 1) * 128)
                dma("sp", xg[:, tt, :], out[b, rows, :], xk, r=[("out", b, t)], w=[xk])
                I("act", "activation", out=h2, in_=xg[:, tt, :], func=AF.Square, accum_out=ss2,
                  r=[xk], w=["h2", "ss2"])
                I("act", "activation", out=lnv2, in_=ss2, func=AF.Ln, scale=1.0 / D, bias=epst,
                  r=["ss2", "epst"], w=["lnv2"])
                I("act", "activation", out=rstd2, in_=lnv2, func=AF.Exp, scale=-0.5, r=["lnv2"], w=["rstd2"])
                I("dve", "scalar_tensor_tensor", out=h2, in0=xg[:, tt, :], scalar=rstd2, in1=g2,
                  op0=ALU.mult, op1=ALU.mult, r=[xk, "rstd2", "g2"], w=["h2"])

            def p_loadB(n, tt):
                for c in range(8):
                    I("pe", "transpose", ptr_h[:, c, :], h2[:, c * 128:(c + 1) * 128], ident_b,
                      r=["h2", "ident_b"], w=["ptr"])
                I("dve", "tensor_copy", h2Ts[n % 2][:, :, tt * 128:(tt + 1) * 128], ptr_h, r=["ptr"], w=["h2T%d" % (n % 2)])

            def p_q(n, hp):
                Sx = pj[:, (hp % 2) * 256:(hp % 2) * 256 + 256]
                skey = "pj"
                h2T = h2Ts[n % 2]
                for c in range(8):
                    I("pe", "matmul", Sx, lhsT=Wq[:, c, hp * 128:(hp + 1) * 128], rhs=h2T[:, c, :],
                      start=(c == 0), stop=(c == 7), skip_group_check=True,
                      r=["Wq", "h2T%d" % (n % 2)], w=["pj0", "pj1"])
                if hp % 2:
                    I("act", "copy", qT[:, hp, :], Sx, r=["pj0", "pj1"], w=[("qT", hp)])
                else:
                    I("dve", "tensor_copy", qT[:, hp, :], Sx, r=["pj0", "pj1"], w=[("qT", hp)])

            def p_topk(n, tt):
                tsl = slice(tt * 128, (tt + 1) * 128)
                IJG_tm = IJG_tms[tt]
                ijk = "IJG_tm%d" % tt
                for q4 in range(4):
                    Sx = pj
                    skey = "pj"
                    for k in range(4):
                        hp = q4 * 4 + k
                        I("pe", "matmul", Sx[:, k * 128:(k + 1) * 128], lhsT=qT[:, hp, tsl], rhs=keysT[:, hp, :],
                          start=True, stop=True, skip_group_check=True, r=[("qT", hp), "keysT"], w=["pj0", "pj1"])
                    I("act", "copy", sc[:, q4 * 4:(q4 + 1) * 4, :], Sx[:, :].rearrange("p (a n) -> p a n", n=128),
                      r=["pj0", "pj1"], w=["sc"])
                for hp in range(16):
                    I("dve", "max", out=m16[:, hp, 0:8], in_=sc[:, hp, :], r=["sc"], w=["m16"])
                    I("dve", "max_index", out=i16[:, hp, 0:8], in_max=m16[:, hp, 0:8], in_values=sc[:, hp, :],
                      r=["sc", "m16"], w=["i16"])
                    I("dve", "match_replace", out=scr, in_to_replace=m16[:, hp, 0:8], in_values=sc[:, hp, :],
                      imm_value=-1e30, r=["sc", "m16"], w=["scr"])
                    I("dve", "max", out=m16[:, hp, 8:16], in_=scr, r=["scr"], w=["m16"])
                    I("dve", "max_index", out=i16[:, hp, 8:16], in_max=m16[:, hp, 8:16], in_values=scr,
                      r=["scr", "m16"], w=["i16"])
                m16v = m16.rearrange("p (h two) k -> p h two k", two=2)
                candv = cand.rearrange("p h (a b) -> p h a b", b=16)
                I("dve", "tensor_tensor", out=candv,
                  in0=m16v[:, :, 0, :].unsqueeze(3).to_broadcast([128, 8, 16, 16]),
                  in1=m16v[:, :, 1, :].unsqueeze(2).to_broadcast([128, 8, 16, 16]), op=ALU.add,
                  r=["m16"], w=["cand"])
                for h in range(8):
                    I("dve", "max", out=b16[:, h, 0:8], in_=cand[:, h, :], r=["cand"], w=["b16"])
                    I("dve", "max_index", out=p16[:, h, 0:8], in_max=b16[:, h, 0:8], in_values=cand[:, h, :],
                      r=["cand", "b16"], w=["p16"])
                    I("dve", "match_replace", out=scr2, in_to_replace=b16[:, h, 0:8], in_values=cand[:, h, :],
                      imm_value=-1e30, r=["cand", "b16"], w=["scr2"])
                    I("dve", "max", out=b16[:, h, 8:16], in_=scr2, r=["scr2"], w=["b16"])
                    I("dve", "max_index", out=p16[:, h, 8:16], in_max=b16[:, h, 8:16], in_values=scr2,
                      r=["scr2", "b16"], w=["p16"])
                I("dve", "tensor_tensor", out=eb, in0=b16, in1=b16[:, :, 0:1].to_broadcast([128, 8, 16]),
                  op=ALU.subtract, r=["b16"], w=["eb"])

            def p_topk2(n, tt):
                IJG_tm = IJG_tms[tt]
                ijk = "IJG_tm%d" % tt
                I("act", "activation", out=eb, in_=eb, func=AF.Exp, r=["eb"], w=["eb"])
                I("dve", "tensor_reduce", out=es, in_=eb, axis=AX.X, op=ALU.add, r=["eb"], w=["es"])
                I("dve", "reciprocal", er, es, r=["es"], w=["er"])
                I("dve", "tensor_tensor", out=IJG_tm[:, 2, :].rearrange("p (h k) -> p h k", k=16), in0=eb,
                  in1=er.unsqueeze(2).to_broadcast([128, 8, 16]), op=ALU.mult, r=["eb", "er"], w=[ijk])
                I("dve", "tensor_single_scalar", pa, p16, 4, ALU.logical_shift_right, r=["p16"], w=["pa"])
                I("dve", "tensor_single_scalar", pb, p16, 15, ALU.bitwise_and, r=["p16"], w=["pb"])
                I("dve", "tensor_copy", paf, pa, r=["pa"], w=["paf"])
                I("dve", "tensor_copy", pbf, pb, r=["pb"], w=["pbf"])
                I("dve", "tensor_copy", i16f, i16, r=["i16"], w=["i16f"])
                i16v = i16f.rearrange("p (h two) k -> p h two k", two=2)
                ohv = oh.rearrange("p h (k a) -> p h k a", a=16)
                for pf, pfk, which in ((paf, "paf", 0), (pbf, "pbf", 1)):
                    I("dve", "tensor_tensor", out=ohv, in0=pf.unsqueeze(3).to_broadcast([128, 8, 16, 16]),
                      in1=iota16.unsqueeze(1).unsqueeze(1).to_broadcast([128, 8, 16, 16]), op=ALU.is_equal,
                      r=[pfk, "iota16"], w=["sc"])
                    I("pool", "tensor_tensor", out=ohv, in0=ohv,
                      in1=i16v[:, :, which, :].unsqueeze(2).to_broadcast([128, 8, 16, 16]), op=ALU.mult,
                      r=["sc", "i16f"], w=["sc"])
                    I("dve", "tensor_reduce", out=IJG_tm[:, which, :].rearrange("p (h k) -> p h k", k=16),
                      in_=ohv, axis=AX.X, op=ALU.add, r=["sc"], w=[ijk])

            def p_tr(n, tt):
                tsl = slice(tt * 128, (tt + 1) * 128)
                IJG_tm = IJG_tms[tt]
                for k3 in range(3):
                    I("pe", "transpose", pj[:, k3 * 128:(k3 + 1) * 128], IJG_tm[:, k3, :], ident_f,
                      r=["IJG_tm%d" % tt, "ident_f"], w=["pj0", "pj1"])
                I("dve", "tensor_copy", IJG[:, :, tsl], pj[:, 0:384].rearrange("p (a t) -> p a t", t=128),
                  r=["pj0", "pj1"], w=["IJG"])

            def prologue_sched(n):
                sch = {}
                sch[0] = [lambda: p_load(n, 0)]
                sch[4] = [lambda: p_loadB(n, 0), lambda: p_load(n, 1)]
                sch[8] = [lambda: p_loadB(n, 1)]
                for hp in range(16):
                    sch[10 + hp] = [lambda hp=hp: p_q(n, hp)]
                sch[26] = [lambda: p_topk(n, 0)]
                sch[66] = [lambda: p_topk2(n, 0)]
                sch[84] = [lambda: p_topk(n, 1)]
                sch[122] = [lambda: p_topk2(n, 1)]
                return sch

            def tr_sched(n):
                return {2: [lambda: p_tr(n, 0)], 6: [lambda: p_tr(n, 1)]}

            def run_prologue(n):
                for sch in (prologue_sched(n), tr_sched(n)):
                    for k in sorted(sch):
                        for fn in sch[k]:
                            fn()

            def U_phase(n, sch):
                h2T = h2Ts[n % 2]
                for i in range(128):
                    bi = i % 3
                    dkey = "dTb%d" % bi
                    dma("sp" if i % 2 == 0 else "pool", dTb[bi], downT_s[i], dkey, r=[("dTs", i)], w=[dkey])
                    Sx = Sb[i % 2]
                    skey = "S%d" % (i % 2)
                    for c in range(8):
                        I("pe", "matmul", Sx[:, 0:256], lhsT=dTb[bi][:, c, :], rhs=h2T[:, c, :],
                          start=(c == 0), stop=(c == 7), r=[dkey, "h2T%d" % (n % 2)], w=[skey])
                    I("act", "activation", out=WT[:, i, :], in_=Sx[:, 0:256], func=AF.Gelu, r=[skey], w=[("WT", i)])
                    for fn in sch.get(i, ()):
                        fn()

            def G_phase(n):
                iob = iota_b.unsqueeze(1).to_broadcast([128, 16, 128])

                def build_oh(ci):
                    t0 = ci * 16
                    pb_ = ci % 2
                    I("dve", "tensor_tensor", out=A01s[pb_], in0=iob,
                      in1=IJG[:, 0, t0:t0 + 16].unsqueeze(2).to_broadcast([128, 16, 128]), op=ALU.is_equal,
                      r=["iota_b", "IJG"], w=["A01_%d" % pb_])
                    I("pool", "tensor_tensor", out=Ags[pb_], in0=A01s[pb_],
                      in1=IJG[:, 2, t0:t0 + 16].unsqueeze(2).to_broadcast([128, 16, 128]), op=ALU.mult,
                      r=["A01_%d" % pb_, "IJG"], w=["Ag_%d" % pb_])
                    I("dve", "tensor_tensor", out=B01s[pb_], in0=iob,
                      in1=IJG[:, 1, t0:t0 + 16].unsqueeze(2).to_broadcast([128, 16, 128]), op=ALU.is_equal,
                      r=["iota_b", "IJG"], w=["B01_%d" % pb_])

                def mm_evac(ci):
                    t0 = ci * 16
                    pb_ = ci % 2
                    for half in range(2):
                        akey = "acc%d" % half
                        accv = acc[half][:, :, :].rearrange("p a (b i) -> p (a b) i", i=128)
                        for tl in range(8):
                            tk = half * 8 + tl
                            I("pe", "matmul", accv[:, tl, :], lhsT=B01s[pb_][:, tk, :], rhs=Ags[pb_][:, tk, :],
                              start=True, stop=True, skip_group_check=True,
                              r=["B01_%d" % pb_, "Ag_%d" % pb_], w=[akey])
                        ts8 = slice(t0 + half * 8, t0 + half * 8 + 8)
                        I("dve", "tensor_tensor", out=WT[:, :, ts8], in0=accv.rearrange("p t i -> p i t"),
                          in1=WT[:, :, ts8], op=ALU.mult, r=[akey] + allWT, w=allWT)

                build_oh(0)
                for ci in range(16):
                    if ci + 1 < 16:
                        build_oh(ci + 1)
                    mm_evac(ci)

            def up_phase(n, sch):
                b, gi = groups[n]
                xg = x1gs[n % 2]
                xk = "x1g%d" % (n % 2)
                for i in range(128):
                    bi = i % 3
                    ukey = "upb%d" % bi
                    dma("sp" if i % 2 == 0 else "pool", upb[bi], up_s[i], ukey, r=[("ups", i)], w=[ukey])
                    for tt in range(2):
                        accf = acc[tt][:, :, :].rearrange("p a b -> p (a b)")
                        for hf in range(2):
                            I("pe", "matmul", accf[:, hf * 512:(hf + 1) * 512], lhsT=WT[:, i, tt * 128:(tt + 1) * 128],
                              rhs=upb[bi][:, hf * 512:(hf + 1) * 512], start=(i == 0), stop=(i == 127),
                              r=[("WT", i), ukey], w=["acc%d" % tt])
                    for fn in sch.get(i, ()):
                        fn()
                for tt in range(2):
                    t = gi * 2 + tt
                    rows = slice(t * 128, (t + 1) * 128)
                    accf = acc[tt][:, :, :].rearrange("p a b -> p (a b)")
                    I("dve", "tensor_tensor", out=xg[:, tt, :], in0=accf, in1=xg[:, tt, :], op=ALU.add,
                      r=["acc%d" % tt, xk], w=[xk])
                    o = dma("sp", out[b, rows, :], xg[:, tt, :], "yo%d" % (n % 2), r=[xk], w=[("out", b, t)])
                    final_ops.append(o)

            run_prologue(0)
            for n in range(len(groups)):
                more = n + 1 < len(groups)
                U_phase(n, prologue_sched(n + 1) if more else {})
                G_phase(n)
                up_phase(n, tr_sched(n + 1) if more else {})

        P.emit(final_ops)
    return nc


_NC_CACHE = {}


def kernel(**inputs):
    B = inputs["x"].shape[0]
    B_loc = B // N_CORES
    if B_loc not in _NC_CACHE:
        _NC_CACHE[B_loc] = build(B_loc)
    nc = _NC_CACHE[B_loc]
    consts = host_consts()
    f = lambda a: np.ascontiguousarray(np.asarray(a, dtype=np.float32))
    shared = {
        "attn_norm": f(inputs["attn_norm"]).reshape(1, D),
        "w_in": f(inputs["w_in"]).reshape(D, 3072),
        "q_norm_moba": f(inputs["q_norm_moba"]).reshape(1, 64),
        "k_norm_moba": f(inputs["k_norm_moba"]).reshape(1, 64),
        "q_norm_diff": f(inputs["q_norm_diff"]).reshape(2, 64),
        "k_norm_diff": f(inputs["k_norm_diff"]).reshape(2, 64),
        "lambda_q1": f(inputs["lambda_q1"]).reshape(1, 64),
        "lambda_k1": f(inputs["lambda_k1"]).reshape(1, 64),
        "lambda_q2": f(inputs["lambda_q2"]).reshape(1, 64),
        "lambda_k2": f(inputs["lambda_k2"]).reshape(1, 64),
        "moba_out_gain": f(inputs["moba_out_gain"]).reshape(1, 512),
        "diff_out_gain": f(inputs["diff_out_gain"]).reshape(1, 512),
        "w_out": f(inputs["w_out"]).reshape(D, D),
        "ffn_norm": f(inputs["ffn_norm"]).reshape(1, D),
        "peer_query": f(inputs["peer_query"]).reshape(D, 2048),
        "peer_sub_keys": f(inputs["peer_sub_keys"]).reshape(2048, 128),
        "peer_down": f(inputs["peer_down"]).reshape(16384, D),
        "peer_up": f(inputs["peer_up"]).reshape(16384, D),
    }
    shared.update(consts)
    xs = f(inputs["x"])
    in_maps = []
    for c in range(N_CORES):
        m = dict(shared)
        m["x"] = xs[c * B_loc:(c + 1) * B_loc]
        in_maps.append(m)
    res = run_bass_kernel_spmd(nc, in_maps, core_ids=list(range(N_CORES)))
    return np.concatenate([r["out"] for r in res.results], axis=0)
```

```python
import math
from contextlib import ExitStack

import numpy as np
import concourse.bass as bass
import concourse.mybir as mybir
from concourse.bass_utils import run_bass_kernel_spmd

F32 = mybir.dt.float32
BF16 = mybir.dt.bfloat16
U32 = mybir.dt.uint32
U8 = mybir.dt.uint8
AF = mybir.ActivationFunctionType
ALU = mybir.AluOpType
AX = mybir.AxisListType

ENGS = ["pe", "dve", "act", "pool", "sp"]
N_CORES = 8
D = 1024
S = 2048
NT = S // 128
EPS = 1e-6
NEG = -30000.0


class Op:
    __slots__ = ("eng", "fn", "idx", "deps", "is_dma", "dsem", "dval", "signal", "sval")

    def __init__(self, eng, fn, idx, is_dma):
        self.eng = eng
        self.fn = fn
        self.idx = idx
        self.deps = []
        self.is_dma = is_dma
        self.dsem = None
        self.dval = 0
        self.signal = False
        self.sval = 0


class Prog:
    def __init__(self, nc):
        self.nc = nc
        self.ops = {e: [] for e in ENGS}
        self.last_w = {}
        self.readers = {}
        self.seen = {e: {} for e in ENGS}
        self.dma_sems = {}
        self.barrier_deps = []
        self.dma_ops = []

    def _skey(self, dep):
        if dep.is_dma:
            return ("d", dep.dsem), dep.dval
        return ("e", dep.eng), dep.idx

    def op(self, eng, fn, r=(), w=(), dma=None):
        lst = self.ops[eng]
        o = Op(eng, fn, len(lst), dma is not None)
        if dma is not None:
            ent = self.dma_sems.setdefault(dma, [len(self.dma_sems), 0])
            ent[1] += 16
            o.dsem = dma
            o.dval = ent[1]
            self.dma_ops.append(o)
        cand = {}

        def add(dep):
            if dep is None:
                return
            if (not dep.is_dma) and dep.eng == eng and eng == "pe":
                return
            k, v = self._skey(dep)
            if k not in cand or cand[k][0] < v:
                cand[k] = (v, dep)

        for d in self.barrier_deps:
            add(d)
        for k in r:
            add(self.last_w.get(k))
        for k in w:
            add(self.last_w.get(k))
            for rd in self.readers.get(k, ()):
                add(rd)
        seen = self.seen[eng]
        for k, (v, dep) in cand.items():
            if seen.get(k, -1) >= v:
                continue
            seen[k] = v
            o.deps.append(dep)
            if not dep.is_dma:
                dep.signal = True
        for k in r:
            self.readers.setdefault(k, []).append(o)
        for k in w:
            self.last_w[k] = o
            self.readers[k] = []
        lst.append(o)
        return o

    def barrier(self):
        deps = [lst[-1] for lst in self.ops.values() if lst]
        deps = [d for d in deps if not d.is_dma]
        self.barrier_deps = deps + list(self.dma_ops)
        self.dma_ops = []

    def emit(self, final_ops):
        nc = self.nc
        with ExitStack() as es:
            esem = {e: es.enter_context(nc.semaphore("s_" + e)) for e in ENGS}
            dsem = {k: es.enter_context(nc.semaphore("d%d" % v[0])) for k, v in self.dma_sems.items()}
            for o in final_ops:
                if not o.is_dma:
                    o.signal = True
            for e in ENGS:
                c = 0
                for o in self.ops[e]:
                    if o.signal and not o.is_dma:
                        c += 1
                        o.sval = c
            block = es.enter_context(nc.Block())
            reg = {"pe": block.tensor, "dve": block.vector, "act": block.scalar,
                   "pool": block.gpsimd, "sp": block.sync}

            def make(e):
                ops = self.ops[e]

                def body(eng):
                    def wait(d):
                        if d.is_dma:
                            eng.wait_ge(dsem[d.dsem], d.dval)
                        else:
                            eng.wait_ge(esem[d.eng], d.sval)
                    for o in ops:
                        for d in o.deps:
                            wait(d)
                        meth, args, kw = o.fn
                        ins = getattr(eng, meth)(*args, **kw)
                        if o.is_dma:
                            ins.then_inc(dsem[o.dsem], 16)
                        elif o.signal:
                            ins.then_inc(esem[e], 1)
                    if e == "sp":
                        for d in final_ops:
                            wait(d)
                return body

            for e in ENGS:
                reg[e](make(e))


class Arena:
    def __init__(self, tile, size):
        self.t = tile
        self.size = size
        self.off = 0

    def alloc(self, shape, dt, nbytes_el):
        n = 1
        for s in shape:
            n *= s
        nb = n * nbytes_el
        nb_al = (nb + 63) // 64 * 64
        assert self.off + nb_al <= self.size, ("arena overflow", self.off, nb_al, self.size)
        ap = self.t[:, self.off:self.off + nb]
        self.off += nb_al
        if dt is not U8:
            ap = ap.bitcast(dt)
        if len(shape) == 2:
            ap = ap.rearrange("p (a b) -> p a b", b=shape[1])
        elif len(shape) == 3:
            ap = ap.rearrange("p (a b c) -> p a b c", b=shape[1], c=shape[2])
        return ap


def rope_consts():
    pos = np.arange(S, dtype=np.float32)
    inv = (1.0 / (np.float32(10000.0) ** (np.arange(0, 64, 2, dtype=np.float32) / np.float32(64)))).astype(np.float32)
    ang = (pos[:, None] * inv[None, :]).astype(np.float32)
    cos = np.cos(ang).astype(np.float32).reshape(NT, 128, 32).transpose(1, 0, 2)
    sin = np.sin(ang).astype(np.float32).reshape(NT, 128, 32).transpose(1, 0, 2)
    return np.ascontiguousarray(cos), np.ascontiguousarray(sin)


def host_consts():
    cos, sin = rope_consts()
    k = np.arange(128)[:, None]
    q = np.arange(128)[None, :]
    tri = np.where(k <= q, 0.0, NEG).astype(np.float32)
    kaug = np.zeros((64, S), np.float32)
    for n in range(8):
        kaug[n, n * 256:(n + 1) * 256] = 1.0
    past = np.zeros((NT, 8), np.float32)
    own = np.full((NT, 8), -1e30, np.float32)
    for t in range(NT):
        qb = t // 2
        for n in range(8):
            if n >= qb:
                past[t, n] = -1e30
            if n == qb or (qb <= 3 and n <= qb):
                own[t, n] = 0.0
    past = np.ascontiguousarray(np.broadcast_to(past[None], (128, NT, 8))).astype(np.float32)
    own = np.ascontiguousarray(np.broadcast_to(own[None], (128, NT, 8))).astype(np.float32)
    ident = np.eye(128, dtype=np.float32)
    iota = np.ascontiguousarray(np.broadcast_to(np.arange(128, dtype=np.float32)[None], (128, 128)))
    return {"c_iota": iota, "c_cos": cos, "c_sin": sin, "c_tri": tri, "c_kaug": kaug, "c_past": past,
            "c_own": own, "c_ident": ident}


def build(B_loc, do_peer=True, do_attn=True, n_groups=None):
    nc = bass.Bass("TRN2", target_bir_lowering=False)

    def din(name, shape, dt=F32):
        return nc.dram_tensor(name, list(shape), dt, kind="ExternalInput").ap()

    x = din("x", [B_loc, S, D])
    attn_norm = din("attn_norm", [1, D])
    w_in = din("w_in", [D, 3072])
    qnm = din("q_norm_moba", [1, 64])
    knm = din("k_norm_moba", [1, 64])
    qnd = din("q_norm_diff", [2, 64])
    knd = din("k_norm_diff", [2, 64])
    lq1 = din("lambda_q1", [1, 64])
    lk1 = din("lambda_k1", [1, 64])
    lq2 = din("lambda_q2", [1, 64])
    lk2 = din("lambda_k2", [1, 64])
    mog = din("moba_out_gain", [1, 512])
    dog = din("diff_out_gain", [1, 512])
    w_out = din("w_out", [D, D])
    c_cos = din("c_cos", [128, NT, 32])
    c_sin = din("c_sin", [128, NT, 32])
    c_tri = din("c_tri", [128, 128])
    c_kaug = din("c_kaug", [64, S])
    c_past = din("c_past", [128, NT, 8])
    c_own = din("c_own", [128, NT, 8])
    c_ident = din("c_ident", [128, 128])
    ffn_norm = din("ffn_norm", [1, D])
    peer_query = din("peer_query", [D, 2048])
    peer_sub_keys = din("peer_sub_keys", [2048, 128])
    peer_down = din("peer_down", [16384, D])
    peer_up = din("peer_up", [16384, D])
    c_iota = din("c_iota", [128, 128])
    downT_s = nc.dram_tensor("downT_s", [128, 128, 8, 128], BF16).ap()
    up_s = nc.dram_tensor("up_s", [128, 128, D], BF16).ap()
    out = nc.dram_tensor("out", [B_loc, S, D], F32, kind="ExternalOutput").ap()

    P = Prog(nc)
    with ExitStack() as es:
        ARENA_BYTES = 212000
        arena_t = es.enter_context(nc.sbuf_tensor("arena", [128, ARENA_BYTES], U8))
        A = Arena(arena_t, ARENA_BYTES)

        def sb(shape, dt=F32):
            return A.alloc(shape, dt, 2 if dt is BF16 else 4)

        def ps(name, shape, dt=F32):
            return es.enter_context(nc.psum_tensor(name, shape, dt))

        S0 = ps("S0", [128, 512])
        S1 = ps("S1", [128, 512])
        acc = [ps("acc0", [128, 4, 256]), ps("acc1", [128, 4, 256])]
        pj = ps("pj", [128, 512])
        ptr = ps("ptr", [128, 1024], BF16)
        Sb = [S0, S1]

        ident_f = sb([128]); ident_b = sb([128], BF16)
        epst = sb([1])
        mark_shared = A.off
        tri_b = sb([128], BF16)
        cos_t = sb([NT, 32]); sin_t = sb([NT, 32])
        past_t = sb([NT, 8]); own_t = sb([NT, 8])
        g1 = sb([D])
        gq_m = sb([256]); gq_d = sb([256])
        og_m = sb([512]); og_d = sb([512])
        lam4 = sb([4, 64]); lamt = sb([2, 64]); lams = sb([2]); lame = sb([2]); neglam = sb([1])
        wout = sb([8, D], BF16)

        def I(eng, meth, *args, r=(), w=(), dma=None, **kw):
            return P.op(eng, (meth, args, kw), r=r, w=w, dma=dma)

        def dma(eng, out_ap, in_ap, key, r=(), w=()):
            return I(eng, "dma_start", out=out_ap, in_=in_ap, r=r, w=w, dma=key)

        dma("sp", ident_f, c_ident, "ident_f", w=["ident_f"])
        dma("pool", ident_b, c_ident, "ident_b", w=["ident_b"])
        dma("pool", tri_b, c_tri, "tri_b", w=["tri_b"])
        dma("sp", cos_t, c_cos, "cos_t", w=["cos_t"])
        dma("sp", sin_t, c_sin, "sin_t", w=["sin_t"])
        dma("sp", past_t, c_past, "past_t", w=["past_t"])
        dma("sp", own_t, c_own, "own_t", w=["own_t"])
        dma("sp", g1, attn_norm[0:1, :].to_broadcast([128, D]), "g1", w=["g1"])
        for i in range(2):
            dma("sp", gq_m[:, i * 64:(i + 1) * 64], qnm[0:1, :].to_broadcast([128, 64]), "gq_m", w=["gq_m"])
            dma("sp", gq_m[:, 128 + i * 64:128 + (i + 1) * 64], knm[0:1, :].to_broadcast([128, 64]), "gq_m", w=["gq_m"])
            dma("sp", gq_d[:, i * 64:(i + 1) * 64], qnd[i:i + 1, :].to_broadcast([128, 64]), "gq_d", w=["gq_d"])
            dma("sp", gq_d[:, 128 + i * 64:128 + (i + 1) * 64], knd[i:i + 1, :].to_broadcast([128, 64]), "gq_d", w=["gq_d"])
        dma("sp", og_m, mog[0:1, :].to_broadcast([128, 512]), "og_m", w=["og_m"])
        dma("sp", og_d, dog[0:1, :].to_broadcast([128, 512]), "og_d", w=["og_d"])
        for i, v in enumerate((lq1, lk1, lq2, lk2)):
            dma("sp", lam4[:, i, :], v[0:1, :].to_broadcast([128, 64]), "lam4", w=["lam4"])
        dma("pool", wout, w_out.rearrange("(c p) n -> p c n", p=128), "wout", w=["wout"])
        I("pool", "memset", epst, EPS, w=["epst"])
        I("dve", "tensor_tensor", out=lamt, in0=lam4[:, 0:4:2, :], in1=lam4[:, 1:4:2, :], op=ALU.mult,
             r=["lam4"], w=["lamt"])
        I("dve", "tensor_reduce", out=lams, in_=lamt, axis=AX.X, op=ALU.add, r=["lamt"], w=["lams"])
        I("act", "activation", out=lame, in_=lams, func=AF.Exp, r=["lams"], w=["lame"])
        I("dve", "tensor_tensor", out=neglam, in0=lame[:, 1:2], in1=lame[:, 0:1], op=ALU.subtract,
             r=["lame"], w=["neglam"])
        I("dve", "tensor_scalar_add", neglam, neglam, -0.2, r=["neglam"], w=["neglam"])
        I("dve", "tensor_scalar_mul", og_d, og_d, 0.8, r=["og_d"], w=["og_d"])

        mark_A = A.off
        hT = sb([8, S], BF16)
        mixed = sb([NT, D], BF16)
        QKs = [sb([4, S], BF16), sb([4, S], BF16)]
        Vbs = [sb([NT, 130], BF16), sb([NT, 130], BF16)]
        wg = [sb([8, 384], BF16), sb([8, 384], BF16)]
        xt = sb([D]); hb = sb([D], BF16); sqj = sb([D], BF16)
        ss = sb([1]); lnv = sb([1]); rstd = sb([1])
        sqt = sb([256]); ssq4 = sb([4]); ln4 = sb([4]); r4 = sb([4])
        t1 = sb([256]); t2 = sb([256]); m1 = sb([256]); m2 = sb([256]); st = sb([256], BF16)
        km = sb([2, 8]); kmb = sb([2, 8], BF16)
        gm = sb([NT, 2, 8]); m8 = sb([NT, 2, 8]); sel = sb([NT, 2, 8])
        bst = sb([NT, 2, 72], BF16)
        PT = [sb([512], BF16) for _ in range(3)]
        rl = [sb([4, 1]), sb([4, 1])]
        of = sb([4, 128]); o2 = sb([4, 128]); tt = sb([4, 128])
        ms4 = sb([4]); ln4b = sb([4]); rr4 = sb([4])
        mT = sb([8, 128], BF16)
        xr = sb([D]); x1 = sb([D])

        for bi_ in range(2):
            dma("pool", QKs[bi_][64:128, 2, :], c_kaug, "QKaug", w=["QK%d" % bi_])
            dma("pool", QKs[bi_][64:128, 3, :], c_kaug, "QKaug", w=["QK%d" % bi_])
            I("pool", "memset", Vbs[bi_][:, :, 128:130], 1.0, w=["Vb%d" % bi_])
        I("pool", "memset", bst, 0.0, w=["bst"])
        I("pool", "memset", m8, 0.0, w=["m8"])

        ptr_h = ptr[:, :].rearrange("p (c t) -> p c t", t=128)

        def group_cols(g):
            if g < 4:
                return (128 * g, 512 + 128 * g, 1024 + 128 * g)
            h = g - 4
            return (1536 + 128 * h, 2048 + 128 * h, 2560 + 128 * h)

        def load_wg(g):
            buf = wg[g % 2]
            key = "wg%d" % (g % 2)
            for i, c0 in enumerate(group_cols(g)):
                dma("pool", buf[:, :, i * 128:(i + 1) * 128],
                    w_in[:, c0:c0 + 128].rearrange("(c p) n -> p c n", p=128), key, w=[key])

        final_ops = []
        pt_i = [0]
        s_i = [0]

        if not do_attn:
            for b in range(B_loc):
                for t in range(NT):
                    rows = slice(t * 128, (t + 1) * 128)
                    dma("sp", xt, x[b, rows, :], "xt", w=["xt"])
                    o = dma("sp", out[b, rows, :], xt, "xto", r=["xt"], w=[("out", b, t)])
                    final_ops.append(o)
        for b in range(B_loc if do_attn else 0):
            for t in range(NT):
                rows = slice(t * 128, (t + 1) * 128)
                dma("sp", xt, x[b, rows, :], "xt", w=["xt"])
                I("act", "activation", out=sqj, in_=xt, func=AF.Square, accum_out=ss,
                     r=["xt"], w=["sqj", "ss"])
                I("act", "activation", out=lnv, in_=ss, func=AF.Ln, scale=1.0 / D, bias=epst,
                     r=["ss", "epst"], w=["lnv"])
                I("act", "activation", out=rstd, in_=lnv, func=AF.Exp, scale=-0.5, r=["lnv"], w=["rstd"])
                I("dve", "scalar_tensor_tensor", out=hb, in0=xt, scalar=rstd, in1=g1,
                                                             op0=ALU.mult, op1=ALU.mult,
                     r=["xt", "rstd", "g1"], w=["hb"])
                for c in range(8):
                    I("pe", "transpose", ptr_h[:, c, :], hb[:, c * 128:(c + 1) * 128], ident_b,
                         r=["hb", "ident_b"], w=["ptr"])
                I("dve", "tensor_copy", hT[:, :, rows], ptr_h, r=["ptr"], w=["hT"])

            def grp_ctx(g):
                moba = g < 4
                return (moba, wg[g % 2], 'wg%d' % (g % 2), gq_m if moba else gq_d, 'gq_m' if moba else 'gq_d',
                        QKs[g % 2], 'QK%d' % (g % 2), Vbs[g % 2], 'Vb%d' % (g % 2))

            def inproj_tile(g, t):
                moba, wgb, wkey, gq, gqk, QK, qkk, Vb, vbk = grp_ctx(g)
                rows = slice(t * 128, (t + 1) * 128)
                for c in range(8):
                    I("pe", "matmul",
                        pj[:, 0:384], lhsT=hT[:, c, rows], rhs=wgb[:, c, :], start=(c == 0), stop=(c == 7),
                        r=["hT", wkey], w=["pj"])
                I("act", "activation", out=sqt, in_=pj[:, 0:256], func=AF.Square, r=["pj"], w=["sqt"])
                I("act", "copy", Vb[:, t, 0:128], pj[:, 256:384], r=["pj"], w=[vbk])
                I("dve", "tensor_reduce", out=ssq4, in_=sqt.rearrange("p (a d) -> p a d", d=64),
                                                      axis=AX.X, op=ALU.add, r=["sqt"], w=["ssq4"])
                I("act", "activation", out=ln4, in_=ssq4, func=AF.Ln, scale=1.0 / 64, bias=epst,
                     r=["ssq4", "epst"], w=["ln4"])
                I("act", "activation", out=r4, in_=ln4, func=AF.Exp, scale=-0.5, r=["ln4"], w=["r4"])
                I("dve", "tensor_tensor",
                    out=t1.rearrange("p (a d) -> p a d", d=64), in0=pj[:, 0:256].rearrange("p (a d) -> p a d", d=64),
                    in1=r4.unsqueeze(2).to_broadcast([128, 4, 64]), op=ALU.mult, r=["pj", "r4"], w=["t1"])
                I("pool", "tensor_tensor", out=t2, in0=t1, in1=gq, op=ALU.mult,
                     r=["t1", gqk], w=["t2"])
                t2v = t2.rearrange("p (a h d) -> p a h d", h=2, d=32)
                m1v = m1.rearrange("p (a h d) -> p a h d", h=2, d=32)
                m2v = m2.rearrange("p (a h d) -> p a h d", h=2, d=32)
                stv = st.rearrange("p (a h d) -> p a h d", h=2, d=32)
                cosb = cos_t[:, t, :].unsqueeze(1).to_broadcast([128, 8, 32])
                sinb = sin_t[:, t, :].unsqueeze(1).to_broadcast([128, 4, 32])
                I("pool", "tensor_tensor",
                    out=m1.rearrange("p (a d) -> p a d", d=32), in0=t2.rearrange("p (a d) -> p a d", d=32),
                    in1=cosb, op=ALU.mult, r=["t2", "cos_t"], w=["m1"])
                I("dve", "tensor_tensor",
                    out=m2v[:, :, 0, :], in0=t2v[:, :, 1, :], in1=sinb, op=ALU.mult, r=["t2", "sin_t"], w=["m2a"])
                I("pool", "tensor_tensor",
                    out=m2v[:, :, 1, :], in0=t2v[:, :, 0, :], in1=sinb, op=ALU.mult, r=["t2", "sin_t"], w=["m2b"])
                I("dve", "tensor_tensor",
                    out=stv[:, :, 0, :], in0=m1v[:, :, 0, :], in1=m2v[:, :, 0, :], op=ALU.subtract,
                    r=["m1", "m2a"], w=["sta"])
                I("pool", "tensor_tensor",
                    out=stv[:, :, 1, :], in0=m1v[:, :, 1, :], in1=m2v[:, :, 1, :], op=ALU.add,
                    r=["m1", "m2b"], w=["stb"])

            def inproj_tr(g, t):
                moba, wgb, wkey, gq, gqk, QK, qkk, Vb, vbk = grp_ctx(g)
                rows = slice(t * 128, (t + 1) * 128)
                for i in range(4):
                    I("pe", "transpose", ptr_h[0:64, i, :], st[:, i * 64:(i + 1) * 64], ident_b,
                         r=["sta", "stb", "ident_b"], w=["ptr"])
                I("dve", "tensor_copy", QK[0:64, :, rows], ptr_h[0:64, 0:4, :],
                     r=["ptr"], w=[qkk])


            def gating(g):
                moba, wgb, wkey, gq, gqk, QK, qkk, Vb, vbk = grp_ctx(g)
                if not moba:
                    return
                I("dve", "tensor_reduce",
                    out=km[0:64], in_=QK[0:64, 2:4, :].rearrange("p m (n k) -> p m n k", k=256),
                    axis=AX.X, op=ALU.add, r=[qkk], w=["km"])
                I("dve", "tensor_copy", kmb[0:64], km[0:64], r=["km"], w=["kmb"])
                pjg = pj[:, 0:256].rearrange("p (t m n) -> p t m n", m=2, n=8)
                for t in range(NT):
                    for m in range(2):
                        I("pe", "matmul",
                            pjg[:, t, m, :], lhsT=QK[0:64, m, t * 128:(t + 1) * 128], rhs=kmb[0:64, m, :],
                            start=True, stop=True, r=[qkk, "kmb"], w=["pj"])
                I("dve", "tensor_tensor",
                    out=gm, in0=pjg, in1=past_t.unsqueeze(2).to_broadcast([128, NT, 2, 8]), op=ALU.add,
                    r=["pj", "past_t"], w=["gm"])
                for t in range(8, NT):
                    for m in range(2):
                        I("dve", "max", out=m8[:, t, m, :], in_=gm[:, t, m, :],
                             r=["gm"], w=["m8"])
                I("dve", "tensor_tensor",
                    out=sel, in0=gm, in1=m8[:, :, :, 2:3].to_broadcast([128, NT, 2, 8]), op=ALU.is_ge,
                    r=["gm", "m8"], w=["sel"])
                I("dve", "tensor_scalar", sel, sel, -NEG, NEG, ALU.mult, ALU.add, r=["sel"], w=["sel"])
                I("dve", "tensor_tensor",
                    out=bst[:, :, :, 64:72], in0=sel, in1=own_t.unsqueeze(2).to_broadcast([128, NT, 2, 8]),
                    op=ALU.max, r=["sel", "own_t"], w=["bst"])
                ptr_a = ptr[:, :].rearrange("p (t m q) -> p t m q", m=2, q=128)
                for t0 in range(0, NT, 4):
                    for tl in range(4):
                        for m in range(2):
                            I("pe", "transpose",
                                ptr_a[0:72, tl, m, :], bst[:, t0 + tl, m, :], ident_b,
                                r=["bst", "ident_b"], w=["ptr"])
                    I("dve", "tensor_copy",
                        QK[64:72, 0:2, t0 * 128:(t0 + 4) * 128].rearrange("p m (t q) -> p t m q", q=128),
                        ptr_a[64:72], r=["ptr"], w=[qkk])


            def attention(g, hooks):
                moba, wgb, wkey, gq, gqk, QK, qkk, Vb, vbk = grp_ctx(g)
                nsc = 0
                K = 72 if moba else 64
                for c in range(4):
                    for m in range(2):
                        ac = acc[m]
                        akey = "acc%d" % m
                        def emit_score(j, c=c, m=m):
                            qlo = max(128 * j, 512 * c)
                            N = 512 * (c + 1) - qlo
                            diag = 128 * j >= 512 * c
                            Sx = Sb[s_i[0] % 2]
                            skey = "S%d" % (s_i[0] % 2)
                            s_i[0] += 1
                            I("pe", "matmul",
                                Sx[:, 0:N], lhsT=QK[0:K, 2 + m, j * 128:(j + 1) * 128], rhs=QK[0:K, m, qlo:qlo + N],
                                start=True, stop=(not diag), r=[qkk], w=[skey])
                            if diag:
                                I("pe", "matmul",
                                    Sx[:, 0:128], lhsT=ident_b, rhs=tri_b, start=False, stop=True,
                                    r=["ident_b", "tri_b"], w=[skey])
                            pti = pt_i[0] % 3
                            pt_i[0] += 1
                            PTx = PT[pti]
                            pkey = "PT%d" % pti
                            I("act", "activation",
                                out=PTx[:, 0:N], in_=Sx[:, 0:N], func=AF.Exp, scale=0.125, r=[skey], w=[pkey])
                            return PTx, pkey, qlo

                        nj = 4 * c + 4
                        cur = emit_score(0)
                        for j in range(nj):
                            nxt = emit_score(j + 1) if j + 1 < nj else None
                            PTx, pkey, qlo = cur
                            nsc += 1
                            hook_now = hooks.get(nsc, ())
                            for i in range(max(j, 4 * c), 4 * c + 4):
                                li = i - 4 * c
                                off = 128 * i - qlo
                                I("pe", "matmul",
                                    ac[:, li, 0:129], lhsT=PTx[:, off:off + 128], rhs=Vb[:, j, 0:129],
                                    start=(j == 0 and li % 2 == 0), stop=(j == i), skip_group_check=True,
                                    r=[pkey, vbk], w=[akey])
                            for fn in hook_now:
                                fn()
                            cur = nxt
                        rlm = rl[m]
                        I("dve", "reciprocal", rlm, ac[:, :, 128:129],
                             r=[akey], w=["rl%d" % m])
                        if moba:
                            h = 2 * g + m
                            I("dve", "tensor_tensor",
                                out=of[:, :, 0:64], in0=ac[:, :, m * 64:(m + 1) * 64],
                                in1=rlm.to_broadcast([128, 4, 64]), op=ALU.mult, r=[akey, "rl%d" % m], w=["of"])
                            W_ = 64
                            gsl = og_m[:, h * 64:(h + 1) * 64]
                            gk = "og_m"
                            col0 = h * 64
                        elif m == 0:
                            I("dve", "tensor_tensor",
                                out=of, in0=ac[:, :, 0:128], in1=rlm.to_broadcast([128, 4, 128]), op=ALU.mult,
                                r=[akey, "rl0"], w=["of"])
                            continue
                        else:
                            h = g - 4
                            I("dve", "tensor_tensor",
                                out=tt, in0=ac[:, :, 0:128], in1=rlm.to_broadcast([128, 4, 128]), op=ALU.mult,
                                r=[akey, "rl1"], w=["tt"])
                            I("dve", "scalar_tensor_tensor",
                                out=of, in0=tt, scalar=neglam, in1=of, op0=ALU.mult, op1=ALU.add,
                                r=["tt", "neglam", "of"], w=["of"])
                            W_ = 128
                            gsl = og_d[:, h * 128:(h + 1) * 128]
                            gk = "og_d"
                            col0 = 512 + h * 128
                        I("pool", "tensor_tensor",
                            out=o2[:, :, 0:W_], in0=of[:, :, 0:W_], in1=of[:, :, 0:W_], op=ALU.mult, r=["of"], w=["o2"])
                        I("dve", "tensor_reduce", out=ms4, in_=o2[:, :, 0:W_], axis=AX.X, op=ALU.add,
                             r=["o2"], w=["ms4"])
                        I("act", "activation", out=ln4b, in_=ms4, func=AF.Ln, scale=1.0 / W_, bias=epst,
                             r=["ms4", "epst"], w=["ln4b"])
                        I("act", "activation", out=rr4, in_=ln4b, func=AF.Exp, scale=-0.5,
                             r=["ln4b"], w=["rr4"])
                        I("dve", "tensor_tensor",
                            out=o2[:, :, 0:W_], in0=of[:, :, 0:W_], in1=rr4.unsqueeze(2).to_broadcast([128, 4, W_]),
                            op=ALU.mult, r=["of", "rr4", "o2"], w=["o2"])
                        I("pool", "tensor_tensor",
                            out=mixed[:, 4 * c:4 * c + 4, col0:col0 + W_], in0=o2[:, :, 0:W_],
                            in1=gsl.unsqueeze(1).to_broadcast([128, 4, W_]), op=ALU.mult,
                            r=["o2", gk], w=["mixed"])


            load_wg(0)
            load_wg(1)
            for t in range(NT):
                inproj_tile(0, t)
                inproj_tr(0, t)
            gating(0)
            for g in range(8):
                hooks = {}
                if g + 1 < 8:
                    for t in range(NT):
                        hk = []
                        if t > 0:
                            hk.append(lambda g=g, t=t: inproj_tr(g + 1, t - 1))
                        hk.append(lambda g=g, t=t: inproj_tile(g + 1, t))
                        hooks[2 + 4 * t] = hk
                    hooks[66] = [lambda g=g: inproj_tr(g + 1, NT - 1)]
                    hooks[70] = [lambda g=g: gating(g + 1)]
                    if g + 2 < 8:
                        hooks[1] = [lambda g=g: load_wg(g + 2)]
                attention(g, hooks)

            for t in range(NT):
                rows = slice(t * 128, (t + 1) * 128)
                dma("sp", xr, x[b, rows, :], "xr", w=["xr"])
                for c in range(8):
                    I("pe", "transpose", ptr_h[:, c, :], mixed[:, t, c * 128:(c + 1) * 128], ident_b,
                         r=["mixed", "ident_b"], w=["ptr"])
                I("act", "copy", mT, ptr_h, r=["ptr"], w=["mT"])
                for hf in range(2):
                    for c in range(8):
                        I("pe", "matmul",
                            Sb[hf][:, 0:512], lhsT=mT[:, c, :], rhs=wout[:, c, hf * 512:(hf + 1) * 512],
                            start=(c == 0), stop=(c == 7), r=["mT", "wout"], w=["S%d" % hf])
                    I("dve", "tensor_tensor",
                        out=x1[:, hf * 512:(hf + 1) * 512], in0=Sb[hf][:, 0:512], in1=xr[:, hf * 512:(hf + 1) * 512],
                        op=ALU.add, r=["S%d" % hf, "xr"], w=["x1"])
                o = dma("sp", out[b, rows, :], x1, "x1", r=["x1"], w=[("out", b, t)])
                final_ops.append(o)

        if do_peer:
            final_ops = []
            P.barrier()
            A.off = mark_shared
            Wq = sb([8, 2048], BF16); keysT = sb([16, 128], BF16); g2 = sb([D])
            iota_b = sb([128], BF16); iota16 = sb([16])
            h2Ts = [sb([8, 256], BF16), sb([8, 256], BF16)]; qT = sb([16, 256], BF16)
            x1g = sb([2, D]); h2 = sb([D], BF16)
            ss2 = sb([1]); lnv2 = sb([1]); rstd2 = sb([1])
            sc = sb([16, 128]); scr = sb([128]); scr2 = sb([256])
            m16 = sb([16, 16]); i16 = sb([16, 16], U32); i16f = sb([16, 16])
            cand = sb([8, 256]); b16 = sb([8, 16]); p16 = sb([8, 16], U32)
            pa = sb([8, 16], U32); pb = sb([8, 16], U32); paf = sb([8, 16]); pbf = sb([8, 16])
            eb = sb([8, 16]); es = sb([8]); er = sb([8])
            IJG_tms = [sb([3, 128]), sb([3, 128])]; IJG = sb([3, 256])
            oh = sc.rearrange("p a b -> p (a b)").rearrange("p (h x) -> p h x", x=256)
            A01s = [sb([16, 128], BF16) for _ in range(2)]; Ags = [sb([16, 128], BF16) for _ in range(2)]
            B01s = [sb([16, 128], BF16) for _ in range(2)]
            NWB = 6
            wbuf = [sb([D], BF16) for _ in range(NWB)]
            dTb = [w_.rearrange("p (c e) -> p c e", e=128) for w_ in wbuf]
            upb = wbuf
            off_WT = A.off
            dn_b = [sb([D], BF16) for _ in range(2)]
            up_b = [sb([D], BF16) for _ in range(2)]
            dT_sb = [sb([8, 128], BF16) for _ in range(2)]
            kst = sb([16, 128], BF16)
            A.off = off_WT
            WT = sb([128, 256], BF16)

            dma("pool", Wq, peer_query.rearrange("(c p) n -> p c n", p=128), "Wq", w=["Wq"])
            dma("pool", kst, peer_sub_keys.rearrange("(hp n) d -> n hp d", n=128), "kst", w=["kst"])
            dma("sp", g2, ffn_norm[0:1, :].to_broadcast([128, D]), "g2", w=["g2"])
            dma("pool", iota_b, c_iota, "iota_b", w=["iota_b"])
            dma("sp", iota16, c_iota[:, 0:16], "iota16", w=["iota16"])
            for h8 in range(2):
                for k in range(8):
                    I("pe", "transpose", ptr_h[:, k, :], kst[:, h8 * 8 + k, :], ident_b, r=["kst", "ident_b"], w=["ptr"])
                I("dve", "tensor_copy", keysT[:, h8 * 8:(h8 + 1) * 8, :], ptr_h, r=["ptr"], w=["keysT"])
            for i in range(128):
                bi = i % 2
                rows = slice(i * 128, (i + 1) * 128)
                dma("pool", dn_b[bi], peer_down[rows, :], "dn_b%d" % bi, w=["dn_b%d" % bi])
                for c in range(8):
                    I("pe", "transpose", ptr_h[:, c, :], dn_b[bi][:, c * 128:(c + 1) * 128], ident_b,
                      r=["dn_b%d" % bi, "ident_b"], w=["ptr"])
                I("act" if i % 2 else "dve", "copy" if i % 2 else "tensor_copy", dT_sb[bi], ptr_h,
                  r=["ptr"], w=["dT_sb%d" % bi])
                dma("sp", downT_s[i], dT_sb[bi], "dT_sbo%d" % bi, r=["dT_sb%d" % bi], w=[("dTs", i)])
                dma("pool", up_b[bi], peer_up[rows, :], "up_bi%d" % bi, w=["up_b%d" % bi])
                dma("sp", up_s[i], up_b[bi], "up_bo%d" % bi, r=["up_b%d" % bi], w=[("ups", i)])
            P.barrier()

            allWT = [("WT", i) for i in range(128)]
            NG = S // 256
            groups = [(b, gi) for b in range(B_loc) for gi in range(NG)]
            if n_groups is not None:
                groups = groups[:n_groups]
            x1gs = [x1g, sb([2, D])]

            def p_load(n, tt):
                b, gi = groups[n]
                xg = x1gs[n % 2]
                xk = "x1g%d" % (n % 2)
                t = gi * 2 + tt
                rows = slice(t * 128, (t + 1) * 128)
                dma("sp", xg[:, tt, :], out[b, rows, :], xk, r=[("out", b, t)], w=[xk])
                I("act", "activation", out=h2, in_=xg[:, tt, :], func=AF.Square, accum_out=ss2,
                  r=[xk], w=["h2", "ss2"])
                I("act", "activation", out=lnv2, in_=ss2, func=AF.Ln, scale=1.0 / D, bias=epst,
                  r=["ss2", "epst"], w=["lnv2"])
                I("act", "activation", out=rstd2, in_=lnv2, func=AF.Exp, scale=-0.5, r=["lnv2"], w=["rstd2"])
                I("dve", "scalar_tensor_tensor", out=h2, in0=xg[:, tt, :], scalar=rstd2, in1=g2,
                  op0=ALU.mult, op1=ALU.mult, r=[xk, "rstd2", "g2"], w=["h2"])

            def p_loadB(n, tt):
                for c in range(8):
                    I("pe", "transpose", ptr_h[:, c, :], h2[:, c * 128:(c + 1) * 128], ident_b,
                      r=["h2", "ident_b"], w=["ptr"])
                I("dve", "tensor_copy", h2Ts[n % 2][:, :, tt * 128:(tt + 1) * 128], ptr_h, r=["ptr"], w=["h2T%d" % (n % 2)])

            def p_q(n, hp):
                Sx = pj[:, (hp % 2) * 256:(hp % 2) * 256 + 256]
                skey = "pj"
                h2T = h2Ts[n % 2]
                for c in range(8):
                    I("pe", "matmul", Sx, lhsT=Wq[:, c, hp * 128:(hp + 1) * 128], rhs=h2T[:, c, :],
                      start=(c == 0), stop=(c == 7), skip_group_check=True,
                      r=["Wq", "h2T%d" % (n % 2)], w=["pj0", "pj1"])
                if hp % 2:
                    I("act", "copy", qT[:, hp, :], Sx, r=["pj0", "pj1"], w=[("qT", hp)])
                else:
                    I("dve", "tensor_copy", qT[:, hp, :], Sx, r=["pj0", "pj1"], w=[("qT", hp)])

            def p_topk(n, tt):
                tsl = slice(tt * 128, (tt + 1) * 128)
                IJG_tm = IJG_tms[tt]
                ijk = "IJG_tm%d" % tt
                for q4 in range(4):
                    Sx = pj
                    skey = "pj"
                    for k in range(4):
                        hp = q4 * 4 + k
                        I("pe", "matmul", Sx[:, k * 128:(k + 1) * 128], lhsT=qT[:, hp, tsl], rhs=keysT[:, hp, :],
                          start=True, stop=True, skip_group_check=True, r=[("qT", hp), "keysT"], w=["pj0", "pj1"])
                    I("act", "copy", sc[:, q4 * 4:(q4 + 1) * 4, :], Sx[:, :].rearrange("p (a n) -> p a n", n=128),
                      r=["pj0", "pj1"], w=["sc"])
                for hp in range(16):
                    I("dve", "max", out=m16[:, hp, 0:8], in_=sc[:, hp, :], r=["sc"], w=["m16"])
                    I("dve", "max_index", out=i16[:, hp, 0:8], in_max=m16[:, hp, 0:8], in_values=sc[:, hp, :],
                      r=["sc", "m16"], w=["i16"])
                    I("dve", "match_replace", out=scr, in_to_replace=m16[:, hp, 0:8], in_values=sc[:, hp, :],
                      imm_value=-1e30, r=["sc", "m16"], w=["scr"])
                    I("dve", "max", out=m16[:, hp, 8:16], in_=scr, r=["scr"], w=["m16"])
                    I("dve", "max_index", out=i16[:, hp, 8:16], in_max=m16[:, hp, 8:16], in_values=scr,
                      r=["scr", "m16"], w=["i16"])
                m16v = m16.rearrange("p (h two) k -> p h two k", two=2)
                candv = cand.rearrange("p h (a b) -> p h a b", b=16)
                I("dve", "tensor_tensor", out=candv,
                  in0=m16v[:, :, 0, :].unsqueeze(3).to_broadcast([128, 8, 16, 16]),
                  in1=m16v[:, :, 1, :].unsqueeze(2).to_broadcast([128, 8, 16, 16]), op=ALU.add,
                  r=["m16"], w=["cand"])
                for h in range(8):
                    I("dve", "max", out=b16[:, h, 0:8], in_=cand[:, h, :], r=["cand"], w=["b16"])
                    I("dve", "max_index", out=p16[:, h, 0:8], in_max=b16[:, h, 0:8], in_values=cand[:, h, :],
                      r=["cand", "b16"], w=["p16"])
                    I("dve", "match_replace", out=scr2, in_to_replace=b16[:, h, 0:8], in_values=cand[:, h, :],
                      imm_value=-1e30, r=["cand", "b16"], w=["scr2"])
                    I("dve", "max", out=b16[:, h, 8:16], in_=scr2, r=["scr2"], w=["b16"])
                    I("dve", "max_index", out=p16[:, h, 8:16], in_max=b16[:, h, 8:16], in_values=scr2,
                      r=["scr2", "b16"], w=["p16"])
                I("dve", "tensor_tensor", out=eb, in0=b16, in1=b16[:, :, 0:1].to_broadcast([128, 8, 16]),
                  op=ALU.subtract, r=["b16"], w=["eb"])

            def p_topk2(n, tt):
                IJG_tm = IJG_tms[tt]
                ijk = "IJG_tm%d" % tt
                I("act", "activation", out=eb, in_=eb, func=AF.Exp, r=["eb"], w=["eb"])
                I("dve", "tensor_reduce", out=es, in_=eb, axis=AX.X, op=ALU.add, r=["eb"], w=["es"])
                I("dve", "reciprocal", er, es, r=["es"], w=["er"])
                I("dve", "tensor_tensor", out=IJG_tm[:, 2, :].rearrange("p (h k) -> p h k", k=16), in0=eb,
                  in1=er.unsqueeze(2).to_broadcast([128, 8, 16]), op=ALU.mult, r=["eb", "er"], w=[ijk])
                I("dve", "tensor_single_scalar", pa, p16, 4, ALU.logical_shift_right, r=["p16"], w=["pa"])
                I("dve", "tensor_single_scalar", pb, p16, 15, ALU.bitwise_and, r=["p16"], w=["pb"])
                I("dve", "tensor_copy", paf, pa, r=["pa"], w=["paf"])
                I("dve", "tensor_copy", pbf, pb, r=["pb"], w=["pbf"])
                I("dve", "tensor_copy", i16f, i16, r=["i16"], w=["i16f"])
                i16v = i16f.rearrange("p (h two) k -> p h two k", two=2)
                ohv = oh.rearrange("p h (k a) -> p h k a", a=16)
                for pf, pfk, which in ((paf, "paf", 0), (pbf, "pbf", 1)):
                    I("dve", "tensor_tensor", out=ohv, in0=pf.unsqueeze(3).to_broadcast([128, 8, 16, 16]),
                      in1=iota16.unsqueeze(1).unsqueeze(1).to_broadcast([128, 8, 16, 16]), op=ALU.is_equal,
                      r=[pfk, "iota16"], w=["sc"])
                    I("pool", "tensor_tensor", out=ohv, in0=ohv,
                      in1=i16v[:, :, which, :].unsqueeze(2).to_broadcast([128, 8, 16, 16]), op=ALU.mult,
                      r=["sc", "i16f"], w=["sc"])
                    I("dve", "tensor_reduce", out=IJG_tm[:, which, :].rearrange("p (h k) -> p h k", k=16),
                      in_=ohv, axis=AX.X, op=ALU.add, r=["sc"], w=[ijk])

            def p_tr(n, tt):
                tsl = slice(tt * 128, (tt + 1) * 128)
                IJG_tm = IJG_tms[tt]
                for k3 in range(3):
                    I("pe", "transpose", pj[:, k3 * 128:(k3 + 1) * 128], IJG_tm[:, k3, :], ident_f,
                      r=["IJG_tm%d" % tt, "ident_f"], w=["pj0", "pj1"])
                I("dve", "tensor_copy", IJG[:, :, tsl], pj[:, 0:384].rearrange("p (a t) -> p a t", t=128),
                  r=["pj0", "pj1"], w=["IJG"])

            def prologue_sched(n):
                sch = {}
                sch[0] = [lambda: p_load(n, 0)]
                sch[4] = [lambda: p_loadB(n, 0), lambda: p_load(n, 1)]
                sch[8] = [lambda: p_loadB(n, 1)]
                for hp in range(16):
                    sch[10 + hp] = [lambda hp=hp: p_q(n, hp)]
                sch[26] = [lambda: p_topk(n, 0)]
                sch[66] = [lambda: p_topk2(n, 0)]
                sch[84] = [lambda: p_topk(n, 1)]
                sch[122] = [lambda: p_topk2(n, 1)]
                return sch

            def tr_sched(n):
                return {2: [lambda: p_tr(n, 0)], 6: [lambda: p_tr(n, 1)]}

            def run_prologue(n):
                for sch in (prologue_sched(n), tr_sched(n)):
                    for k in sorted(sch):
                        for fn in sch[k]:
                            fn()

            def U_phase(n, sch):
                h2T = h2Ts[n % 2]
                for i in range(128):
                    bi = i % NWB
                    dkey = "wb%d" % bi
                    dma("sp" if i % 2 == 0 else "pool", dTb[bi], downT_s[i], dkey, r=[("dTs", i)], w=[dkey])
                    Sx = Sb[i % 2]
                    skey = "S%d" % (i % 2)
                    for c in range(8):
                        I("pe", "matmul", Sx[:, 0:256], lhsT=dTb[bi][:, c, :], rhs=h2T[:, c, :],
                          start=(c == 0), stop=(c == 7), r=[dkey, "h2T%d" % (n % 2)], w=[skey])
                    I("act", "activation", out=WT[:, i, :], in_=Sx[:, 0:256], func=AF.Gelu, r=[skey], w=[("WT", i)])
                    for fn in sch.get(i, ()):
                        fn()

            def G_phase(n):
                iob = iota_b.unsqueeze(1).to_broadcast([128, 16, 128])

                def build_oh(ci):
                    t0 = ci * 16
                    pb_ = ci % 2
                    I("dve", "tensor_tensor", out=A01s[pb_], in0=iob,
                      in1=IJG[:, 0, t0:t0 + 16].unsqueeze(2).to_broadcast([128, 16, 128]), op=ALU.is_equal,
                      r=["iota_b", "IJG"], w=["A01_%d" % pb_])
                    I("pool", "tensor_tensor", out=Ags[pb_], in0=A01s[pb_],
                      in1=IJG[:, 2, t0:t0 + 16].unsqueeze(2).to_broadcast([128, 16, 128]), op=ALU.mult,
                      r=["A01_%d" % pb_, "IJG"], w=["Ag_%d" % pb_])
                    I("dve", "tensor_tensor", out=B01s[pb_], in0=iob,
                      in1=IJG[:, 1, t0:t0 + 16].unsqueeze(2).to_broadcast([128, 16, 128]), op=ALU.is_equal,
                      r=["iota_b", "IJG"], w=["B01_%d" % pb_])

                def mm_evac(ci):
                    t0 = ci * 16
                    pb_ = ci % 2
                    for half in range(2):
                        akey = "acc%d" % half
                        accv = acc[half][:, :, :].rearrange("p a (b i) -> p (a b) i", i=128)
                        for tl in range(8):
                            tk = half * 8 + tl
                            I("pe", "matmul", accv[:, tl, :], lhsT=B01s[pb_][:, tk, :], rhs=Ags[pb_][:, tk, :],
                              start=True, stop=True, skip_group_check=True,
                              r=["B01_%d" % pb_, "Ag_%d" % pb_], w=[akey])
                        ts8 = slice(t0 + half * 8, t0 + half * 8 + 8)
                        I("dve", "tensor_tensor", out=WT[:, :, ts8], in0=accv.rearrange("p t i -> p i t"),
                          in1=WT[:, :, ts8], op=ALU.mult, r=[akey] + allWT, w=allWT)

                build_oh(0)
                for ci in range(16):
                    if ci + 1 < 16:
                        build_oh(ci + 1)
                    mm_evac(ci)

            def up_phase(n, sch):
                b, gi = groups[n]
                xg = x1gs[n % 2]
                xk = "x1g%d" % (n % 2)
                for i in range(128):
                    bi = (i + 2) % NWB
                    ukey = "wb%d" % bi
                    dma("sp" if i % 2 == 0 else "pool", upb[bi], up_s[i], ukey, r=[("ups", i)], w=[ukey])
                    for tt in range(2):
                        accf = acc[tt][:, :, :].rearrange("p a b -> p (a b)")
                        for hf in range(2):
                            I("pe", "matmul", accf[:, hf * 512:(hf + 1) * 512], lhsT=WT[:, i, tt * 128:(tt + 1) * 128],
                              rhs=upb[bi][:, hf * 512:(hf + 1) * 512], start=(i == 0), stop=(i == 127),
                              r=[("WT", i), ukey], w=["acc%d" % tt])
                    for fn in sch.get(i, ()):
                        fn()
                for tt in range(2):
                    t = gi * 2 + tt
                    rows = slice(t * 128, (t + 1) * 128)
                    accf = acc[tt][:, :, :].rearrange("p a b -> p (a b)")
                    I("dve", "tensor_tensor", out=xg[:, tt, :], in0=accf, in1=xg[:, tt, :], op=ALU.add,
                      r=["acc%d" % tt, xk], w=[xk])
                    o = dma("sp", out[b, rows, :], xg[:, tt, :], "yo%d" % (n % 2), r=[xk], w=[("out", b, t)])
                    final_ops.append(o)

            run_prologue(0)
            for n in range(len(groups)):
                more = n + 1 < len(groups)
                U_phase(n, prologue_sched(n + 1) if more else {})
                G_phase(n)
                up_phase(n, tr_sched(n + 1) if more else {})

        P.emit(final_ops)
    return nc


_NC_CACHE = {}


def kernel(**inputs):
    B = inputs["x"].shape[0]
    B_loc = B // N_CORES
    if B_loc not in _NC_CACHE:
        _NC_CACHE[B_loc] = build(B_loc)
    nc = _NC_CACHE[B_loc]
    consts = host_consts()
    f = lambda a: np.ascontiguousarray(np.asarray(a, dtype=np.float32))
    shared = {
        "attn_norm": f(inputs["attn_norm"]).reshape(1, D),
        "w_in": f(inputs["w_in"]).reshape(D, 3072),
        "q_norm_moba": f(inputs["q_norm_moba"]).reshape(1, 64),
        "k_norm_moba": f(inputs["k_norm_moba"]).reshape(1, 64),
        "q_norm_diff": f(inputs["q_norm_diff"]).reshape(2, 64),
        "k_norm_diff": f(inputs["k_norm_diff"]).reshape(2, 64),
        "lambda_q1": f(inputs["lambda_q1"]).reshape(1, 64),
        "lambda_k1": f(inputs["lambda_k1"]).reshape(1, 64),
        "lambda_q2": f(inputs["lambda_q2"]).reshape(1, 64),
        "lambda_k2": f(inputs["lambda_k2"]).reshape(1, 64),
        "moba_out_gain": f(inputs["moba_out_gain"]).reshape(1, 512),
        "diff_out_gain": f(inputs["diff_out_gain"]).reshape(1, 512),
        "w_out": f(inputs["w_out"]).reshape(D, D),
        "ffn_norm": f(inputs["ffn_norm"]).reshape(1, D),
        "peer_query": f(inputs["peer_query"]).reshape(D, 2048),
        "peer_sub_keys": f(inputs["peer_sub_keys"]).reshape(2048, 128),
        "peer_down": f(inputs["peer_down"]).reshape(16384, D),
        "peer_up": f(inputs["peer_up"]).reshape(16384, D),
    }
    shared.update(consts)
    xs = f(inputs["x"])
    in_maps = []
    for c in range(N_CORES):
        m = dict(shared)
        m["x"] = xs[c * B_loc:(c + 1) * B_loc]
        in_maps.append(m)
    res = run_bass_kernel_spmd(nc, in_maps, core_ids=list(range(N_CORES)))
    return np.concatenate([r["out"] for r in res.results], axis=0)
```
